# Optimizing a Trainium2 kernel written in Bass

```python
import jax, jax.numpy as jnp
from jax import lax
import numpy as np

D_MODEL = 2048
BATCH = 2
SEQ = 8192
DEPTH = 1

HEAD_DIM = 128
ATTN_WIDTH = D_MODEL // 2
N_ATTN_HEADS = ATTN_WIDTH // HEAD_DIM
POOL_WIDTH = D_MODEL - ATTN_WIDTH
POOL_SIZES = (2, 4, 8, 16)
N_POOL_GROUPS = len(POOL_SIZES)
POOL_GROUP = POOL_WIDTH // N_POOL_GROUPS
MIX_WIDTH = ATTN_WIDTH + POOL_WIDTH
IN_WIDTH = 3 * ATTN_WIDTH + POOL_WIDTH
DILATED_PATTERNS = ((128, 1), (512, 4), (2048, 16))
ATTN_BLOCK = 128
ROPE_THETA = 500000.0
ROPE_DIM = HEAD_DIM // 4
PEER_HEADS = 8
PEER_NKEYS = 128
PEER_EXPERTS = PEER_NKEYS * PEER_NKEYS
PEER_QDIM = 256
PEER_TOPK = 16
PEER_CHUNK = 128
NORM_EPS = 1e-6
NEG_BIG = -1e30

kernel_name = "hybrid_dilated_pool_peer_adaln"


def _rmsnorm(x, g):
    xf = x.astype(jnp.float32)
    y = xf * lax.rsqrt(jnp.mean(xf * xf, axis=-1, keepdims=True) + NORM_EPS)
    return (y * g.astype(jnp.float32)).astype(x.dtype)


def _modulate(h, shift, scale):
    return h * (1 + scale[:, None, :]) + shift[:, None, :]


def _rotary(x, positions):
    half = ROPE_DIM // 2
    inv = ROPE_THETA ** (-jnp.arange(half, dtype=jnp.float32) * 2.0 / ROPE_DIM)
    ang = positions.astype(jnp.float32)[..., None] * inv
    cos = jnp.cos(ang)[:, :, None, :]
    sin = jnp.sin(ang)[:, :, None, :]
    xf = x.astype(jnp.float32)
    x1 = xf[..., :half]
    x2 = xf[..., half:ROPE_DIM]
    out = jnp.concatenate([x1 * cos - x2 * sin, x2 * cos + x1 * sin, xf[..., ROPE_DIM:]], axis=-1)
    return out.astype(x.dtype)


def _dilated_window_attention(q, k, v, window, dil):
    B, H, S, hd = q.shape
    n_back = window // dil
    L = S // dil
    nb = -(-L // ATTN_BLOCK)
    Lp = nb * ATTN_BLOCK

    def to_blocks(a):
        a = a.reshape(B, H, L, dil, hd).transpose(0, 1, 3, 2, 4)
        a = jnp.pad(a, ((0, 0), (0, 0), (0, 0), (0, Lp - L), (0, 0)))
        return a.reshape(B, H, dil, nb, ATTN_BLOCK, hd)

    def with_prev(a):
        prev = jnp.pad(a, ((0, 0), (0, 0), (0, 0), (1, 0), (0, 0), (0, 0)))[:, :, :, :nb]
        return jnp.concatenate([prev, a], axis=4)

    qb, kb, vb = to_blocks(q), to_blocks(k), to_blocks(v)
    kk, vv = with_prev(kb), with_prev(vb)
    s = jnp.einsum('bhrnqd,bhrnkd->bhrnqk', qb, kk, preferred_element_type=jnp.float32)
    q_idx = jnp.arange(nb)[:, None, None] * ATTN_BLOCK + jnp.arange(ATTN_BLOCK)[None, :, None]
    k_idx = jnp.arange(nb)[:, None, None] * ATTN_BLOCK - ATTN_BLOCK + jnp.arange(2 * ATTN_BLOCK)[None, None, :]
    dist = q_idx - k_idx
    valid = (dist >= 0) & (dist <= n_back) & (k_idx >= 0)
    s = jnp.where(valid, s, NEG_BIG)
    m = jnp.max(s, axis=-1, keepdims=True)
    p = jnp.exp(s - m)
    den = jnp.sum(p, axis=-1, keepdims=True)
    o = jnp.einsum('bhrnqk,bhrnkd->bhrnqd', p, vv.astype(jnp.float32)) / den
    lse = (m + jnp.log(den))[..., 0]

    def from_blocks(a, tail):
        a = a.reshape((B, H, dil, Lp) + tail)[:, :, :, :L]
        a = jnp.moveaxis(a, 2, 3)
        return a.reshape((B, H, S) + tail)

    return from_blocks(o, (hd,)), from_blocks(lse, ())


def _mixed_dilated_attention(q, k, v):
    outs, lses = [], []
    for window, dil in DILATED_PATTERNS:
        o, l = _dilated_window_attention(q, k, v, window, dil)
        outs.append(o)
        lses.append(l)
    w = jax.nn.softmax(jnp.stack(lses, axis=0), axis=0)
    return jnp.einsum('pbhs,pbhsd->bhsd', w, jnp.stack(outs, axis=0))


def _multiscale_pool(u, w_pool, pool_scale):
    B, S, _ = u.shape
    uf = u.astype(jnp.float32).reshape(B, S, N_POOL_GROUPS, POOL_GROUP)
    cs = jnp.cumsum(uf, axis=1)
    t = jnp.arange(S)
    means = []
    for gi, p in enumerate(POOL_SIZES):
        c_g = cs[:, :, gi]
        prev = jnp.pad(c_g, ((0, 0), (p, 0), (0, 0)))[:, :S]
        cnt = jnp.minimum(t + 1, p).astype(jnp.float32)[None, :, None]
        means.append((c_g - prev) / cnt)
    r = jnp.stack(means, axis=2) - uf
    y = jnp.einsum('bsgc,gce->bsge', r, w_pool.astype(jnp.float32))
    return (y.reshape(B, S, POOL_WIDTH) * pool_scale.astype(jnp.float32)).astype(u.dtype)


def _peer(h, w_query, sub_keys, peer_u, peer_v):
    B, S, D = h.shape
    T = B * S
    xt = h.reshape(T, D)
    q = (xt @ w_query).reshape(T, PEER_HEADS, 2, PEER_QDIM // 2).astype(jnp.float32)
    scores = jnp.einsum('thpc,hpnc->thpn', q, sub_keys.astype(jnp.float32))
    s_half, i_half = lax.top_k(scores, PEER_TOPK)
    cand = (s_half[:, :, 0, :, None] + s_half[:, :, 1, None, :]).reshape(T, PEER_HEADS, PEER_TOPK * PEER_TOPK)
    cand_idx = (i_half[:, :, 0, :, None] * PEER_NKEYS + i_half[:, :, 1, None, :]).reshape(T, PEER_HEADS, PEER_TOPK * PEER_TOPK)
    top_s, pos = lax.top_k(cand, PEER_TOPK)
    idx = jnp.take_along_axis(cand_idx, pos, axis=-1)
    gate = jax.nn.softmax(top_s, axis=-1)
    n_chunks = T // PEER_CHUNK

    def chunk(args):
        xc, ic, gc = args
        u = peer_u[ic]
        a = jnp.einsum('cd,chkd->chk', xc, u, preferred_element_type=jnp.float32)
        act = jax.nn.gelu(a, approximate=False) * gc
        vg = peer_v[ic]
        return jnp.einsum('chk,chkd->cd', act, vg.astype(jnp.float32)).astype(h.dtype)

    y = lax.map(chunk, (xt.reshape(n_chunks, PEER_CHUNK, D),
                        idx.reshape(n_chunks, PEER_CHUNK, PEER_HEADS, PEER_TOPK),
                        gate.reshape(n_chunks, PEER_CHUNK, PEER_HEADS, PEER_TOPK)))
    return y.reshape(B, S, D)


def setup_inputs(seed: int = 0) -> dict:
    key = jax.random.key(seed)
    ks = jax.random.split(key, 16)
    f32 = jnp.float32
    x = jax.random.normal(ks[0], (BATCH, SEQ, D_MODEL), f32)
    c = jax.random.normal(ks[1], (BATCH, D_MODEL), f32)
    positions = (jnp.arange(SEQ, dtype=jnp.int32)[None, :]
                 + jax.random.randint(ks[2], (BATCH, 1), 0, 1024, dtype=jnp.int32))
    w_mod = jax.random.normal(ks[3], (DEPTH, D_MODEL, 6 * D_MODEL), f32) * D_MODEL ** -0.5
    b_mod = jax.random.normal(ks[4], (DEPTH, 6 * D_MODEL), f32) * 0.02
    norm1_g = 1.0 + 0.02 * jax.random.normal(ks[5], (DEPTH, D_MODEL), f32)
    w_in = jax.random.normal(ks[6], (DEPTH, D_MODEL, IN_WIDTH), f32) * D_MODEL ** -0.5
    w_pool = jax.random.normal(ks[7], (DEPTH, N_POOL_GROUPS, POOL_GROUP, POOL_GROUP), f32) * POOL_GROUP ** -0.5
    pool_scale = 1.0 + 0.1 * jax.random.normal(ks[8], (DEPTH, POOL_WIDTH), f32)
    w_out = jax.random.normal(ks[9], (DEPTH, MIX_WIDTH, D_MODEL), f32) * MIX_WIDTH ** -0.5
    norm2_g = 1.0 + 0.02 * jax.random.normal(ks[10], (DEPTH, D_MODEL), f32)
    w_query = jax.random.normal(ks[11], (DEPTH, D_MODEL, PEER_HEADS * PEER_QDIM), f32) * D_MODEL ** -0.5
    sub_keys = jax.random.normal(ks[12], (DEPTH, PEER_HEADS, 2, PEER_NKEYS, PEER_QDIM // 2), f32) * (PEER_QDIM // 2) ** -0.5
    peer_u = jax.random.normal(ks[13], (DEPTH, PEER_EXPERTS, D_MODEL), f32) * D_MODEL ** -0.5
    peer_v = jax.random.normal(ks[14], (DEPTH, PEER_EXPERTS, D_MODEL), f32) * PEER_HEADS ** -0.5
    final_g = 1.0 + 0.02 * jax.random.normal(ks[15], (D_MODEL,), f32)
    return {"x": x, "c": c, "positions": positions, "w_mod": w_mod, "b_mod": b_mod,
            "norm1_g": norm1_g, "w_in": w_in, "w_pool": w_pool, "pool_scale": pool_scale,
            "w_out": w_out, "norm2_g": norm2_g, "w_query": w_query, "sub_keys": sub_keys,
            "peer_u": peer_u, "peer_v": peer_v, "final_g": final_g}


def reference(x, c, positions, w_mod, b_mod, norm1_g, w_in, w_pool, pool_scale,
              w_out, norm2_g, w_query, sub_keys, peer_u, peer_v, final_g):
    B, S, D = x.shape
    A = ATTN_WIDTH
    for l in range(DEPTH):
        mod = jax.nn.silu(c) @ w_mod[l] + b_mod[l]
        shift1, scale1, gate1, shift2, scale2, gate2 = jnp.split(mod, 6, axis=-1)

        h = _modulate(_rmsnorm(x, norm1_g[l]), shift1, scale1)
        z = h @ w_in[l]
        q, k, v, u = jnp.split(z, [A, 2 * A, 3 * A], axis=-1)
        q = _rotary(q.reshape(B, S, N_ATTN_HEADS, HEAD_DIM), positions) * (HEAD_DIM ** -0.5)
        k = _rotary(k.reshape(B, S, N_ATTN_HEADS, HEAD_DIM), positions)
        v = v.reshape(B, S, N_ATTN_HEADS, HEAD_DIM)
        q, k, v = (t.transpose(0, 2, 1, 3) for t in (q, k, v))
        attn = _mixed_dilated_attention(q, k, v)
        attn = attn.transpose(0, 2, 1, 3).reshape(B, S, A).astype(x.dtype)
        pool = _multiscale_pool(u, w_pool[l], pool_scale[l])
        mix = jnp.concatenate([attn, pool], axis=-1) @ w_out[l]
        x = x + gate1[:, None, :] * mix

        h = _modulate(_rmsnorm(x, norm2_g[l]), shift2, scale2)
        x = x + gate2[:, None, :] * _peer(h, w_query[l], sub_keys[l], peer_u[l], peer_v[l])
    return _rmsnorm(x, final_g)
```

```python
import numpy as np
import concourse.bass as bass
import concourse.mybir as mybir
from concourse.bass_utils import run_bass_kernel_spmd
from contextlib import ExitStack

F32 = mybir.dt.float32
BF16 = mybir.dt.bfloat16
I32 = mybir.dt.int32
U32 = mybir.dt.uint32
AF = mybir.ActivationFunctionType
ALU = mybir.AluOpType
AX = mybir.AxisListType

NDS = 48
D = 2048
NT = 2048
NTL = 4096
NEG = -30000.0


class Buf:
    __slots__ = ("w", "r")

    def __init__(self):
        self.w = None
        self.r = []


def _compress(toks):
    best = {}
    for t in toks:
        key = (t[0], t[1])
        if key not in best or best[key][2] < t[2]:
            best[key] = t
    return list(best.values())


class KB:
    def __init__(self, nc):
        self.nc = nc
        self.engs = {"pe": nc.tensor, "dve": nc.vector, "act": nc.scalar,
                     "pool": nc.gpsimd, "sp": nc.sync}
        self.sem = {e: nc.alloc_semaphore(name="s_" + e) for e in self.engs}
        self.cnt = {e: 0 for e in self.engs}
        self.seen = {e: {} for e in self.engs}
        self.dsem = [nc.alloc_semaphore(name="d%d" % i) for i in range(NDS)]
        self.dcnt = [0] * NDS
        self.dnext = 0
        self.ninst = 0

    def _wait(self, eng, tok):
        kind, src, k = tok
        if kind == "e":
            if src == eng and eng == "pe":
                return
            key = src
            sem = self.sem[src]
        else:
            key = ("d", src)
            sem = self.dsem[src]
        if self.seen[eng].get(key, 0) >= k:
            return
        self.engs[eng].wait_ge(sem, k)
        self.seen[eng][key] = k

    def _deps(self, eng, reads, writes):
        deps = []
        for b in reads:
            if b.w is not None:
                deps.append(b.w)
        for b in writes:
            if b.w is not None:
                deps.append(b.w)
            deps.extend(b.r)
        for d in deps:
            self._wait(eng, d)

    def _mark(self, tok, reads, writes):
        for b in reads:
            b.r.append(tok)
            if len(b.r) > 48:
                b.r = _compress(b.r)
        for b in writes:
            b.w = tok
            b.r = []

    def op(self, eng, fn, reads=(), writes=()):
        self._deps(eng, reads, writes)
        ins = fn(self.engs[eng])
        self.cnt[eng] += 1
        ins.then_inc(self.sem[eng], 1)
        tok = ("e", eng, self.cnt[eng])
        self._mark(tok, reads, writes)
        self.ninst += 1
        return tok

    def dma(self, q, out, in_, reads=(), writes=(), **kw):
        self._deps(q, reads, writes)
        j = self.dnext
        self.dnext = (self.dnext + 1) % NDS
        if self.dcnt[j] > 0:
            self._wait(q, ("d", j, self.dcnt[j]))
        ins = self.engs[q].dma_start(out=out, in_=in_, **kw)
        self.dcnt[j] += 16
        ins.then_inc(self.dsem[j], 16)
        tok = ("d", j, self.dcnt[j])
        self._mark(tok, reads, writes)
        self.ninst += 1
        return tok

    def barrier(self, engines=None):
        engines = engines or list(self.engs)
        for e in engines:
            for s in self.engs:
                if self.cnt[s] > 0 and not (s == e and e == "pe"):
                    self._wait(e, ("e", s, self.cnt[s]))
            for j in range(NDS):
                if self.dcnt[j] > 0:
                    self._wait(e, ("d", j, self.dcnt[j]))


def build(debug=False, stop_after=None):
    nc = bass.Bass("TRN2", target_bir_lowering=False)

    def din(name, shape, dt=F32):
        return nc.dram_tensor(name, shape, dt, kind="ExternalInput").ap()

    def dscr(name, shape, dt=F32):
        return nc.dram_tensor(name, shape, dt, kind="Internal").ap()

    x_own = din("x_own", [NT, D])
    x_halo = din("x_halo", [NT, D])
    cT_d = din("cT", [128, 16])
    pos_d = din("pos", [128, 32], I32)
    w_mod = din("w_mod", [D, 6 * D])
    b_mod = din("b_mod", [1, 6 * D])
    g1_d = din("norm1_g", [1, D])
    w_in = din("w_in", [D, 4096])
    w_pool = din("w_pool", [4, 256, 256])
    psT_d = din("psT", [128, 8])
    w_out = din("w_out", [D, D])
    g2_d = din("norm2_g", [1, D])
    w_query = din("w_query", [D, D])
    skT_d = din("skT", [128, 16, 128])
    peer_u = din("peer_u", [16384, D])
    peer_v = din("peer_v", [16384, D])
    fg_d = din("final_g", [1, D])
    ident_d = din("ident", [128, 128])
    masks_d = din("masks", [128, 3, 128])
    inv2_d = din("inv2", [1, 16])
    invcnt_d = din("invcnt", [4, NT])
    flag_d = din("flag", [128, 1])
    iota_d = din("iota", [1, 128])
    out_d = nc.dram_tensor("out", [NT, D], F32, kind="ExternalOutput").ap()

    okind = "ExternalOutput" if debug else "Internal"
    MODB = dscr("MODB", [128, 6 * D])
    QTd = dscr("QTd", [8, 128, NT], BF16)
    KTd = dscr("KTd", [8, 128, NTL], BF16)
    Vd = dscr("Vd", [NTL, 1024], BF16)
    UTd = dscr("UTd", [8, 128, 128 + NT])
    X1d = nc.dram_tensor("X1d", [NT, D], F32, kind=okind).ap()
    H2Td = dscr("H2Td", [16, 128, NT], BF16)
    Q2Td = dscr("Q2Td", [16, 128, NT], BF16)
    Gd = dscr("Gd", [128, 128, NT], BF16)
    MIXd = dscr("MIXd", [16, 128, NT], BF16)

    kb = KB(nc)
    op = kb.op
    dma = kb.dma
    B = Buf

    def phase_end():
        kb.barrier()

    with ExitStack() as es0:
        def sb0(n, s, d):
            return es0.enter_context(nc.sbuf_tensor(n, s, d))

        identf = sb0("identf", [128, 128], F32)
        identb = sb0("identb", [128, 128], BF16)
        onesb = sb0("onesb", [128, 128], BF16)
        iota_f = sb0("iota_f", [128, 128], F32)
        flag = sb0("flag_s", [128, 1], F32)
        b_const = B()
        dma("sp", identf[:], ident_d[:, :], writes=[b_const])
        dma("sp", iota_f[:], iota_d.partition_broadcast(128), writes=[b_const])
        dma("sp", flag[:], flag_d[:, :], writes=[b_const])
        op("dve", lambda e: e.tensor_copy(out=identb[:], in_=identf[:]), [b_const], [b_const])
        op("dve", lambda e: e.memset(onesb[:], 1.0), [], [b_const])
        iota_b = sb0("iota_b", [128, 128], BF16)
        op("dve", lambda e: e.tensor_copy(out=iota_b[:], in_=iota_f[:]), [b_const], [b_const])
        CONST = [b_const]

        with ExitStack() as es:
            def sb(n, s, d):
                return es.enter_context(nc.sbuf_tensor(n, s, d))

            def ps(n, s, d):
                return es.enter_context(nc.psum_tensor(n, s, d))
            cT = sb("cT_s", [128, 16], F32)
            sc = sb("sc_s", [128, 16], F32)
            srep = sb("srep", [128, 16, 128], BF16)
            NWM = 3
            wm = [sb("wm%d" % i, [128, 16, 512], BF16) for i in range(NWM)]
            bwm = [B() for _ in range(NWM)]
            bmb = sb("bmb", [128, 6 * D], F32)
            bbmb = B()
            mo = [sb("mo%d" % i, [128, 512], F32) for i in range(2)]
            bmo = [B(), B()]
            pm = [ps("pm%d" % i, [128, 512], F32) for i in range(2)]
            bpm = [B(), B()]
            b0 = B()
            dma("sp", cT[:], cT_d[:, :], writes=[b0])
            dma("sp", bmb[:], b_mod.partition_broadcast(128), writes=[bbmb])
            op("act", lambda e: e.activation(out=sc[:], in_=cT[:], func=AF.Silu), [b0], [b0])
            op("dve", lambda e: e.tensor_copy(out=srep[:], in_=sc[:].unsqueeze(2).to_broadcast([128, 16, 128])), [b0], [b0])

            def lw(g):
                dma("pool", wm[g % NWM][:], w_mod[:, g * 512:(g + 1) * 512].rearrange("(c p) n -> p c n", p=128), writes=[bwm[g % NWM]])
            lw(0)
            lw(1)
            for g in range(24):
                s = g % 2
                w3 = g % NWM
                if g + 2 < 24:
                    lw(g + 2)
                for c in range(16):
                    op("pe", lambda e: e.matmul(pm[s][:], lhsT=srep[:, c, :], rhs=wm[w3][:, c, :], start=(c == 0), stop=(c == 15)),
                       [b0, bwm[w3]], [bpm[s]])
                op("dve", lambda e: e.tensor_tensor(out=mo[s][:], in0=pm[s][:], in1=bmb[:, g * 512:(g + 1) * 512], op=ALU.add),
                   [bpm[s], bbmb], [bmo[s]])
                dma("sp", MODB[:, g * 512:(g + 1) * 512], mo[s][:], reads=[bmo[s]])
            phase_end()
        if stop_after == 0:
            return nc

        with ExitStack() as esM:

            with ExitStack() as es:
                def sb(n, s, d):
                    return es.enter_context(nc.sbuf_tensor(n, s, d))

                def ps(n, s, d):
                    return es.enter_context(nc.psum_tensor(n, s, d))
                wib = sb("wib", [128, 16, 2048], BF16)
                bwib = B()

                def load_w(pas):
                    if pas == 0:
                        for c4 in range(4):
                            dma("pool", wib[:, c4 * 4:(c4 + 1) * 4, :],
                                w_in[c4 * 512:(c4 + 1) * 512, 1024:3072].rearrange("(c p) n -> p c n", p=128), writes=[bwib])
                    else:
                        for c4 in range(4):
                            dma("pool", wib[:, c4 * 4:(c4 + 1) * 4, 0:1024],
                                w_in[c4 * 512:(c4 + 1) * 512, 0:1024].rearrange("(c p) n -> p c n", p=128), writes=[bwib])
                            dma("pool", wib[:, c4 * 4:(c4 + 1) * 4, 1024:2048],
                                w_in[c4 * 512:(c4 + 1) * 512, 3072:4096].rearrange("(c p) n -> p c n", p=128), writes=[bwib])
                A1 = sb("A1", [128, D], F32)
                B1 = sb("B1", [128, D], F32)
                bA = B()
                g1b = sb("g1b", [128, D], F32)
                dma("sp", A1[:, 0:D], MODB[:, D:2 * D], writes=[bA])
                dma("sp", B1[:, 0:D], MODB[:, 0:D], writes=[bA])
                dma("sp", g1b[:], g1_d.partition_broadcast(128), writes=[bA])
                op("dve", lambda e: e.scalar_tensor_tensor(out=A1[:, 0:D], in0=A1[:, 0:D], scalar=1.0, in1=g1b[:],
                                                           op0=ALU.add, op1=ALU.mult), [bA], [bA])
                posi = sb("posi", [128, 32], I32)
                posf = sb("posf", [128, 32], F32)
                inv2 = sb("inv2s", [128, 16], F32)
                uu = sb("uu", [128, 32, 32], F32)
                ki = sb("ki", [128, 32, 32], I32)
                kf = sb("kf", [128, 32, 32], F32)
                tab = sb("tab", [128, 32, 32], F32)
                tabq = sb("tabq", [128, 32, 32], F32)
                bt = B()
                dma("sp", posi[:], pos_d[:, :], writes=[bt])
                dma("sp", inv2[:], inv2_d.partition_broadcast(128), writes=[bt])
                T = [bt]
                op("dve", lambda e: e.tensor_copy(out=posf[:], in_=posi[:]), T, T)
                op("dve", lambda e: e.tensor_tensor(out=uu[:, :, 0:16], in0=posf[:].unsqueeze(2).to_broadcast([128, 32, 16]),
                                                    in1=inv2[:].unsqueeze(1).to_broadcast([128, 32, 16]), op=ALU.mult), T, T)
                op("dve", lambda e: e.tensor_scalar(out=uu[:, :, 16:32], in0=uu[:, :, 0:16], scalar1=0.25, scalar2=None, op0=ALU.add), T, T)
                op("dve", lambda e: e.tensor_copy(out=ki[:], in_=uu[:]), T, T)
                op("dve", lambda e: e.tensor_copy(out=kf[:], in_=ki[:]), T, T)
                op("dve", lambda e: e.tensor_tensor(out=uu[:], in0=uu[:], in1=kf[:], op=ALU.subtract), T, T)
                op("dve", lambda e: e.tensor_single_scalar(out=kf[:], in_=uu[:], scalar=0.5, op=ALU.is_gt), T, T)
                op("dve", lambda e: e.tensor_tensor(out=uu[:], in0=uu[:], in1=kf[:], op=ALU.subtract), T, T)
                op("dve", lambda e: e.tensor_single_scalar(out=kf[:], in_=uu[:], scalar=-0.5, op=ALU.is_lt), T, T)
                op("dve", lambda e: e.tensor_tensor(out=uu[:], in0=uu[:], in1=kf[:], op=ALU.add), T, T)
                op("act", lambda e: e.activation(out=tab[:], in_=uu[:], func=AF.Sin, scale=2 * np.pi), T, T)
                op("dve", lambda e: e.tensor_scalar(out=tabq[:], in0=tab[:], scalar1=128 ** -0.5, scalar2=None, op0=ALU.mult), T, T)

                xt = [sb("xt%d" % i, [128, D], F32) for i in range(2)]
                bxt = [B(), B()]
                ss = [sb("ss%d" % i, [128, 1], F32) for i in range(2)]
                bss = [B(), B()]
                hf = [g1b, sb("hf1", [128, D], F32)]
                bhf = [bA, B()]
                hb = [sb("hb%d" % i, [128, D], BF16) for i in range(2)]
                bhb = [B(), B()]
                hT = [sb("hT%d" % i, [128, 16, 128], BF16) for i in range(2)]
                bhT = [B(), B()]
                zr = [sb("zr%d" % i, [128, 1024], F32) for i in range(2)]
                bzr = [B(), B()]
                zu = sb("zu", [128, 1024], F32)
                bzu = B()
                vb = [sb("vb%d" % i, [128, 1024], BF16) for i in range(2)]
                bvb = [B(), B()]
                rb = [sb("rb%d" % i, [128, 1024], BF16) for i in range(2)]
                brb = [B(), B()]
                rt = [sb("rt%d" % i, [128, 8, 16], F32) for i in range(4)]
                brt = B()
                rT = [sb("rTa%d" % i, [128, 8, 128], BF16) for i in range(2)]
                uT = [sb("uT%d" % i, [128, 8, 128], F32) for i in range(2)]
                brT, buT = [B(), B()], [B(), B()]
                pT = ps("pT", [128, 16 * 128], BF16)
                bpT = B()
                pz = [ps("pz%d" % i, [128, 512], F32) for i in range(3)]
                bpz = [B(), B(), B()]
                pqk = ps("pqk", [128, 8 * 128], BF16)
                bpqk = B()
                pu = ps("pu", [128, 8 * 128], F32)
                bpu = B()
                zc = [0]

                def Nn(pas, lt, s):
                    own = lt >= 16
                    src = x_own[(lt - 16) * 128:(lt - 15) * 128, :] if own else x_halo[lt * 128:(lt + 1) * 128, :]
                    dma("sp", xt[s][:], src, writes=[bxt[s]])
                    op("act", lambda e: e.activation(out=hf[s][:], in_=xt[s][:], func=AF.Square, accum_out=ss[s][:]),
                       [bxt[s]], [bhf[s], bss[s]])
                    op("act", lambda e: e.activation(out=ss[s][:], in_=ss[s][:], func=AF.Sqrt, scale=1.0 / D, bias=1e-6),
                       [bss[s]], [bss[s]])
                    op("dve", lambda e: e.reciprocal(out=ss[s][:], in_=ss[s][:]), [bss[s]], [bss[s]])
                    op("dve", lambda e: e.scalar_tensor_tensor(out=hf[s][:], in0=xt[s][:], scalar=ss[s][:], in1=A1[:, 0:D],
                                                               op0=ALU.mult, op1=ALU.mult), [bxt[s], bss[s], bA], [bhf[s]])
                    op("pool", lambda e: e.tensor_tensor(out=hb[s][:], in0=hf[s][:], in1=B1[:, 0:D], op=ALU.add), [bhf[s], bA], [bhb[s]])

                def TH(pas, lt, s):
                    for c in range(16):
                        op("pe", lambda e: e.transpose(out=pT[:, c * 128:(c + 1) * 128], in_=hb[s][:, c * 128:(c + 1) * 128], identity=identb[:]),
                           [bhb[s]] + CONST, [bpT])
                    op("act", lambda e: e.copy(out=hT[s][:].rearrange("p c t -> p (c t)"), in_=pT[:]), [bpT], [bhT[s]])

                def MM(pas, lt, s):
                    own = lt >= 16
                    if pas == 0:
                        groups = [(0, "r"), (1, "r"), (2, "v"), (3, "v")]
                    else:
                        groups = [(0, "r"), (1, "r"), (2, "u"), (3, "u")] if own else [(2, "u"), (3, "u")]
                    for wc, kind in groups:
                        z = zc[0] % 3
                        zc[0] += 1
                        for c in range(16):
                            op("pe", lambda e: e.matmul(pz[z][:], lhsT=hT[s][:, c, :], rhs=wib[:, c, wc * 512:(wc + 1) * 512],
                                                        start=(c == 0), stop=(c == 15)), [bhT[s], bwib], [bpz[z]])
                        half = (wc % 2) * 512
                        if kind == "r":
                            op("act", lambda e: e.copy(out=zr[s][:, half:half + 512], in_=pz[z][:]), [bpz[z]], [bzr[s]])
                        elif kind == "v":
                            op("act", lambda e: e.copy(out=vb[s][:, half:half + 512], in_=pz[z][:]), [bpz[z]], [bvb[s]])
                        else:
                            op("act", lambda e: e.copy(out=zu[:, half:half + 512], in_=pz[z][:]), [bpz[z]], [bzu])
                        if kind == "r" and wc == 1:
                            rot(pas, lt, s)
                    if pas == 0:
                        dma("sp", Vd[lt * 128:(lt + 1) * 128, :], vb[s][:], reads=[bvb[s]])

                def rot(pas, lt, s):
                    table, scale = (tab, 1.0) if pas == 0 else (tabq, 128 ** -0.5)
                    zv = zr[s][:].rearrange("p (h d) -> p h d", h=8)
                    dv = rb[s][:].rearrange("p (h d) -> p h d", h=8)
                    sn = table[:, lt, 0:16].unsqueeze(1).to_broadcast([128, 8, 16])
                    cs = table[:, lt, 16:32].unsqueeze(1).to_broadcast([128, 8, 16])
                    x1 = zv[:, :, 0:16]
                    x2 = zv[:, :, 16:32]
                    R = [bzr[s], bt, brt]
                    op("dve", lambda e: e.tensor_tensor(out=rt[0][:], in0=x1, in1=cs, op=ALU.mult), R, [brt])
                    op("dve", lambda e: e.tensor_tensor(out=rt[1][:], in0=x2, in1=sn, op=ALU.mult), R, [brt])
                    op("dve", lambda e: e.tensor_tensor(out=rt[2][:], in0=x2, in1=cs, op=ALU.mult), R, [brt])
                    op("dve", lambda e: e.tensor_tensor(out=rt[3][:], in0=x1, in1=sn, op=ALU.mult), R, [brt])
                    op("dve", lambda e: e.tensor_tensor(out=dv[:, :, 0:16], in0=rt[0][:], in1=rt[1][:], op=ALU.subtract), [brt], [brb[s]])
                    op("dve", lambda e: e.tensor_tensor(out=dv[:, :, 16:32], in0=rt[2][:], in1=rt[3][:], op=ALU.add), [brt], [brb[s]])
                    op("act", lambda e: e.activation(out=dv[:, :, 32:128], in_=zv[:, :, 32:128], func=AF.Copy, scale=scale), [bzr[s]], [brb[s]])

                def RO(pas, lt, s):
                    own = lt >= 16
                    if pas == 0 or own:
                        for h in range(8):
                            op("pe", lambda e: e.transpose(out=pqk[:, h * 128:(h + 1) * 128], in_=rb[s][:, h * 128:(h + 1) * 128], identity=identb[:]),
                               [brb[s]] + CONST, [bpqk])
                        op("act", lambda e: e.copy(out=rT[s][:].rearrange("p h t -> p (h t)"), in_=pqk[:]), [bpqk], [brT[s]])
                        if pas == 0:
                            dma("sp", KTd[:, :, lt * 128:(lt + 1) * 128].rearrange("h d t -> d h t"), rT[s][:], reads=[brT[s]])
                        else:
                            dma("sp", QTd[:, :, (lt - 16) * 128:(lt - 15) * 128].rearrange("h d t -> d h t"), rT[s][:], reads=[brT[s]])
                    if pas == 1:
                        for j in range(8):
                            op("pe", lambda e: e.transpose(out=pu[:, j * 128:(j + 1) * 128], in_=zu[:, j * 128:(j + 1) * 128], identity=identf[:]),
                               [bzu] + CONST, [bpu])
                        op("act", lambda e: e.copy(out=uT[s][:].rearrange("p c t -> p (c t)"), in_=pu[:]), [bpu], [buT[s]])
                        dma("sp", UTd[:, :, (lt - 15) * 128:(lt - 14) * 128].rearrange("c p t -> p c t"), uT[s][:], reads=[buT[s]])

                for pas in (0, 1):
                    load_w(pas)
                    tl_ = list(range(32)) if pas == 0 else list(range(15, 32))
                    n_ = len(tl_)
                    Nn(pas, tl_[0], 0)
                    TH(pas, tl_[0], 0)
                    if n_ > 1:
                        Nn(pas, tl_[1], 1)
                    for k in range(n_):
                        s = k % 2
                        MM(pas, tl_[k], s)
                        if k + 1 < n_:
                            TH(pas, tl_[k + 1], (k + 1) % 2)
                        if k + 2 < n_:
                            Nn(pas, tl_[k + 2], s)
                        RO(pas, tl_[k], s)
                phase_end()
            if stop_after == 1:
                return nc

            with ExitStack() as es:
                def sb(n, s, d):
                    return es.enter_context(nc.sbuf_tensor(n, s, d))

                def ps(n, s, d):
                    return es.enter_context(nc.psum_tensor(n, s, d))
                maskf = sb("maskf", [128, 3, 128], F32)
                maskb = sb("maskb", [128, 3, 128], BF16)
                bmk = B()
                dma("sp", maskf[:], masks_d[:, :, :], writes=[bmk])
                op("dve", lambda e: e.tensor_copy(out=maskb[:], in_=maskf[:]), [bmk], [bmk])
                QT = [sb("QT%d" % i, [128, NT], BF16) for i in range(2)]
                KT = [sb("KT%d" % i, [128, NTL], BF16) for i in range(2)]
                V1 = [sb("V1_%d" % i, [128, 17, 128], BF16) for i in range(2)]
                V4 = [sb("V4_%d" % i, [128, 4, 5, 128], BF16) for i in range(2)]
                V16 = [sb("V16_%d" % i, [128, 16, 2, 128], BF16) for i in range(2)]
                bld = [B(), B()]
                accs = [sb("acc%d" % i, [128, 2, NT], F32) for i in range(2)]
                baccs = [B(), B()]
                rec = sb("rec", [128, NT], F32)
                brec = B()
                mixh = [sb("mixh%d" % i, [128, NT], BF16) for i in range(2)]
                bmixh = [B(), B()]
                PT = [sb("PT%d" % i, [128, 2, 128], BF16) for i in range(2)]
                bPT = [B(), B()]
                pS = [ps("pS%d" % i, [128, 512], F32) for i in range(2)]
                bpS = [B(), B()]
                pO = [ps("pO%d" % i, [128, 512], F32) for i in range(2)]
                bpO = [B(), B()]
                def loads(h):
                    s = h % 2
                    W = [bld[s]]
                    dma("sp", QT[s][:], QTd[h, :, :], writes=W)
                    dma("sp", KT[s][:], KTd[h, :, :], writes=W)
                    hc = slice(h * 128, (h + 1) * 128)
                    dma("sp", V1[s][:], Vd[1920:4096, hc].rearrange("(b k) d -> k b d", k=128), writes=W)
                    v4src = Vd[1536:4096, hc].rearrange("(b k r) d -> r k b d", k=128, r=4)
                    for r in range(4):
                        dma("sp", V4[s][:, r, :, :], v4src[r], writes=W)
                    v16src = Vd[0:4096, hc].rearrange("(b k r) d -> r k b d", k=128, r=16)
                    for r in range(16):
                        dma("sp", V16[s][:, r, :, :], v16src[r], writes=W)

                def iters(h):
                    s = h % 2
                    out = []
                    for dil in (1, 4, 16):
                        nblk = NT // (128 * dil)
                        qv = QT[s][:].rearrange("d (n q r) -> d r n q", r=dil, q=128)
                        kv = KT[s][:].rearrange("d (b k r) -> d r b k", r=dil, k=128)
                        av = accs[s][:].rearrange("d s (n q r) -> d r n s q", r=dil, q=128)
                        for r in range(dil):
                            for n in range(nblk):
                                bo = nblk + n
                                bp = bo - 1
                                if dil == 1:
                                    vp, vo = V1[s][:, bp - 15, :], V1[s][:, bo - 15, :]
                                elif dil == 4:
                                    vp, vo = V4[s][:, r, bp - 3, :], V4[s][:, r, bo - 3, :]
                                else:
                                    vp, vo = V16[s][:, r, bp, :], V16[s][:, r, bo, :]
                                out.append(dict(s=s, q=qv[:, r, n, :], kp=kv[:, r, bp, :], ko=kv[:, r, bo, :], vp=vp, vo=vo,
                                                mprev=(maskb[:, 2, :] if n == 0 else maskb[:, 1, :]), acc=av[:, r, n, :, :], first=(dil == 1)))
                    return out

                def Sst(it, z):
                    s = it["s"]
                    R = [bld[s], bmk] + CONST
                    op("pe", lambda e: e.matmul(pS[z][:, 0:128], lhsT=it["kp"], rhs=it["q"], start=True, stop=False), R, [bpS[z]])
                    op("pe", lambda e: e.matmul(pS[z][:, 0:128], lhsT=identb[:], rhs=it["mprev"], start=False, stop=True), R, [bpS[z]])
                    op("pe", lambda e: e.matmul(pS[z][:, 128:256], lhsT=it["ko"], rhs=it["q"], start=True, stop=False), R, [bpS[z]])
                    op("pe", lambda e: e.matmul(pS[z][:, 128:256], lhsT=identb[:], rhs=maskb[:, 0, :], start=False, stop=True), R, [bpS[z]])
                    op("act", lambda e: e.activation(out=PT[z][:].rearrange("p a b -> p (a b)"), in_=pS[z][:, 0:256], func=AF.Exp),
                       [bpS[z]], [bPT[z]])

                def PVst(it, z):
                    s = it["s"]
                    R2 = [bld[s], bPT[z]] + CONST
                    op("pe", lambda e: e.matmul(pO[z][:, 0:128], lhsT=it["vp"], rhs=PT[z][:, 0, :], start=True, stop=False), R2, [bpO[z]])
                    op("pe", lambda e: e.matmul(pO[z][:, 0:128], lhsT=it["vo"], rhs=PT[z][:, 1, :], start=False, stop=True), R2, [bpO[z]])
                    op("pe", lambda e: e.matmul(pO[z][:, 128:256], lhsT=onesb[:], rhs=PT[z][:, 0, :], start=True, stop=False), R2, [bpO[z]])
                    op("pe", lambda e: e.matmul(pO[z][:, 128:256], lhsT=onesb[:], rhs=PT[z][:, 1, :], start=False, stop=True), R2, [bpO[z]])
                    po_v = pO[z][:, 0:256].rearrange("p (s q) -> p s q", s=2)
                    bacc = baccs[s]
                    if it["first"]:
                        op("dve", lambda e: e.tensor_copy(out=it["acc"], in_=po_v), [bpO[z]], [bacc])
                    else:
                        op("dve", lambda e: e.tensor_tensor(out=it["acc"], in0=po_v, in1=it["acc"], op=ALU.add), [bpO[z], bacc], [bacc])

                loads(0)
                for h in range(8):
                    s = h % 2
                    if h + 1 < 8:
                        loads(h + 1)
                    its = iters(h)
                    NI = len(its)
                    Sst(its[0], 0)
                    Sst(its[1], 1)
                    for i in range(NI):
                        PVst(its[i], i % 3 if False else i % 2)
                        if i + 2 < NI:
                            Sst(its[i + 2], i % 2)
                    op("act", lambda e: e.activation(out=rec[:], in_=accs[s][:, 1, :], func=AF.Ln), [baccs[s]], [brec])
                    op("act", lambda e: e.activation(out=rec[:], in_=rec[:], func=AF.Exp, scale=-1.0), [brec], [brec])
                    op("pool", lambda e: e.tensor_tensor(out=mixh[s][:], in0=accs[s][:, 0, :], in1=rec[:], op=ALU.mult),
                       [baccs[s], brec], [bmixh[s]])
                    dma("sp", MIXd[h, :, :], mixh[s][:], reads=[bmixh[s]])
                phase_end()
            if stop_after == 2:
                return nc

            with ExitStack() as es:
                def sb(n, s, d):
                    return es.enter_context(nc.sbuf_tensor(n, s, d))

                def ps(n, s, d):
                    return es.enter_context(nc.psum_tensor(n, s, d))
                NU = 128 + NT
                psT = sb("psT_s", [128, 8], F32)
                bps = B()
                dma("sp", psT[:], psT_d[:, :], writes=[bps])
                ut = [sb("ut%d" % i, [128, NU], F32) for i in range(2)]
                but = [B(), B()]
                sa = sb("sa", [128, NU], F32)
                sbb = sb("sbb", [128, NU], F32)
                bsa, bsb = B(), B()
                icb = sb("icb", [128, NT], F32)
                bic = B()
                tmp = sb("ptmp", [128, NT], F32)
                btmp = B()
                rT = [sb("rT%d" % i, [128, NT], BF16) for i in range(2)]
                brT = [B(), B()]
                wpf = sb("wpf", [128, 2, 256], F32)
                wpb = sb("wpb", [128, 2, 256], BF16)
                bwp = B()
                bwpb = B()
                pp = [ps("pp%d" % i, [128, 512], F32) for i in range(2)]
                bpp = [B(), B()]
                mo_c = [sb("mo_c%d" % i, [128, NT], BF16) for i in range(2)]
                bmo_c = [B(), B()]
                pit = 0
                for g in range(4):
                    p = (2, 4, 8, 16)[g]
                    dma("sp", wpf[:], w_pool[g].rearrange("(cc p) e -> p cc e", p=128), writes=[bwp])
                    op("dve", lambda e: e.tensor_copy(out=wpb[:], in_=wpf[:]), [bwp], [bwpb])
                    dma("sp", icb[:], invcnt_d[g:g + 1, :].partition_broadcast(128), writes=[bic])
                    for cc in range(2):
                        ch = 2 * g + cc
                        u = ut[cc]
                        dma("sp", u[:], UTd[ch, :, :], writes=[but[cc]])
                        op("dve", lambda e: e.tensor_scalar(out=u[:, 0:128], in0=u[:, 0:128], scalar1=flag[:], scalar2=None, op0=ALU.mult),
                           [but[cc]] + CONST, [but[cc]])
                        cur, bcur = u, but[cc]
                        nxt = [(sa, bsa), (sbb, bsb)]
                        step = 1
                        k = 0
                        while step < p:
                            lo = 2 * step - 1
                            dst, bdst = nxt[k % 2]
                            k += 1
                            eng = "dve"
                            op(eng, lambda e: e.tensor_tensor(out=dst[:, lo:NU], in0=cur[:, lo:NU], in1=cur[:, lo - step:NU - step], op=ALU.add),
                               [bcur], [bdst])
                            cur, bcur = dst, bdst
                            step *= 2
                        op("dve", lambda e: e.tensor_tensor(out=tmp[:], in0=cur[:, 128:NU], in1=icb[:], op=ALU.mult), [bcur, bic], [btmp])
                        op("pool", lambda e: e.tensor_tensor(out=rT[cc][:], in0=tmp[:], in1=u[:, 128:NU], op=ALU.subtract),
                           [btmp, but[cc]], [brT[cc]])
                    for ec in range(2):
                        for tg in range(4):
                            z = pit % 2
                            pit += 1
                            for cc in range(2):
                                op("pe", lambda e: e.matmul(pp[z][:], lhsT=wpb[:, cc, ec * 128:(ec + 1) * 128], rhs=rT[cc][:, tg * 512:(tg + 1) * 512],
                                                            start=(cc == 0), stop=(cc == 1)), [bwpb, brT[cc]], [bpp[z]])
                            chn = 8 + 2 * g + ec
                            op("act", lambda e: e.activation(out=mo_c[ec][:, tg * 512:(tg + 1) * 512], in_=pp[z][:], func=AF.Copy,
                                                             scale=psT[:, 2 * g + ec:2 * g + ec + 1]), [bpp[z], bps], [bmo_c[ec]])
                        dma("sp", MIXd[8 + 2 * g + ec, :, :], mo_c[ec][:], reads=[bmo_c[ec]])
                phase_end()
            if stop_after == 3:
                return nc

            with ExitStack() as es:
                def sb(n, s, d):
                    return es.enter_context(nc.sbuf_tensor(n, s, d))

                def ps(n, s, d):
                    return es.enter_context(nc.psum_tensor(n, s, d))
                wob = sb("wob", [128, 16, D], BF16)
                bwob = B()
                for c4 in range(4):
                    dma("pool", wob[:, c4 * 4:(c4 + 1) * 4, :], w_out[c4 * 512:(c4 + 1) * 512, :].rearrange("(c p) n -> p c n", p=128), writes=[bwob])
                mixT = sb("mixT", [128, 16, NT], BF16)
                bmix = [B() for _ in range(16)]
                for c in range(16):
                    dma("act", mixT[:, c, :], MIXd[c, :, :], writes=[bmix[c]])
                gate1 = sb("gate1", [128, D], F32)
                bg1 = B()
                dma("sp", gate1[:], MODB[:, 2 * D:3 * D], writes=[bg1])
                xt = [sb("xtd%d" % i, [128, D], F32) for i in range(2)]
                bxt = [B(), B()]
                t1 = [sb("t1d%d" % i, [128, D], F32) for i in range(2)]
                bt1 = [B(), B()]
                po = [ps("po%d" % i, [128, 512], F32) for i in range(4)]
                bpo = [B() for _ in range(4)]
                for tt in range(16):
                    s = tt % 2
                    dma("act", xt[s][:], x_own[tt * 128:(tt + 1) * 128, :], writes=[bxt[s]])
                    for ng in range(4):
                        for c in range(16):
                            op("pe", lambda e: e.matmul(po[ng][:], lhsT=mixT[:, c, tt * 128:(tt + 1) * 128], rhs=wob[:, c, ng * 512:(ng + 1) * 512],
                                                        start=(c == 0), stop=(c == 15)), [bmix[c], bwob], [bpo[ng]])
                        op("dve", lambda e: e.tensor_tensor(out=t1[s][:, ng * 512:(ng + 1) * 512], in0=po[ng][:], in1=gate1[:, ng * 512:(ng + 1) * 512],
                                                            op=ALU.mult), [bpo[ng], bg1], [bt1[s]])
                    op("pool", lambda e: e.tensor_tensor(out=t1[s][:], in0=t1[s][:], in1=xt[s][:], op=ALU.add), [bt1[s], bxt[s]], [bt1[s]])
                    dma("sp", X1d[tt * 128:(tt + 1) * 128, :], t1[s][:], reads=[bt1[s]])
                phase_end()
        if stop_after == 4:
            return nc

        with ExitStack() as es:
            def sb(n, s, d):
                return es.enter_context(nc.sbuf_tensor(n, s, d))

            def ps(n, s, d):
                return es.enter_context(nc.psum_tensor(n, s, d))
            wqb = sb("wqb", [128, 16, D], BF16)
            bwqb = B()
            for c4 in range(4):
                dma("pool", wqb[:, c4 * 4:(c4 + 1) * 4, :], w_query[c4 * 512:(c4 + 1) * 512, :].rearrange("(c p) n -> p c n", p=128), writes=[bwqb])
            A2 = sb("A2", [128, D], F32)
            B2 = sb("B2", [128, D], F32)
            g2b = sb("g2b", [128, D], F32)
            bA = B()
            dma("sp", A2[:], MODB[:, 4 * D:5 * D], writes=[bA])
            dma("sp", B2[:], MODB[:, 3 * D:4 * D], writes=[bA])
            dma("sp", g2b[:], g2_d.partition_broadcast(128), writes=[bA])
            op("dve", lambda e: e.scalar_tensor_tensor(out=A2[:], in0=A2[:], scalar=1.0, in1=g2b[:], op0=ALU.add, op1=ALU.mult), [bA], [bA])
            xt = [sb("xte%d" % i, [128, D], F32) for i in range(2)]
            bxt = [B(), B()]
            ss = [sb("sse%d" % i, [128, 1], F32) for i in range(2)]
            bss = [B(), B()]
            hf = [g2b, sb("hfe1", [128, D], F32)]
            bhf = [bA, B()]
            hb = [sb("hbe%d" % i, [128, D], BF16) for i in range(2)]
            bhb = [B(), B()]
            hT4 = [sb("hT4_%d" % i, [128, 16, 512], BF16) for i in range(2)]
            bhT4 = [B(), B()]
            q2 = [sb("q2e%d" % i, [128, 4, 512], BF16) for i in range(2)]
            bq2 = [B(), B()]
            pT = [ps("pTe%d" % i, [128, 16 * 128], BF16) for i in range(2)]
            bpT = [B(), B()]
            pq = [ps("pqe%d" % i, [128, 512], F32) for i in range(4)]
            bpq = [B() for _ in range(4)]

            def Nn(tt):
                s = tt % 2
                dma("sp", xt[s][:], X1d[tt * 128:(tt + 1) * 128, :], writes=[bxt[s]])
                op("act", lambda e: e.activation(out=hf[s][:], in_=xt[s][:], func=AF.Square, accum_out=ss[s][:]), [bxt[s]], [bhf[s], bss[s]])
                op("act", lambda e: e.activation(out=ss[s][:], in_=ss[s][:], func=AF.Sqrt, scale=1.0 / D, bias=1e-6), [bss[s]], [bss[s]])
                op("dve", lambda e: e.reciprocal(out=ss[s][:], in_=ss[s][:]), [bss[s]], [bss[s]])
                op("dve", lambda e: e.scalar_tensor_tensor(out=hf[s][:], in0=xt[s][:], scalar=ss[s][:], in1=A2[:], op0=ALU.mult, op1=ALU.mult),
                   [bxt[s], bss[s], bA], [bhf[s]])
                op("pool", lambda e: e.tensor_tensor(out=hb[s][:], in0=hf[s][:], in1=B2[:], op=ALU.add), [bhf[s], bA], [bhb[s]])

            def TH(tt):
                s = tt % 2
                g = (tt // 4) % 2
                j = tt % 4
                for c in range(16):
                    op("pe", lambda e: e.transpose(out=pT[s][:, c * 128:(c + 1) * 128], in_=hb[s][:, c * 128:(c + 1) * 128], identity=identb[:]),
                       [bhb[s]] + CONST, [bpT[s]])
                op("act", lambda e: e.copy(out=hT4[g][:, :, j * 128:(j + 1) * 128], in_=pT[s][:].rearrange("p (c t) -> p c t", c=16)), [bpT[s]], [bhT4[g]])

            def QM(grp):
                g = grp % 2
                dma("sp", H2Td[:, :, grp * 512:(grp + 1) * 512].rearrange("c p t -> p c t"), hT4[g][:], reads=[bhT4[g]])
                for hp4 in range(4):
                    z = hp4 % 2
                    for j in range(4):
                        hp = hp4 * 4 + j
                        for c in range(16):
                            op("pe", lambda e: e.matmul(pq[j][:], lhsT=wqb[:, c, hp * 128:(hp + 1) * 128], rhs=hT4[g][:, c, :],
                                                        start=(c == 0), stop=(c == 15)), [bwqb, bhT4[g]], [bpq[j]])
                        op("act", lambda e: e.copy(out=q2[z][:, j, :], in_=pq[j][:]), [bpq[j]], [bq2[z]])
                    dma("sp", Q2Td[hp4 * 4:(hp4 + 1) * 4, :, grp * 512:(grp + 1) * 512].rearrange("c p t -> p c t"), q2[z][:], reads=[bq2[z]])

            Nn(0)
            Nn(1)
            for tt in range(16):
                TH(tt)
                if tt + 2 < 16:
                    Nn(tt + 2)
                if tt % 4 == 3:
                    QM(tt // 4)
            phase_end()
        if stop_after == 5:
            return nc

        with ExitStack() as es:
            def sb(n, s, d):
                return es.enter_context(nc.sbuf_tensor(n, s, d))

            def ps(n, s, d):
                return es.enter_context(nc.psum_tensor(n, s, d))
            skb = sb("skb", [128, 16, 128], BF16)
            bsk = B()
            dma("pool", skb[:], skT_d[:, :, :], writes=[bsk])
            q2 = [sb("q2b%d" % i, [128, 16, 128], BF16) for i in range(2)]
            bq2 = [B(), B()]
            S = [sb("S%d" % i, [128, 16, 128], F32) for i in range(2)]
            bSS = [B(), B()]
            S2 = sb("S2", [128, 16, 128], F32)
            V16 = sb("V16", [128, 16, 16], F32)
            I16 = sb("I16", [128, 16, 16], U32)
            I16f = sb("I16f", [128, 16, 16], F32)
            cand = sb("cand", [128, 8, 256], F32)
            cand2 = sb("cand2", [128, 8, 256], F32)
            T16 = sb("T16", [128, 8, 16], F32)
            P16 = sb("P16", [128, 8, 16], U32)
            Pa = sb("Pa", [128, 8, 16], U32)
            Pb = sb("Pb", [128, 8, 16], U32)
            Paf = sb("Paf", [128, 8, 16], F32)
            Pbf = sb("Pbf", [128, 8, 16], F32)
            negm = sb("negm", [128, 8], F32)
            Z = sb("Z", [128, 8], F32)
            E = sb("E", [128, 8, 16], F32)
            oh = sb("oh", [128, 8, 16, 16], F32)
            tok = sb("tok", [128, 3, 128], F32)
            tokT = [sb("tokT%d" % i, [128, 3, 128], F32) for i in range(2)]
            btokT = [B(), B()]
            bS = B()
            bH = [B() for _ in range(16)]
            bG2 = [B() for _ in range(8)]
            TB = 16
            NB = 4
            Ab = [sb("Ab%d" % i, [128, TB, 128], BF16) for i in range(NB)]
            Bb = [sb("Bb%d" % i, [128, TB, 128], BF16) for i in range(NB)]
            bAb = [[B() for _ in range(TB)] for _ in range(NB)]
            bBb = [B() for _ in range(NB)]
            Gs = [sb("Gs%d" % i, [128, 128, 128], BF16) for i in range(2)]
            bGs = [B(), B()]
            pG = [ps("pG%d" % i, [128, 1024], F32) for i in range(2)]
            bpG = [B(), B()]
            pSc = ps("pSc", [128, 1024], F32)
            bpSc = B()
            pX = ps("pXb", [128, 512], F32)
            bpX = B()
            iota_b16 = iota_f[:, 0:16]

            def scores(tt):
                s = tt % 2
                dma("sp", q2[s][:], Q2Td[:, :, tt * 128:(tt + 1) * 128].rearrange("c p t -> p c t"), writes=[bq2[s]])
                for rnd in range(2):
                    for h8 in range(8):
                        hp = rnd * 8 + h8
                        op("pe", lambda e: e.matmul(pSc[:, h8 * 128:(h8 + 1) * 128], lhsT=q2[s][:, hp, :], rhs=skb[:, hp, :], start=True, stop=True),
                           [bq2[s], bsk], [bpSc])
                    op("act", lambda e: e.copy(out=S[s][:, rnd * 8:(rnd + 1) * 8, :].rearrange("p a n -> p (a n)"), in_=pSc[:]), [bpSc], [bSS[s]])

            gcount = [0]

            def R1(tt):
                s = tt % 2
                Sc = S[s]
                R = [bS]
                HB = [[bH[hp]] for hp in range(16)]
                for hp in range(16):
                    op("dve", lambda e: e.max(out=V16[:, hp, 0:8], in_=Sc[:, hp, :]), [bSS[s]], HB[hp])
                for hp in range(16):
                    op("dve", lambda e: e.max_index(out=I16[:, hp, 0:8], in_max=V16[:, hp, 0:8], in_values=Sc[:, hp, :]), [bSS[s]] + HB[hp], HB[hp])
                for hp in range(16):
                    op("dve", lambda e: e.match_replace(out=S2[:, hp, :], in_to_replace=V16[:, hp, 0:8], in_values=Sc[:, hp, :], imm_value=-1e30),
                       [bSS[s]] + HB[hp], HB[hp])
                for hp in range(16):
                    op("dve", lambda e: e.max(out=V16[:, hp, 8:16], in_=S2[:, hp, :]), HB[hp], HB[hp])
                for hp in range(16):
                    op("dve", lambda e: e.max_index(out=I16[:, hp, 8:16], in_max=V16[:, hp, 8:16], in_values=S2[:, hp, :]), HB[hp], HB[hp])
                ALLH = [bH[hp] for hp in range(16)]
                op("dve", lambda e: e.tensor_copy(out=I16f[:], in_=I16[:]), ALLH, R)
                Vv = V16[:].rearrange("p (h two) k -> p h two k", two=2)
                cv = cand[:].rearrange("p h (a b) -> p h a b", a=16)
                op("dve", lambda e: e.tensor_tensor(out=cv, in0=Vv[:, :, 0, :].unsqueeze(3).to_broadcast([128, 8, 16, 16]),
                                                    in1=Vv[:, :, 1, :].unsqueeze(2).to_broadcast([128, 8, 16, 16]), op=ALU.add), ALLH + R, R)
                GB = [[bG2[h]] for h in range(8)]
                for h in range(8):
                    op("dve", lambda e: e.max(out=T16[:, h, 0:8], in_=cand[:, h, :]), R, GB[h])
                for h in range(8):
                    op("dve", lambda e: e.max_index(out=P16[:, h, 0:8], in_max=T16[:, h, 0:8], in_values=cand[:, h, :]), R + GB[h], GB[h])
                for h in range(8):
                    op("dve", lambda e: e.match_replace(out=cand2[:, h, :], in_to_replace=T16[:, h, 0:8], in_values=cand[:, h, :], imm_value=-1e30),
                       R + GB[h], GB[h])
                for h in range(8):
                    op("dve", lambda e: e.max(out=T16[:, h, 8:16], in_=cand2[:, h, :]), GB[h], GB[h])
                for h in range(8):
                    op("dve", lambda e: e.max_index(out=P16[:, h, 8:16], in_max=T16[:, h, 8:16], in_values=cand2[:, h, :]), GB[h], GB[h])
                ALLG = [bG2[h] for h in range(8)]
                op("dve", lambda e: e.tensor_scalar(out=negm[:], in0=T16[:, :, 0], scalar1=-1.0, scalar2=None, op0=ALU.mult), R + ALLG, R + ALLG)
                for h in range(8):
                    op("act", lambda e: e.activation(out=E[:, h, :], in_=T16[:, h, :], func=AF.Exp, bias=negm[:, h:h + 1], accum_out=Z[:, h:h + 1]),
                       R + [bG2[h]], R)

            def R2(tt):
                s = tt % 2
                R = [bS]
                Iv = I16f[:].rearrange("p (h two) k -> p h two k", two=2)
                op("dve", lambda e: e.reciprocal(out=Z[:], in_=Z[:]), R, R)
                gv = tok[:, 2, :].rearrange("p (h k) -> p h k", h=8)
                op("dve", lambda e: e.tensor_tensor(out=gv, in0=E[:], in1=Z[:].unsqueeze(2).to_broadcast([128, 8, 16]), op=ALU.mult), R, R)
                ALLG = [bG2[h] for h in range(8)]
                op("dve", lambda e: e.tensor_single_scalar(out=Pa[:], in_=P16[:], scalar=4, op=ALU.logical_shift_right), R + ALLG, R)
                op("dve", lambda e: e.tensor_single_scalar(out=Pb[:], in_=P16[:], scalar=15, op=ALU.bitwise_and), R + ALLG, R)
                op("dve", lambda e: e.tensor_copy(out=Paf[:], in_=Pa[:]), R, R)
                op("dve", lambda e: e.tensor_copy(out=Pbf[:], in_=Pb[:]), R, R)
                io4 = iota_b16.unsqueeze(1).unsqueeze(1).to_broadcast([128, 8, 16, 16])
                for which, Pf in ((0, Paf), (1, Pbf)):
                    op("dve", lambda e: e.tensor_tensor(out=oh[:], in0=Pf[:].unsqueeze(3).to_broadcast([128, 8, 16, 16]), in1=io4, op=ALU.is_equal), R + CONST, R)
                    op("dve", lambda e: e.tensor_tensor(out=oh[:], in0=oh[:], in1=Iv[:, :, which, :].unsqueeze(2).to_broadcast([128, 8, 16, 16]),
                                                        op=ALU.mult), R, R)
                    op("dve", lambda e: e.tensor_reduce(out=tok[:, which, :].rearrange("p (h k) -> p h k", h=8), in_=oh[:], axis=AX.X, op=ALU.add), R, R)
                for j in range(3):
                    op("pe", lambda e: e.transpose(out=pX[:, j * 128:(j + 1) * 128], in_=tok[:, j, :], identity=identf[:]), R + CONST, [bpX])
                op("act", lambda e: e.copy(out=tokT[s][:].rearrange("p a t -> p (a t)"), in_=pX[:, 0:384]), [bpX], [btokT[s]])

            def OH(tt, sb_lo, sb_hi):
                s = tt % 2
                g = tt % 2
                tT = tokT[s]
                RT = [btokT[s]]
                for sbi in range(sb_lo, sb_hi):
                    a = sbi % NB
                    t0 = sbi * TB
                    iob = iota_f[:].unsqueeze(1).to_broadcast([128, TB, 128])
                    op("dve", lambda e: e.tensor_tensor(out=Bb[a][:], in0=iob, in1=tT[:, 1, t0:t0 + TB].unsqueeze(2).to_broadcast([128, TB, 128]),
                                                        op=ALU.is_equal), RT + CONST, [bBb[a]])
                    for tl in range(TB):
                        op("dve", lambda e: e.tensor_scalar(out=Ab[a][:, tl, :], in0=iota_b[:], scalar1=tT[:, 0, t0 + tl:t0 + tl + 1],
                                                            scalar2=tT[:, 2, t0 + tl:t0 + tl + 1], op0=ALU.is_equal, op1=ALU.mult),
                           RT + CONST, [bAb[a][tl]])
                    for q8 in range(TB // 8):
                        pz = gcount[0] % 2
                        gcount[0] += 1
                        for tl in range(8):
                            tloc = q8 * 8 + tl
                            bank = tl // 4
                            oap = pG[pz][:, bank * 512:(bank + 1) * 512].rearrange("j (i t) -> j t i", t=4)[:, tl % 4, :]
                            op("pe", lambda e: e.matmul(oap, lhsT=Bb[a][:, tloc, :], rhs=Ab[a][:, tloc, :], start=True, stop=True),
                               [bBb[a], bAb[a][tloc]], [bpG[pz]])
                        tg0 = t0 + q8 * 8
                        op("act", lambda e: e.copy(out=Gs[g][:, :, tg0:tg0 + 8].rearrange("j i (b t) -> j b i t", b=2),
                                                   in_=pG[pz][:].rearrange("j (b i t) -> j b i t", b=2, t=4)), [bpG[pz]], [bGs[g]])

            def Gst(tt):
                g = tt % 2
                for i8 in range(8):
                    dma("sp" if i8 % 2 else "act", Gd[i8 * 16:(i8 + 1) * 16, :, tt * 128:(tt + 1) * 128].rearrange("i j t -> j i t"),
                        Gs[g][:, i8 * 16:(i8 + 1) * 16, :], reads=[bGs[g]])

            scores(0)
            R1(0)
            R2(0)
            scores(1)
            NSB = 128 // TB
            for tt in range(16):
                if tt + 1 < 16:
                    R1(tt + 1)
                OH(tt, 0, NSB - 2)
                if tt + 1 < 16:
                    R2(tt + 1)
                if tt + 2 < 16:
                    scores(tt + 2)
                OH(tt, NSB - 2, NSB)
                Gst(tt)
            phase_end()
        if stop_after == 6:
            return nc

        with ExitStack() as es:
            def sb(n, s, d):
                return es.enter_context(nc.sbuf_tensor(n, s, d))

            def ps(n, s, d):
                return es.enter_context(nc.psum_tensor(n, s, d))
            TP = 1024
            NTT = TP // 128
            GI = 4
            NCH = 128
            yacc = sb("yacc", [128, NTT, D], F32)
            byacc = [[B(), B()] for _ in range(NTT)]
            h2T = sb("h2T", [128, 16, TP], BF16)
            bh2T = B()
            NU_ = 4
            ub = [sb("ub%d" % i, [128, D], BF16) for i in range(NU_)]
            bub = [B() for _ in range(NU_)]
            UT = [sb("UT%d" % i, [128, 16, 128], BF16) for i in range(2)]
            bUT = [B(), B()]
            Vb = [sb("Vb%d" % i, [128, GI, D], BF16) for i in range(2)]
            bVb = [[B() for _ in range(GI)] for _ in range(2)]
            gst = [sb("gst%d" % i, [128, TP], BF16) for i in range(NU_)]
            bgst = [B() for _ in range(NU_)]
            ga = [sb("ga%d" % i, [128, 512], BF16) for i in range(2)]
            bga = [B(), B()]
            NW = GI + 1
            Wg = sb("Wg", [128, NW, TP], BF16)
            bWg = [B() for _ in range(NW)]
            gate2 = sb("gate2", [128, D], F32)
            fgb = sb("fgb", [128, D], F32)
            bgf = B()
            xb_ = [sb("xbf%d" % i, [128, D], F32) for i in range(2)]
            bxb_ = [B(), B()]
            ssf = [sb("ssf%d" % i, [128, 1], F32) for i in range(2)]
            bssf = [B(), B()]
            dma("sp", gate2[:], MODB[:, 5 * D:6 * D], writes=[bgf])
            dma("sp", fgb[:], fg_d.partition_broadcast(128), writes=[bgf])
            pTu = ps("pTu", [128, 16 * 128], BF16)
            bpTu = B()
            pa = [ps("pa%d" % i, [128, 512], F32) for i in range(4)]
            bpa = [B() for _ in range(4)]
            py = [ps("py%d" % i, [128, 512], F32) for i in range(2)]
            bpy = [B(), B()]
            ycnt = [0]
            def final_tile(tb_, tt):
                s = tt % 2
                xb, bxb = xb_[s], bxb_[s]
                r0 = tb_ + tt * 128
                YB = byacc[tt]
                dma("sp", xb[:], X1d[r0:r0 + 128, :], writes=[bxb])
                op("dve", lambda e: e.tensor_tensor(out=yacc[:, tt, :], in0=yacc[:, tt, :], in1=gate2[:], op=ALU.mult), YB + [bgf], YB)
                op("dve", lambda e: e.tensor_tensor(out=yacc[:, tt, :], in0=yacc[:, tt, :], in1=xb[:], op=ALU.add), YB + [bxb], YB)
                op("act", lambda e: e.activation(out=xb[:], in_=yacc[:, tt, :], func=AF.Square, accum_out=ssf[s][:]), YB, [bxb, bssf[s]])
                op("act", lambda e: e.activation(out=ssf[s][:], in_=ssf[s][:], func=AF.Sqrt, scale=1.0 / D, bias=1e-6), [bssf[s]], [bssf[s]])
                op("dve", lambda e: e.reciprocal(out=ssf[s][:], in_=ssf[s][:]), [bssf[s]], [bssf[s]])
                op("dve", lambda e: e.scalar_tensor_tensor(out=xb[:], in0=yacc[:, tt, :], scalar=ssf[s][:], in1=fgb[:], op0=ALU.mult, op1=ALU.mult),
                   YB + [bssf[s], bgf], [bxb])
                dma("sp", out_d[r0:r0 + 128, :], xb[:], reads=[bxb])

            pending_final = []
            for pas in range(NT // TP):
                tbase = pas * TP
                dma("sp", h2T[:], H2Td[:, :, tbase:tbase + TP].rearrange("c p t -> p c t"), writes=[bh2T])

                def loadU(i):
                    dma("pool", ub[i % NU_][:], peer_u[i * 128:(i + 1) * 128, :], writes=[bub[i % NU_]])
                    dma("sp", gst[i % NU_][:], Gd[i, :, tbase:tbase + TP], writes=[bgst[i % NU_]])

                def loadV(grp):
                    for gi in range(GI):
                        i = grp * GI + gi
                        dma("pool", Vb[grp % 2][:, gi, :], peer_v[i * 128:(i + 1) * 128, :], writes=[bVb[grp % 2][gi]])

                def Tr(i):
                    u2 = i % 2
                    for dc in range(16):
                        op("pe", lambda e: e.transpose(out=pTu[:, dc * 128:(dc + 1) * 128], in_=ub[i % NU_][:, dc * 128:(dc + 1) * 128], identity=identb[:]),
                           [bub[i % NU_]] + CONST, [bpTu])
                    op("act", lambda e: e.copy(out=UT[u2][:].rearrange("p a j -> p (a j)"), in_=pTu[:]), [bpTu], [bUT[u2]])

                def Amm(i):
                    u2 = i % 2
                    ws = i % NW
                    for dc in range(16):
                        for tg in range(2):
                            pz = (i % 2) * 2 + tg
                            op("pe", lambda e: e.matmul(pa[pz][:], lhsT=UT[u2][:, dc, :], rhs=h2T[:, dc, tg * 512:(tg + 1) * 512],
                                                        start=(dc == 0), stop=(dc == 15)), [bUT[u2], bh2T], [bpa[pz]])
                    for tg in range(2):
                        pz = (i % 2) * 2 + tg
                        op("act", lambda e: e.activation(out=ga[tg][:], in_=pa[pz][:], func=AF.Gelu), [bpa[pz]], [bga[tg]])
                        op("dve", lambda e: e.tensor_tensor(out=Wg[:, ws, tg * 512:(tg + 1) * 512], in0=ga[tg][:], in1=gst[i % NU_][:, tg * 512:(tg + 1) * 512],
                                                            op=ALU.mult), [bga[tg], bgst[i % NU_]], [bWg[ws]])

                def Ymm(grp, final_tb=None):
                    vb_ = Vb[grp % 2]
                    bv_ = bVb[grp % 2]
                    for tt in range(NTT):
                        for qd in range(4):
                            z = ycnt[0] % 2
                            ycnt[0] += 1
                            for gi in range(GI):
                                ws = (grp * GI + gi) % NW
                                op("pe", lambda e: e.matmul(py[z][:], lhsT=Wg[:, ws, tt * 128:(tt + 1) * 128],
                                                            rhs=vb_[:, gi, qd * 512:(qd + 1) * 512], start=(gi == 0), stop=(gi == GI - 1)),
                                   [bWg[ws], bv_[gi]], [bpy[z]])
                            ysl = yacc[:, tt, qd * 512:(qd + 1) * 512]
                            if grp == 0:
                                op("dve", lambda e: e.tensor_copy(out=ysl, in_=py[z][:]), [bpy[z]], [byacc[tt][qd // 2]])
                            else:
                                op("dve", lambda e: e.tensor_tensor(out=ysl, in0=py[z][:], in1=ysl, op=ALU.add),
                                   [bpy[z], byacc[tt][qd // 2]], [byacc[tt][qd // 2]])
                        if final_tb is not None:
                            final_tile(final_tb, tt)

                loadU(0)
                loadU(1)
                loadU(2)
                loadV(0)
                loadV(1)
                Tr(0)
                for i in range(NCH):
                    if i + 1 < NCH:
                        Tr(i + 1)
                    if i + 3 < NCH:
                        loadU(i + 3)
                    Amm(i)
                    if pending_final and i < GI:
                        for _ in range(NTT // GI):
                            final_tile(*pending_final.pop(0))
                    if i % GI == 0 and i > 0:
                        g_ = i // GI - 1
                        Ymm(g_)
                        if g_ + 2 < NCH // GI:
                            loadV(g_ + 2)
                last_pass = (pas + 1 == NT // TP)
                Ymm(NCH // GI - 1, final_tb=(tbase if last_pass else None))
                if pas + 1 < NT // TP:
                    pending_final = [(tbase, tt) for tt in range(NTT)]
                else:
                    pass
            phase_end()
    return nc


def make_in_maps(x, c, positions, w_mod, b_mod, norm1_g, w_in, w_pool, pool_scale,
                 w_out, norm2_g, w_query, sub_keys, peer_u, peer_v, final_g):
    f = np.float32
    x = np.asarray(x, f)
    shared = {
        "w_mod": np.ascontiguousarray(np.asarray(w_mod, f)[0]),
        "b_mod": np.ascontiguousarray(np.asarray(b_mod, f)[0][None, :]),
        "norm1_g": np.ascontiguousarray(np.asarray(norm1_g, f)[0][None, :]),
        "w_in": np.ascontiguousarray(np.asarray(w_in, f)[0]),
        "w_pool": np.ascontiguousarray(np.asarray(w_pool, f)[0]),
        "psT": np.ascontiguousarray(np.asarray(pool_scale, f)[0].reshape(8, 128).T),
        "w_out": np.ascontiguousarray(np.asarray(w_out, f)[0]),
        "norm2_g": np.ascontiguousarray(np.asarray(norm2_g, f)[0][None, :]),
        "w_query": np.ascontiguousarray(np.asarray(w_query, f)[0]),
        "skT": np.ascontiguousarray(np.asarray(sub_keys, f)[0].reshape(16, 128, 128).transpose(2, 0, 1)),
        "peer_u": np.ascontiguousarray(np.asarray(peer_u, f)[0]),
        "peer_v": np.ascontiguousarray(np.asarray(peer_v, f)[0]),
        "final_g": np.ascontiguousarray(np.asarray(final_g, f)[None, :]),
        "ident": np.eye(128, dtype=f),
        "iota": np.arange(128, dtype=f)[None, :],
    }
    inv = 500000.0 ** (-np.arange(16, dtype=np.float64) * 2.0 / 32.0)
    shared["inv2"] = (inv / (2 * np.pi)).astype(f)[None, :]
    kq = np.arange(128)
    m_own = np.where(kq[None, :] >= kq[:, None], 0.0, NEG).astype(f)
    m_prev = np.where(kq[None, :] <= kq[:, None], 0.0, NEG).astype(f)
    m_none = np.full((128, 128), NEG, f)
    positions = np.asarray(positions, np.int32)
    c = np.asarray(c, f)
    maps = []
    for core in range(8):
        b, qt = divmod(core, 4)
        t0 = qt * NT
        m = dict(shared)
        m["x_own"] = np.ascontiguousarray(x[b, t0:t0 + NT])
        m["x_halo"] = np.ascontiguousarray(x[b, t0 - NT:t0]) if qt > 0 else np.zeros((NT, D), f)
        m["cT"] = np.ascontiguousarray(c[b].reshape(16, 128).T)
        pl = np.zeros(NTL, np.int32)
        pl[NT:] = positions[b, t0:t0 + NT]
        if qt > 0:
            pl[:NT] = positions[b, t0 - NT:t0]
        m["pos"] = np.ascontiguousarray(pl.reshape(32, 128).T)
        m["masks"] = np.ascontiguousarray(np.stack([m_own, m_prev, m_prev if qt > 0 else m_none], axis=1))
        tg = t0 + np.arange(NT)
        m["invcnt"] = np.stack([1.0 / np.minimum(tg + 1, p) for p in (2, 4, 8, 16)]).astype(f)
        m["flag"] = np.full((128, 1), 1.0 if qt > 0 else 0.0, f)
        maps.append(m)
    return maps


_NC = None


def kernel(**inputs):
    global _NC
    maps = make_in_maps(**inputs)
    nc = build()
    res = run_bass_kernel_spmd(nc, maps, core_ids=list(range(8)))
    out = np.zeros((2, 8192, D), np.float32)
    for core in range(8):
        b, qt = divmod(core, 4)
        out[b, qt * NT:(qt + 1) * NT] = res.results[core]["out"]
    return out
```

```python
import numpy as np
import concourse.bass as bass
import concourse.mybir as mybir
from concourse.bass_utils import run_bass_kernel_spmd
from contextlib import ExitStack

F32 = mybir.dt.float32
BF16 = mybir.dt.bfloat16
I32 = mybir.dt.int32
U32 = mybir.dt.uint32
AF = mybir.ActivationFunctionType
ALU = mybir.AluOpType
AX = mybir.AxisListType

NDS = 48
D = 2048
NT = 2048
NTL = 4096
NEG = -30000.0


class Buf:
    __slots__ = ("w", "r")

    def __init__(self):
        self.w = None
        self.r = []


def _compress(toks):
    best = {}
    for t in toks:
        key = (t[0], t[1])
        if key not in best or best[key][2] < t[2]:
            best[key] = t
    return list(best.values())


class KB:
    def __init__(self, nc):
        self.nc = nc
        self.engs = {"pe": nc.tensor, "dve": nc.vector, "act": nc.scalar,
                     "pool": nc.gpsimd, "sp": nc.sync}
        self.sem = {e: nc.alloc_semaphore(name="s_" + e) for e in self.engs}
        self.cnt = {e: 0 for e in self.engs}
        self.seen = {e: {} for e in self.engs}
        self.dsem = [nc.alloc_semaphore(name="d%d" % i) for i in range(NDS)]
        self.dcnt = [0] * NDS
        self.dnext = 0
        self.ninst = 0

    def _wait(self, eng, tok):
        kind, src, k = tok
        if kind == "e":
            if src == eng and eng == "pe":
                return
            key = src
            sem = self.sem[src]
        else:
            key = ("d", src)
            sem = self.dsem[src]
        if self.seen[eng].get(key, 0) >= k:
            return
        self.engs[eng].wait_ge(sem, k)
        self.seen[eng][key] = k

    def _deps(self, eng, reads, writes):
        deps = []
        for b in reads:
            if b.w is not None:
                deps.append(b.w)
        for b in writes:
            if b.w is not None:
                deps.append(b.w)
            deps.extend(b.r)
        for d in deps:
            self._wait(eng, d)

    def _mark(self, tok, reads, writes):
        for b in reads:
            b.r.append(tok)
            if len(b.r) > 48:
                b.r = _compress(b.r)
        for b in writes:
            b.w = tok
            b.r = []

    def op(self, eng, fn, reads=(), writes=()):
        self._deps(eng, reads, writes)
        ins = fn(self.engs[eng])
        self.cnt[eng] += 1
        ins.then_inc(self.sem[eng], 1)
        tok = ("e", eng, self.cnt[eng])
        self._mark(tok, reads, writes)
        self.ninst += 1
        return tok

    def dma(self, q, out, in_, reads=(), writes=(), **kw):
        self._deps(q, reads, writes)
        j = self.dnext
        self.dnext = (self.dnext + 1) % NDS
        if self.dcnt[j] > 0:
            self._wait(q, ("d", j, self.dcnt[j]))
        ins = self.engs[q].dma_start(out=out, in_=in_, **kw)
        self.dcnt[j] += 16
        ins.then_inc(self.dsem[j], 16)
        tok = ("d", j, self.dcnt[j])
        self._mark(tok, reads, writes)
        self.ninst += 1
        return tok

    def barrier(self, engines=None):
        engines = engines or list(self.engs)
        for e in engines:
            for s in self.engs:
                if self.cnt[s] > 0 and not (s == e and e == "pe"):
                    self._wait(e, ("e", s, self.cnt[s]))
            for j in range(NDS):
                if self.dcnt[j] > 0:
                    self._wait(e, ("d", j, self.dcnt[j]))


def build(debug=False, stop_after=None):
    nc = bass.Bass("TRN2", target_bir_lowering=False)

    def din(name, shape, dt=F32):
        return nc.dram_tensor(name, shape, dt, kind="ExternalInput").ap()

    def dscr(name, shape, dt=F32):
        return nc.dram_tensor(name, shape, dt, kind="Internal").ap()

    x_own = din("x_own", [NT, D])
    x_halo = din("x_halo", [NT, D])
    cT_d = din("cT", [128, 16])
    pos_d = din("pos", [128, 32], I32)
    w_mod = din("w_mod", [D, 6 * D])
    b_mod = din("b_mod", [1, 6 * D])
    g1_d = din("norm1_g", [1, D])
    w_in = din("w_in", [D, 4096])
    w_pool = din("w_pool", [4, 256, 256])
    psT_d = din("psT", [128, 8])
    w_out = din("w_out", [D, D])
    g2_d = din("norm2_g", [1, D])
    w_query = din("w_query", [D, D])
    skT_d = din("skT", [128, 16, 128])
    peer_u = din("peer_u", [16384, D])
    peer_v = din("peer_v", [16384, D])
    fg_d = din("final_g", [1, D])
    ident_d = din("ident", [128, 128])
    masks_d = din("masks", [128, 3, 128])
    inv2_d = din("inv2", [1, 16])
    invcnt_d = din("invcnt", [4, NT])
    flag_d = din("flag", [128, 1])
    iota_d = din("iota", [1, 128])
    out_d = nc.dram_tensor("out", [NT, D], F32, kind="ExternalOutput").ap()

    okind = "ExternalOutput" if debug else "Internal"
    MODB = dscr("MODB", [128, 6 * D])
    QTd = dscr("QTd", [8, 128, NT], BF16)
    KTd = dscr("KTd", [8, 128, NTL], BF16)
    Vd = dscr("Vd", [NTL, 1024], BF16)
    UTd = dscr("UTd", [8, 128, 128 + NT])
    X1d = nc.dram_tensor("X1d", [NT, D], F32, kind=okind).ap()
    H2Td = dscr("H2Td", [16, 128, NT], BF16)
    Q2Td = dscr("Q2Td", [16, 128, NT], BF16)
    Gd = dscr("Gd", [128, 128, NT], BF16)
    MIXd = dscr("MIXd", [16, 128, NT], BF16)

    kb = KB(nc)
    op = kb.op
    dma = kb.dma
    B = Buf

    def phase_end():
        kb.barrier()

    with ExitStack() as es0:
        def sb0(n, s, d):
            return es0.enter_context(nc.sbuf_tensor(n, s, d))

        identf = sb0("identf", [128, 128], F32)
        identb = sb0("identb", [128, 128], BF16)
        onesb = sb0("onesb", [128, 128], BF16)
        iota_f = sb0("iota_f", [128, 128], F32)
        flag = sb0("flag_s", [128, 1], F32)
        b_const = B()
        dma("sp", identf[:], ident_d[:, :], writes=[b_const])
        dma("sp", iota_f[:], iota_d.partition_broadcast(128), writes=[b_const])
        dma("sp", flag[:], flag_d[:, :], writes=[b_const])
        op("dve", lambda e: e.tensor_copy(out=identb[:], in_=identf[:]), [b_const], [b_const])
        op("dve", lambda e: e.memset(onesb[:], 1.0), [], [b_const])
        iota_b = sb0("iota_b", [128, 128], BF16)
        op("dve", lambda e: e.tensor_copy(out=iota_b[:], in_=iota_f[:]), [b_const], [b_const])
        CONST = [b_const]

        with ExitStack() as es:
            def sb(n, s, d):
                return es.enter_context(nc.sbuf_tensor(n, s, d))

            def ps(n, s, d):
                return es.enter_context(nc.psum_tensor(n, s, d))
            cT = sb("cT_s", [128, 16], F32)
            sc = sb("sc_s", [128, 16], F32)
            srep = sb("srep", [128, 16, 128], BF16)
            NWM = 3
            wm = [sb("wm%d" % i, [128, 16, 512], BF16) for i in range(NWM)]
            bwm = [B() for _ in range(NWM)]
            bmb = sb("bmb", [128, 6 * D], F32)
            bbmb = B()
            mo = [sb("mo%d" % i, [128, 512], F32) for i in range(2)]
            bmo = [B(), B()]
            pm = [ps("pm%d" % i, [128, 512], F32) for i in range(2)]
            bpm = [B(), B()]
            b0 = B()
            dma("sp", cT[:], cT_d[:, :], writes=[b0])
            dma("sp", bmb[:], b_mod.partition_broadcast(128), writes=[bbmb])
            op("act", lambda e: e.activation(out=sc[:], in_=cT[:], func=AF.Silu), [b0], [b0])
            op("dve", lambda e: e.tensor_copy(out=srep[:], in_=sc[:].unsqueeze(2).to_broadcast([128, 16, 128])), [b0], [b0])

            def lw(g):
                dma("pool", wm[g % NWM][:], w_mod[:, g * 512:(g + 1) * 512].rearrange("(c p) n -> p c n", p=128), writes=[bwm[g % NWM]])
            lw(0)
            lw(1)
            for g in range(24):
                s = g % 2
                w3 = g % NWM
                if g + 2 < 24:
                    lw(g + 2)
                for c in range(16):
                    op("pe", lambda e: e.matmul(pm[s][:], lhsT=srep[:, c, :], rhs=wm[w3][:, c, :], start=(c == 0), stop=(c == 15)),
                       [b0, bwm[w3]], [bpm[s]])
                op("dve", lambda e: e.tensor_tensor(out=mo[s][:], in0=pm[s][:], in1=bmb[:, g * 512:(g + 1) * 512], op=ALU.add),
                   [bpm[s], bbmb], [bmo[s]])
                dma("sp", MODB[:, g * 512:(g + 1) * 512], mo[s][:], reads=[bmo[s]])
            phase_end()
        if stop_after == 0:
            return nc

        with ExitStack() as esM:

            with ExitStack() as es:
                def sb(n, s, d):
                    return es.enter_context(nc.sbuf_tensor(n, s, d))

                def ps(n, s, d):
                    return es.enter_context(nc.psum_tensor(n, s, d))
                wib = sb("wib", [128, 16, 2048], BF16)
                bwib = B()

                def load_w(pas):
                    if pas == 0:
                        for c4 in range(4):
                            dma("pool", wib[:, c4 * 4:(c4 + 1) * 4, :],
                                w_in[c4 * 512:(c4 + 1) * 512, 1024:3072].rearrange("(c p) n -> p c n", p=128), writes=[bwib])
                    else:
                        for c4 in range(4):
                            dma("pool", wib[:, c4 * 4:(c4 + 1) * 4, 0:1024],
                                w_in[c4 * 512:(c4 + 1) * 512, 0:1024].rearrange("(c p) n -> p c n", p=128), writes=[bwib])
                            dma("pool", wib[:, c4 * 4:(c4 + 1) * 4, 1024:2048],
                                w_in[c4 * 512:(c4 + 1) * 512, 3072:4096].rearrange("(c p) n -> p c n", p=128), writes=[bwib])
                A1 = sb("A1", [128, D], F32)
                B1 = sb("B1", [128, D], F32)
                bA = B()
                g1b = sb("g1b", [128, D], F32)
                dma("sp", A1[:, 0:D], MODB[:, D:2 * D], writes=[bA])
                dma("sp", B1[:, 0:D], MODB[:, 0:D], writes=[bA])
                dma("sp", g1b[:], g1_d.partition_broadcast(128), writes=[bA])
                op("dve", lambda e: e.scalar_tensor_tensor(out=A1[:, 0:D], in0=A1[:, 0:D], scalar=1.0, in1=g1b[:],
                                                           op0=ALU.add, op1=ALU.mult), [bA], [bA])
                posi = sb("posi", [128, 32], I32)
                posf = sb("posf", [128, 32], F32)
                inv2 = sb("inv2s", [128, 16], F32)
                uu = sb("uu", [128, 32, 32], F32)
                ki = sb("ki", [128, 32, 32], I32)
                kf = sb("kf", [128, 32, 32], F32)
                tab = sb("tab", [128, 32, 32], F32)
                tabq = sb("tabq", [128, 32, 32], F32)
                bt = B()
                dma("sp", posi[:], pos_d[:, :], writes=[bt])
                dma("sp", inv2[:], inv2_d.partition_broadcast(128), writes=[bt])
                T = [bt]
                op("dve", lambda e: e.tensor_copy(out=posf[:], in_=posi[:]), T, T)
                op("dve", lambda e: e.tensor_tensor(out=uu[:, :, 0:16], in0=posf[:].unsqueeze(2).to_broadcast([128, 32, 16]),
                                                    in1=inv2[:].unsqueeze(1).to_broadcast([128, 32, 16]), op=ALU.mult), T, T)
                op("dve", lambda e: e.tensor_scalar(out=uu[:, :, 16:32], in0=uu[:, :, 0:16], scalar1=0.25, scalar2=None, op0=ALU.add), T, T)
                op("dve", lambda e: e.tensor_copy(out=ki[:], in_=uu[:]), T, T)
                op("dve", lambda e: e.tensor_copy(out=kf[:], in_=ki[:]), T, T)
                op("dve", lambda e: e.tensor_tensor(out=uu[:], in0=uu[:], in1=kf[:], op=ALU.subtract), T, T)
                op("dve", lambda e: e.tensor_single_scalar(out=kf[:], in_=uu[:], scalar=0.5, op=ALU.is_gt), T, T)
                op("dve", lambda e: e.tensor_tensor(out=uu[:], in0=uu[:], in1=kf[:], op=ALU.subtract), T, T)
                op("dve", lambda e: e.tensor_single_scalar(out=kf[:], in_=uu[:], scalar=-0.5, op=ALU.is_lt), T, T)
                op("dve", lambda e: e.tensor_tensor(out=uu[:], in0=uu[:], in1=kf[:], op=ALU.add), T, T)
                op("act", lambda e: e.activation(out=tab[:], in_=uu[:], func=AF.Sin, scale=2 * np.pi), T, T)
                op("dve", lambda e: e.tensor_scalar(out=tabq[:], in0=tab[:], scalar1=128 ** -0.5, scalar2=None, op0=ALU.mult), T, T)

                xt = [sb("xt%d" % i, [128, D], F32) for i in range(2)]
                bxt = [B(), B()]
                ss = [sb("ss%d" % i, [128, 1], F32) for i in range(2)]
                bss = [B(), B()]
                hf = [g1b, sb("hf1", [128, D], F32)]
                bhf = [bA, B()]
                hb = [sb("hb%d" % i, [128, D], BF16) for i in range(2)]
                bhb = [B(), B()]
                hT = [sb("hT%d" % i, [128, 16, 128], BF16) for i in range(2)]
                bhT = [B(), B()]
                zr = [sb("zr%d" % i, [128, 1024], F32) for i in range(2)]
                bzr = [B(), B()]
                zu = sb("zu", [128, 1024], F32)
                bzu = B()
                vb = [sb("vb%d" % i, [128, 1024], BF16) for i in range(2)]
                bvb = [B(), B()]
                rb = [sb("rb%d" % i, [128, 1024], BF16) for i in range(2)]
                brb = [B(), B()]
                rt = [sb("rt%d" % i, [128, 8, 16], F32) for i in range(4)]
                brt = B()
                rT = [sb("rTa%d" % i, [128, 8, 128], BF16) for i in range(2)]
                uT = [sb("uT%d" % i, [128, 8, 128], F32) for i in range(2)]
                brT, buT = [B(), B()], [B(), B()]
                pT = ps("pT", [128, 16 * 128], BF16)
                bpT = B()
                pz = [ps("pz%d" % i, [128, 512], F32) for i in range(3)]
                bpz = [B(), B(), B()]
                pqk = ps("pqk", [128, 8 * 128], BF16)
                bpqk = B()
                pu = ps("pu", [128, 8 * 128], F32)
                bpu = B()
                zc = [0]

                def Nn(pas, lt, s):
                    own = lt >= 16
                    src = x_own[(lt - 16) * 128:(lt - 15) * 128, :] if own else x_halo[lt * 128:(lt + 1) * 128, :]
                    dma("pool", xt[s][:], src, writes=[bxt[s]])
                    op("act", lambda e: e.activation(out=hf[s][:], in_=xt[s][:], func=AF.Square, accum_out=ss[s][:]),
                       [bxt[s]], [bhf[s], bss[s]])
                    op("act", lambda e: e.activation(out=ss[s][:], in_=ss[s][:], func=AF.Sqrt, scale=1.0 / D, bias=1e-6),
                       [bss[s]], [bss[s]])
                    op("dve", lambda e: e.reciprocal(out=ss[s][:], in_=ss[s][:]), [bss[s]], [bss[s]])
                    op("dve", lambda e: e.scalar_tensor_tensor(out=hf[s][:], in0=xt[s][:], scalar=ss[s][:], in1=A1[:, 0:D],
                                                               op0=ALU.mult, op1=ALU.mult), [bxt[s], bss[s], bA], [bhf[s]])
                    op("dve", lambda e: e.tensor_tensor(out=hb[s][:], in0=hf[s][:], in1=B1[:, 0:D], op=ALU.add), [bhf[s], bA], [bhb[s]])

                def TH(pas, lt, s):
                    for c in range(16):
                        op("pe", lambda e: e.transpose(out=pT[:, c * 128:(c + 1) * 128], in_=hb[s][:, c * 128:(c + 1) * 128], identity=identb[:]),
                           [bhb[s]] + CONST, [bpT])
                    op("act", lambda e: e.copy(out=hT[s][:].rearrange("p c t -> p (c t)"), in_=pT[:]), [bpT], [bhT[s]])

                def MM(pas, lt, s, mid=None):
                    own = lt >= 16
                    if pas == 0:
                        groups = [(0, "r"), (1, "r"), (2, "v"), (3, "v")]
                    else:
                        groups = [(0, "r"), (1, "r"), (2, "u"), (3, "u")] if own else [(2, "u"), (3, "u")]
                    for wc, kind in groups:
                        z = zc[0] % 3
                        zc[0] += 1
                        for c in range(16):
                            op("pe", lambda e: e.matmul(pz[z][:], lhsT=hT[s][:, c, :], rhs=wib[:, c, wc * 512:(wc + 1) * 512],
                                                        start=(c == 0), stop=(c == 15)), [bhT[s], bwib], [bpz[z]])
                        half = (wc % 2) * 512
                        if kind == "r":
                            op("act", lambda e: e.copy(out=zr[s][:, half:half + 512], in_=pz[z][:]), [bpz[z]], [bzr[s]])
                        elif kind == "v":
                            op("act", lambda e: e.copy(out=vb[s][:, half:half + 512], in_=pz[z][:]), [bpz[z]], [bvb[s]])
                        else:
                            op("act", lambda e: e.copy(out=zu[:, half:half + 512], in_=pz[z][:]), [bpz[z]], [bzu])
                        if kind == "r" and wc == 1:
                            rot(pas, lt, s)
                        if mid is not None and wc == 2:
                            mid()
                            mid = None
                    if mid is not None:
                        mid()
                    if pas == 0:
                        dma("sp", Vd[lt * 128:(lt + 1) * 128, :], vb[s][:], reads=[bvb[s]])

                def rot(pas, lt, s):
                    table, scale = (tab, 1.0) if pas == 0 else (tabq, 128 ** -0.5)
                    zv = zr[s][:].rearrange("p (h d) -> p h d", h=8)
                    dv = rb[s][:].rearrange("p (h d) -> p h d", h=8)
                    sn = table[:, lt, 0:16].unsqueeze(1).to_broadcast([128, 8, 16])
                    cs = table[:, lt, 16:32].unsqueeze(1).to_broadcast([128, 8, 16])
                    x1 = zv[:, :, 0:16]
                    x2 = zv[:, :, 16:32]
                    R = [bzr[s], bt, brt]
                    op("dve", lambda e: e.tensor_tensor(out=rt[0][:], in0=x1, in1=cs, op=ALU.mult), R, [brt])
                    op("dve", lambda e: e.tensor_tensor(out=rt[1][:], in0=x2, in1=sn, op=ALU.mult), R, [brt])
                    op("dve", lambda e: e.tensor_tensor(out=rt[2][:], in0=x2, in1=cs, op=ALU.mult), R, [brt])
                    op("dve", lambda e: e.tensor_tensor(out=rt[3][:], in0=x1, in1=sn, op=ALU.mult), R, [brt])
                    op("dve", lambda e: e.tensor_tensor(out=dv[:, :, 0:16], in0=rt[0][:], in1=rt[1][:], op=ALU.subtract), [brt], [brb[s]])
                    op("dve", lambda e: e.tensor_tensor(out=dv[:, :, 16:32], in0=rt[2][:], in1=rt[3][:], op=ALU.add), [brt], [brb[s]])
                    op("act", lambda e: e.activation(out=dv[:, :, 32:128], in_=zv[:, :, 32:128], func=AF.Copy, scale=scale), [bzr[s]], [brb[s]])

                def RO(pas, lt, s):
                    own = lt >= 16
                    if pas == 0 or own:
                        for h in range(8):
                            op("pe", lambda e: e.transpose(out=pqk[:, h * 128:(h + 1) * 128], in_=rb[s][:, h * 128:(h + 1) * 128], identity=identb[:]),
                               [brb[s]] + CONST, [bpqk])
                        op("act", lambda e: e.copy(out=rT[s][:].rearrange("p h t -> p (h t)"), in_=pqk[:]), [bpqk], [brT[s]])
                        if pas == 0:
                            dma("sp", KTd[:, :, lt * 128:(lt + 1) * 128].rearrange("h d t -> d h t"), rT[s][:], reads=[brT[s]])
                        else:
                            dma("sp", QTd[:, :, (lt - 16) * 128:(lt - 15) * 128].rearrange("h d t -> d h t"), rT[s][:], reads=[brT[s]])
                    if pas == 1:
                        for j in range(8):
                            op("pe", lambda e: e.transpose(out=pu[:, j * 128:(j + 1) * 128], in_=zu[:, j * 128:(j + 1) * 128], identity=identf[:]),
                               [bzu] + CONST, [bpu])
                        op("act", lambda e: e.copy(out=uT[s][:].rearrange("p c t -> p (c t)"), in_=pu[:]), [bpu], [buT[s]])
                        dma("sp", UTd[:, :, (lt - 15) * 128:(lt - 14) * 128].rearrange("c p t -> p c t"), uT[s][:], reads=[buT[s]])

                for pas in (0, 1):
                    load_w(pas)
                    tl_ = list(range(32)) if pas == 0 else list(range(15, 32))
                    n_ = len(tl_)
                    Nn(pas, tl_[0], 0)
                    TH(pas, tl_[0], 0)
                    if n_ > 1:
                        Nn(pas, tl_[1], 1)
                    for k in range(n_):
                        s = k % 2
                        mid_ = None
                        if k + 1 < n_:
                            mid_ = (lambda kk=k: TH(pas, tl_[kk + 1], (kk + 1) % 2))
                        MM(pas, tl_[k], s, mid=mid_)
                        if k + 2 < n_:
                            Nn(pas, tl_[k + 2], s)
                        RO(pas, tl_[k], s)
                phase_end()
            if stop_after == 1:
                return nc

            with ExitStack() as es:
                def sb(n, s, d):
                    return es.enter_context(nc.sbuf_tensor(n, s, d))

                def ps(n, s, d):
                    return es.enter_context(nc.psum_tensor(n, s, d))
                maskf = sb("maskf", [128, 3, 128], F32)
                maskb = sb("maskb", [128, 3, 128], BF16)
                bmk = B()
                dma("sp", maskf[:], masks_d[:, :, :], writes=[bmk])
                op("dve", lambda e: e.tensor_copy(out=maskb[:], in_=maskf[:]), [bmk], [bmk])
                QT = [sb("QT%d" % i, [128, NT], BF16) for i in range(2)]
                KT = [sb("KT%d" % i, [128, NTL], BF16) for i in range(2)]
                V1 = [sb("V1_%d" % i, [128, 17, 128], BF16) for i in range(2)]
                V4 = [sb("V4_%d" % i, [128, 4, 5, 128], BF16) for i in range(2)]
                V16 = [sb("V16_%d" % i, [128, 16, 2, 128], BF16) for i in range(2)]
                bld = [B(), B()]
                accs = [sb("acc%d" % i, [128, 2, NT], F32) for i in range(2)]
                baccs = [B(), B()]
                rec = sb("rec", [128, NT], F32)
                brec = B()
                mixh = [sb("mixh%d" % i, [128, NT], BF16) for i in range(2)]
                bmixh = [B(), B()]
                PT = [sb("PT%d" % i, [128, 2, 128], BF16) for i in range(2)]
                bPT = [B(), B()]
                pS = [ps("pS%d" % i, [128, 512], F32) for i in range(2)]
                bpS = [B(), B()]
                pO = [ps("pO%d" % i, [128, 512], F32) for i in range(2)]
                bpO = [B(), B()]
                def loads(h):
                    s = h % 2
                    W = [bld[s]]
                    dma("sp", QT[s][:], QTd[h, :, :], writes=W)
                    dma("sp", KT[s][:], KTd[h, :, :], writes=W)
                    hc = slice(h * 128, (h + 1) * 128)
                    dma("sp", V1[s][:], Vd[1920:4096, hc].rearrange("(b k) d -> k b d", k=128), writes=W)
                    v4src = Vd[1536:4096, hc].rearrange("(b k r) d -> r k b d", k=128, r=4)
                    for r in range(4):
                        dma("sp", V4[s][:, r, :, :], v4src[r], writes=W)
                    v16src = Vd[0:4096, hc].rearrange("(b k r) d -> r k b d", k=128, r=16)
                    for r in range(16):
                        dma("sp", V16[s][:, r, :, :], v16src[r], writes=W)

                def iters(h):
                    s = h % 2
                    out = []
                    for dil in (1, 4, 16):
                        nblk = NT // (128 * dil)
                        qv = QT[s][:].rearrange("d (n q r) -> d r n q", r=dil, q=128)
                        kv = KT[s][:].rearrange("d (b k r) -> d r b k", r=dil, k=128)
                        av = accs[s][:].rearrange("d s (n q r) -> d r n s q", r=dil, q=128)
                        for r in range(dil):
                            for n in range(nblk):
                                bo = nblk + n
                                bp = bo - 1
                                if dil == 1:
                                    vp, vo = V1[s][:, bp - 15, :], V1[s][:, bo - 15, :]
                                elif dil == 4:
                                    vp, vo = V4[s][:, r, bp - 3, :], V4[s][:, r, bo - 3, :]
                                else:
                                    vp, vo = V16[s][:, r, bp, :], V16[s][:, r, bo, :]
                                out.append(dict(s=s, q=qv[:, r, n, :], kp=kv[:, r, bp, :], ko=kv[:, r, bo, :], vp=vp, vo=vo,
                                                mprev=(maskb[:, 2, :] if n == 0 else maskb[:, 1, :]), acc=av[:, r, n, :, :], first=(dil == 1)))
                    return out

                def Sst(it, z):
                    s = it["s"]
                    R = [bld[s], bmk] + CONST
                    op("pe", lambda e: e.matmul(pS[z][:, 0:128], lhsT=it["kp"], rhs=it["q"], start=True, stop=False), R, [bpS[z]])
                    op("pe", lambda e: e.matmul(pS[z][:, 0:128], lhsT=identb[:], rhs=it["mprev"], start=False, stop=True), R, [bpS[z]])
                    op("pe", lambda e: e.matmul(pS[z][:, 128:256], lhsT=it["ko"], rhs=it["q"], start=True, stop=False), R, [bpS[z]])
                    op("pe", lambda e: e.matmul(pS[z][:, 128:256], lhsT=identb[:], rhs=maskb[:, 0, :], start=False, stop=True), R, [bpS[z]])
                    op("act", lambda e: e.activation(out=PT[z][:].rearrange("p a b -> p (a b)"), in_=pS[z][:, 0:256], func=AF.Exp),
                       [bpS[z]], [bPT[z]])

                def PVst(it, z):
                    s = it["s"]
                    R2 = [bld[s], bPT[z]] + CONST
                    op("pe", lambda e: e.matmul(pO[z][:, 0:128], lhsT=it["vp"], rhs=PT[z][:, 0, :], start=True, stop=False), R2, [bpO[z]])
                    op("pe", lambda e: e.matmul(pO[z][:, 0:128], lhsT=it["vo"], rhs=PT[z][:, 1, :], start=False, stop=True), R2, [bpO[z]])
                    op("pe", lambda e: e.matmul(pO[z][:, 128:256], lhsT=onesb[:], rhs=PT[z][:, 0, :], start=True, stop=False), R2, [bpO[z]])
                    op("pe", lambda e: e.matmul(pO[z][:, 128:256], lhsT=onesb[:], rhs=PT[z][:, 1, :], start=False, stop=True), R2, [bpO[z]])
                    po_v = pO[z][:, 0:256].rearrange("p (s q) -> p s q", s=2)
                    bacc = baccs[s]
                    if it["first"]:
                        op("dve", lambda e: e.tensor_copy(out=it["acc"], in_=po_v), [bpO[z]], [bacc])
                    else:
                        op("dve", lambda e: e.tensor_tensor(out=it["acc"], in0=po_v, in1=it["acc"], op=ALU.add), [bpO[z], bacc], [bacc])

                loads(0)
                for h in range(8):
                    s = h % 2
                    if h + 1 < 8:
                        loads(h + 1)
                    its = iters(h)
                    NI = len(its)
                    Sst(its[0], 0)
                    Sst(its[1], 1)
                    for i in range(NI):
                        PVst(its[i], i % 3 if False else i % 2)
                        if i + 2 < NI:
                            Sst(its[i + 2], i % 2)
                    op("act", lambda e: e.activation(out=rec[:], in_=accs[s][:, 1, :], func=AF.Ln), [baccs[s]], [brec])
                    op("act", lambda e: e.activation(out=rec[:], in_=rec[:], func=AF.Exp, scale=-1.0), [brec], [brec])
                    op("pool", lambda e: e.tensor_tensor(out=mixh[s][:], in0=accs[s][:, 0, :], in1=rec[:], op=ALU.mult),
                       [baccs[s], brec], [bmixh[s]])
                    dma("sp", MIXd[h, :, :], mixh[s][:], reads=[bmixh[s]])
                phase_end()
            if stop_after == 2:
                return nc

            with ExitStack() as es:
                def sb(n, s, d):
                    return es.enter_context(nc.sbuf_tensor(n, s, d))

                def ps(n, s, d):
                    return es.enter_context(nc.psum_tensor(n, s, d))
                NU = 128 + NT
                psT = sb("psT_s", [128, 8], F32)
                bps = B()
                dma("sp", psT[:], psT_d[:, :], writes=[bps])
                ut = [sb("ut%d" % i, [128, NU], F32) for i in range(2)]
                but = [B(), B()]
                sa = sb("sa", [128, NU], F32)
                sbb = sb("sbb", [128, NU], F32)
                bsa, bsb = B(), B()
                icb = sb("icb", [128, NT], F32)
                bic = B()
                tmp = sb("ptmp", [128, NT], F32)
                btmp = B()
                rT = [sb("rT%d" % i, [128, NT], BF16) for i in range(2)]
                brT = [B(), B()]
                wpf = sb("wpf", [128, 2, 256], F32)
                wpb = sb("wpb", [128, 2, 256], BF16)
                bwp = B()
                bwpb = B()
                pp = [ps("pp%d" % i, [128, 512], F32) for i in range(2)]
                bpp = [B(), B()]
                mo_c = [sb("mo_c%d" % i, [128, NT], BF16) for i in range(2)]
                bmo_c = [B(), B()]
                pit = 0
                for g in range(4):
                    p = (2, 4, 8, 16)[g]
                    dma("sp", wpf[:], w_pool[g].rearrange("(cc p) e -> p cc e", p=128), writes=[bwp])
                    op("dve", lambda e: e.tensor_copy(out=wpb[:], in_=wpf[:]), [bwp], [bwpb])
                    dma("sp", icb[:], invcnt_d[g:g + 1, :].partition_broadcast(128), writes=[bic])
                    for cc in range(2):
                        ch = 2 * g + cc
                        u = ut[cc]
                        dma("sp", u[:], UTd[ch, :, :], writes=[but[cc]])
                        op("dve", lambda e: e.tensor_scalar(out=u[:, 0:128], in0=u[:, 0:128], scalar1=flag[:], scalar2=None, op0=ALU.mult),
                           [but[cc]] + CONST, [but[cc]])
                        cur, bcur = u, but[cc]
                        nxt = [(sa, bsa), (sbb, bsb)]
                        step = 1
                        k = 0
                        while step < p:
                            lo = 2 * step - 1
                            dst, bdst = nxt[k % 2]
                            k += 1
                            eng = "dve"
                            op(eng, lambda e: e.tensor_tensor(out=dst[:, lo:NU], in0=cur[:, lo:NU], in1=cur[:, lo - step:NU - step], op=ALU.add),
                               [bcur], [bdst])
                            cur, bcur = dst, bdst
                            step *= 2
                        op("dve", lambda e: e.tensor_tensor(out=tmp[:], in0=cur[:, 128:NU], in1=icb[:], op=ALU.mult), [bcur, bic], [btmp])
                        op("pool", lambda e: e.tensor_tensor(out=rT[cc][:], in0=tmp[:], in1=u[:, 128:NU], op=ALU.subtract),
                           [btmp, but[cc]], [brT[cc]])
                    for ec in range(2):
                        for tg in range(4):
                            z = pit % 2
                            pit += 1
                            for cc in range(2):
                                op("pe", lambda e: e.matmul(pp[z][:], lhsT=wpb[:, cc, ec * 128:(ec + 1) * 128], rhs=rT[cc][:, tg * 512:(tg + 1) * 512],
                                                            start=(cc == 0), stop=(cc == 1)), [bwpb, brT[cc]], [bpp[z]])
                            chn = 8 + 2 * g + ec
                            op("act", lambda e: e.activation(out=mo_c[ec][:, tg * 512:(tg + 1) * 512], in_=pp[z][:], func=AF.Copy,
                                                             scale=psT[:, 2 * g + ec:2 * g + ec + 1]), [bpp[z], bps], [bmo_c[ec]])
                        dma("sp", MIXd[8 + 2 * g + ec, :, :], mo_c[ec][:], reads=[bmo_c[ec]])
                phase_end()
            if stop_after == 3:
                return nc

            with ExitStack() as es:
                def sb(n, s, d):
                    return es.enter_context(nc.sbuf_tensor(n, s, d))

                def ps(n, s, d):
                    return es.enter_context(nc.psum_tensor(n, s, d))
                wob = sb("wob", [128, 16, D], BF16)
                bwob = B()
                for c4 in range(4):
                    dma("pool", wob[:, c4 * 4:(c4 + 1) * 4, :], w_out[c4 * 512:(c4 + 1) * 512, :].rearrange("(c p) n -> p c n", p=128), writes=[bwob])
                mixT = sb("mixT", [128, 16, NT], BF16)
                bmix = [B() for _ in range(16)]
                for c in range(16):
                    dma("act", mixT[:, c, :], MIXd[c, :, :], writes=[bmix[c]])
                gate1 = sb("gate1", [128, D], F32)
                bg1 = B()
                dma("sp", gate1[:], MODB[:, 2 * D:3 * D], writes=[bg1])
                xt = [sb("xtd%d" % i, [128, D], F32) for i in range(2)]
                bxt = [B(), B()]
                t1 = [sb("t1d%d" % i, [128, D], F32) for i in range(2)]
                bt1 = [B(), B()]
                po = [ps("po%d" % i, [128, 512], F32) for i in range(4)]
                bpo = [B() for _ in range(4)]
                for tt in range(16):
                    s = tt % 2
                    dma("act", xt[s][:], x_own[tt * 128:(tt + 1) * 128, :], writes=[bxt[s]])
                    for ng in range(4):
                        for c in range(16):
                            op("pe", lambda e: e.matmul(po[ng][:], lhsT=mixT[:, c, tt * 128:(tt + 1) * 128], rhs=wob[:, c, ng * 512:(ng + 1) * 512],
                                                        start=(c == 0), stop=(c == 15)), [bmix[c], bwob], [bpo[ng]])
                        op("dve", lambda e: e.tensor_tensor(out=t1[s][:, ng * 512:(ng + 1) * 512], in0=po[ng][:], in1=gate1[:, ng * 512:(ng + 1) * 512],
                                                            op=ALU.mult), [bpo[ng], bg1], [bt1[s]])
                    op("pool", lambda e: e.tensor_tensor(out=t1[s][:], in0=t1[s][:], in1=xt[s][:], op=ALU.add), [bt1[s], bxt[s]], [bt1[s]])
                    dma("sp", X1d[tt * 128:(tt + 1) * 128, :], t1[s][:], reads=[bt1[s]])
                phase_end()
        if stop_after == 4:
            return nc

        with ExitStack() as es:
            def sb(n, s, d):
                return es.enter_context(nc.sbuf_tensor(n, s, d))

            def ps(n, s, d):
                return es.enter_context(nc.psum_tensor(n, s, d))
            wqb = sb("wqb", [128, 16, D], BF16)
            bwqb = B()
            for c4 in range(4):
                dma("pool", wqb[:, c4 * 4:(c4 + 1) * 4, :], w_query[c4 * 512:(c4 + 1) * 512, :].rearrange("(c p) n -> p c n", p=128), writes=[bwqb])
            A2 = sb("A2", [128, D], F32)
            B2 = sb("B2", [128, D], F32)
            g2b = sb("g2b", [128, D], F32)
            bA = B()
            dma("sp", A2[:], MODB[:, 4 * D:5 * D], writes=[bA])
            dma("sp", B2[:], MODB[:, 3 * D:4 * D], writes=[bA])
            dma("sp", g2b[:], g2_d.partition_broadcast(128), writes=[bA])
            op("dve", lambda e: e.scalar_tensor_tensor(out=A2[:], in0=A2[:], scalar=1.0, in1=g2b[:], op0=ALU.add, op1=ALU.mult), [bA], [bA])
            xt = [sb("xte%d" % i, [128, D], F32) for i in range(2)]
            bxt = [B(), B()]
            ss = [sb("sse%d" % i, [128, 1], F32) for i in range(2)]
            bss = [B(), B()]
            hf = [g2b, sb("hfe1", [128, D], F32)]
            bhf = [bA, B()]
            hb = [sb("hbe%d" % i, [128, D], BF16) for i in range(2)]
            bhb = [B(), B()]
            hT4 = [sb("hT4_%d" % i, [128, 16, 512], BF16) for i in range(2)]
            bhT4 = [B(), B()]
            q2 = [sb("q2e%d" % i, [128, 4, 512], BF16) for i in range(2)]
            bq2 = [B(), B()]
            pT = [ps("pTe%d" % i, [128, 16 * 128], BF16) for i in range(2)]
            bpT = [B(), B()]
            pq = [ps("pqe%d" % i, [128, 512], F32) for i in range(4)]
            bpq = [B() for _ in range(4)]

            def Nn(tt):
                s = tt % 2
                dma("pool", xt[s][:], X1d[tt * 128:(tt + 1) * 128, :], writes=[bxt[s]])
                op("act", lambda e: e.activation(out=hf[s][:], in_=xt[s][:], func=AF.Square, accum_out=ss[s][:]), [bxt[s]], [bhf[s], bss[s]])
                op("act", lambda e: e.activation(out=ss[s][:], in_=ss[s][:], func=AF.Sqrt, scale=1.0 / D, bias=1e-6), [bss[s]], [bss[s]])
                op("dve", lambda e: e.reciprocal(out=ss[s][:], in_=ss[s][:]), [bss[s]], [bss[s]])
                op("dve", lambda e: e.scalar_tensor_tensor(out=hf[s][:], in0=xt[s][:], scalar=ss[s][:], in1=A2[:], op0=ALU.mult, op1=ALU.mult),
                   [bxt[s], bss[s], bA], [bhf[s]])
                op("pool", lambda e: e.tensor_tensor(out=hb[s][:], in0=hf[s][:], in1=B2[:], op=ALU.add), [bhf[s], bA], [bhb[s]])

            def TH(tt):
                s = tt % 2
                g = (tt // 4) % 2
                j = tt % 4
                for c in range(16):
                    op("pe", lambda e: e.transpose(out=pT[s][:, c * 128:(c + 1) * 128], in_=hb[s][:, c * 128:(c + 1) * 128], identity=identb[:]),
                       [bhb[s]] + CONST, [bpT[s]])
                op("act", lambda e: e.copy(out=hT4[g][:, :, j * 128:(j + 1) * 128], in_=pT[s][:].rearrange("p (c t) -> p c t", c=16)), [bpT[s]], [bhT4[g]])

            def QM(grp):
                g = grp % 2
                dma("sp", H2Td[:, :, grp * 512:(grp + 1) * 512].rearrange("c p t -> p c t"), hT4[g][:], reads=[bhT4[g]])
                for hp4 in range(4):
                    z = hp4 % 2
                    for j in range(4):
                        hp = hp4 * 4 + j
                        for c in range(16):
                            op("pe", lambda e: e.matmul(pq[j][:], lhsT=wqb[:, c, hp * 128:(hp + 1) * 128], rhs=hT4[g][:, c, :],
                                                        start=(c == 0), stop=(c == 15)), [bwqb, bhT4[g]], [bpq[j]])
                        op("act", lambda e: e.copy(out=q2[z][:, j, :], in_=pq[j][:]), [bpq[j]], [bq2[z]])
                    dma("sp", Q2Td[hp4 * 4:(hp4 + 1) * 4, :, grp * 512:(grp + 1) * 512].rearrange("c p t -> p c t"), q2[z][:], reads=[bq2[z]])

            Nn(0)
            Nn(1)
            for tt in range(16):
                TH(tt)
                if tt + 2 < 16:
                    Nn(tt + 2)
                if tt % 4 == 3:
                    QM(tt // 4)
            phase_end()
        if stop_after == 5:
            return nc

        with ExitStack() as es:
            def sb(n, s, d):
                return es.enter_context(nc.sbuf_tensor(n, s, d))

            def ps(n, s, d):
                return es.enter_context(nc.psum_tensor(n, s, d))
            skb = sb("skb", [128, 16, 128], BF16)
            bsk = B()
            dma("pool", skb[:], skT_d[:, :, :], writes=[bsk])
            q2 = [sb("q2b%d" % i, [128, 16, 128], BF16) for i in range(2)]
            bq2 = [B(), B()]
            S = [sb("S%d" % i, [128, 16, 128], F32) for i in range(2)]
            bSS = [B(), B()]
            S2 = sb("S2", [128, 16, 128], F32)
            V16 = sb("V16", [128, 16, 16], F32)
            I16 = sb("I16", [128, 16, 16], U32)
            I16f = sb("I16f", [128, 16, 16], F32)
            cand = sb("cand", [128, 8, 256], F32)
            cand2 = sb("cand2", [128, 8, 256], F32)
            T16 = sb("T16", [128, 8, 16], F32)
            P16 = sb("P16", [128, 8, 16], U32)
            Pa = sb("Pa", [128, 8, 16], U32)
            Pb = sb("Pb", [128, 8, 16], U32)
            Paf = sb("Paf", [128, 8, 16], F32)
            Pbf = sb("Pbf", [128, 8, 16], F32)
            negm = sb("negm", [128, 8], F32)
            Z = sb("Z", [128, 8], F32)
            E = sb("E", [128, 8, 16], F32)
            oh = sb("oh", [128, 8, 16, 16], F32)
            tok = sb("tok", [128, 3, 128], F32)
            tokT = [sb("tokT%d" % i, [128, 3, 128], F32) for i in range(2)]
            btokT = [B(), B()]
            bS = B()
            bH = [B() for _ in range(16)]
            bG2 = [B() for _ in range(8)]
            TB = 16
            NB = 4
            Ab = [sb("Ab%d" % i, [128, TB, 128], BF16) for i in range(NB)]
            Bb = [sb("Bb%d" % i, [128, TB, 128], BF16) for i in range(NB)]
            bAb = [[B() for _ in range(TB)] for _ in range(NB)]
            bBb = [B() for _ in range(NB)]
            Gs = [sb("Gs%d" % i, [128, 128, 128], BF16) for i in range(2)]
            bGs = [B(), B()]
            pG = [ps("pG%d" % i, [128, 1024], F32) for i in range(2)]
            bpG = [B(), B()]
            pSc = ps("pSc", [128, 1024], F32)
            bpSc = B()
            pX = ps("pXb", [128, 512], F32)
            bpX = B()
            iota_b16 = iota_f[:, 0:16]

            def scores(tt):
                s = tt % 2
                dma("sp", q2[s][:], Q2Td[:, :, tt * 128:(tt + 1) * 128].rearrange("c p t -> p c t"), writes=[bq2[s]])
                for rnd in range(2):
                    for h8 in range(8):
                        hp = rnd * 8 + h8
                        op("pe", lambda e: e.matmul(pSc[:, h8 * 128:(h8 + 1) * 128], lhsT=q2[s][:, hp, :], rhs=skb[:, hp, :], start=True, stop=True),
                           [bq2[s], bsk], [bpSc])
                    op("act", lambda e: e.copy(out=S[s][:, rnd * 8:(rnd + 1) * 8, :].rearrange("p a n -> p (a n)"), in_=pSc[:]), [bpSc], [bSS[s]])

            gcount = [0]

            def R1(tt):
                s = tt % 2
                Sc = S[s]
                R = [bS]
                HB = [[bH[hp]] for hp in range(16)]
                for hp in range(16):
                    op("dve", lambda e: e.max(out=V16[:, hp, 0:8], in_=Sc[:, hp, :]), [bSS[s]], HB[hp])
                for hp in range(16):
                    op("dve", lambda e: e.max_index(out=I16[:, hp, 0:8], in_max=V16[:, hp, 0:8], in_values=Sc[:, hp, :]), [bSS[s]] + HB[hp], HB[hp])
                for hp in range(16):
                    op("dve", lambda e: e.match_replace(out=S2[:, hp, :], in_to_replace=V16[:, hp, 0:8], in_values=Sc[:, hp, :], imm_value=-1e30),
                       [bSS[s]] + HB[hp], HB[hp])
                for hp in range(16):
                    op("dve", lambda e: e.max(out=V16[:, hp, 8:16], in_=S2[:, hp, :]), HB[hp], HB[hp])
                for hp in range(16):
                    op("dve", lambda e: e.max_index(out=I16[:, hp, 8:16], in_max=V16[:, hp, 8:16], in_values=S2[:, hp, :]), HB[hp], HB[hp])
                ALLH = [bH[hp] for hp in range(16)]
                op("dve", lambda e: e.tensor_copy(out=I16f[:], in_=I16[:]), ALLH, R)
                Vv = V16[:].rearrange("p (h two) k -> p h two k", two=2)
                cv = cand[:].rearrange("p h (a b) -> p h a b", a=16)
                op("dve", lambda e: e.tensor_tensor(out=cv, in0=Vv[:, :, 0, :].unsqueeze(3).to_broadcast([128, 8, 16, 16]),
                                                    in1=Vv[:, :, 1, :].unsqueeze(2).to_broadcast([128, 8, 16, 16]), op=ALU.add), ALLH + R, R)
                GB = [[bG2[h]] for h in range(8)]
                for h in range(8):
                    op("dve", lambda e: e.max(out=T16[:, h, 0:8], in_=cand[:, h, :]), R, GB[h])
                for h in range(8):
                    op("dve", lambda e: e.max_index(out=P16[:, h, 0:8], in_max=T16[:, h, 0:8], in_values=cand[:, h, :]), R + GB[h], GB[h])
                for h in range(8):
                    op("dve", lambda e: e.match_replace(out=cand2[:, h, :], in_to_replace=T16[:, h, 0:8], in_values=cand[:, h, :], imm_value=-1e30),
                       R + GB[h], GB[h])
                for h in range(8):
                    op("dve", lambda e: e.max(out=T16[:, h, 8:16], in_=cand2[:, h, :]), GB[h], GB[h])
                for h in range(8):
                    op("dve", lambda e: e.max_index(out=P16[:, h, 8:16], in_max=T16[:, h, 8:16], in_values=cand2[:, h, :]), GB[h], GB[h])
                ALLG = [bG2[h] for h in range(8)]
                op("dve", lambda e: e.tensor_scalar(out=negm[:], in0=T16[:, :, 0], scalar1=-1.0, scalar2=None, op0=ALU.mult), R + ALLG, R + ALLG)
                for h in range(8):
                    op("act", lambda e: e.activation(out=E[:, h, :], in_=T16[:, h, :], func=AF.Exp, bias=negm[:, h:h + 1], accum_out=Z[:, h:h + 1]),
                       R + [bG2[h]], R)

            def R2(tt):
                s = tt % 2
                R = [bS]
                Iv = I16f[:].rearrange("p (h two) k -> p h two k", two=2)
                op("dve", lambda e: e.reciprocal(out=Z[:], in_=Z[:]), R, R)
                gv = tok[:, 2, :].rearrange("p (h k) -> p h k", h=8)
                op("dve", lambda e: e.tensor_tensor(out=gv, in0=E[:], in1=Z[:].unsqueeze(2).to_broadcast([128, 8, 16]), op=ALU.mult), R, R)
                ALLG = [bG2[h] for h in range(8)]
                op("dve", lambda e: e.tensor_single_scalar(out=Pa[:], in_=P16[:], scalar=4, op=ALU.logical_shift_right), R + ALLG, R)
                op("dve", lambda e: e.tensor_single_scalar(out=Pb[:], in_=P16[:], scalar=15, op=ALU.bitwise_and), R + ALLG, R)
                op("dve", lambda e: e.tensor_copy(out=Paf[:], in_=Pa[:]), R, R)
                op("dve", lambda e: e.tensor_copy(out=Pbf[:], in_=Pb[:]), R, R)
                io4 = iota_b16.unsqueeze(1).unsqueeze(1).to_broadcast([128, 8, 16, 16])
                for which, Pf in ((0, Paf), (1, Pbf)):
                    op("dve", lambda e: e.tensor_tensor(out=oh[:], in0=Pf[:].unsqueeze(3).to_broadcast([128, 8, 16, 16]), in1=io4, op=ALU.is_equal), R + CONST, R)
                    op("dve", lambda e: e.tensor_tensor(out=oh[:], in0=oh[:], in1=Iv[:, :, which, :].unsqueeze(2).to_broadcast([128, 8, 16, 16]),
                                                        op=ALU.mult), R, R)
                    op("dve", lambda e: e.tensor_reduce(out=tok[:, which, :].rearrange("p (h k) -> p h k", h=8), in_=oh[:], axis=AX.X, op=ALU.add), R, R)
                for j in range(3):
                    op("pe", lambda e: e.transpose(out=pX[:, j * 128:(j + 1) * 128], in_=tok[:, j, :], identity=identf[:]), R + CONST, [bpX])
                op("act", lambda e: e.copy(out=tokT[s][:].rearrange("p a t -> p (a t)"), in_=pX[:, 0:384]), [bpX], [btokT[s]])

            def OH(tt, sb_lo, sb_hi):
                s = tt % 2
                g = tt % 2
                tT = tokT[s]
                RT = [btokT[s]]
                for sbi in range(sb_lo, sb_hi):
                    a = sbi % NB
                    t0 = sbi * TB
                    iob = iota_f[:].unsqueeze(1).to_broadcast([128, TB, 128])
                    op("dve", lambda e: e.tensor_tensor(out=Bb[a][:], in0=iob, in1=tT[:, 1, t0:t0 + TB].unsqueeze(2).to_broadcast([128, TB, 128]),
                                                        op=ALU.is_equal), RT + CONST, [bBb[a]])
                    for tl in range(TB):
                        op("dve", lambda e: e.tensor_scalar(out=Ab[a][:, tl, :], in0=iota_b[:], scalar1=tT[:, 0, t0 + tl:t0 + tl + 1],
                                                            scalar2=tT[:, 2, t0 + tl:t0 + tl + 1], op0=ALU.is_equal, op1=ALU.mult),
                           RT + CONST, [bAb[a][tl]])
                    for q8 in range(TB // 8):
                        pz = gcount[0] % 2
                        gcount[0] += 1
                        for tl in range(8):
                            tloc = q8 * 8 + tl
                            bank = tl // 4
                            oap = pG[pz][:, bank * 512:(bank + 1) * 512].rearrange("j (i t) -> j t i", t=4)[:, tl % 4, :]
                            op("pe", lambda e: e.matmul(oap, lhsT=Bb[a][:, tloc, :], rhs=Ab[a][:, tloc, :], start=True, stop=True),
                               [bBb[a], bAb[a][tloc]], [bpG[pz]])
                        tg0 = t0 + q8 * 8
                        op("act", lambda e: e.copy(out=Gs[g][:, :, tg0:tg0 + 8].rearrange("j i (b t) -> j b i t", b=2),
                                                   in_=pG[pz][:].rearrange("j (b i t) -> j b i t", b=2, t=4)), [bpG[pz]], [bGs[g]])

            def Gst(tt):
                g = tt % 2
                for i8 in range(8):
                    dma("sp" if i8 % 2 else "act", Gd[i8 * 16:(i8 + 1) * 16, :, tt * 128:(tt + 1) * 128].rearrange("i j t -> j i t"),
                        Gs[g][:, i8 * 16:(i8 + 1) * 16, :], reads=[bGs[g]])

            scores(0)
            R1(0)
            R2(0)
            scores(1)
            NSB = 128 // TB
            for tt in range(16):
                if tt + 1 < 16:
                    R1(tt + 1)
                OH(tt, 0, NSB - 2)
                if tt + 1 < 16:
                    R2(tt + 1)
                if tt + 2 < 16:
                    scores(tt + 2)
                OH(tt, NSB - 2, NSB)
                Gst(tt)
            phase_end()
        if stop_after == 6:
            return nc

        with ExitStack() as es:
            def sb(n, s, d):
                return es.enter_context(nc.sbuf_tensor(n, s, d))

            def ps(n, s, d):
                return es.enter_context(nc.psum_tensor(n, s, d))
            TP = 1024
            NTT = TP // 128
            GI = 4
            NCH = 128
            yacc = sb("yacc", [128, NTT, D], F32)
            byacc = [[B(), B()] for _ in range(NTT)]
            h2T = sb("h2T", [128, 16, TP], BF16)
            bh2T = B()
            NU_ = 4
            ub = [sb("ub%d" % i, [128, D], BF16) for i in range(NU_)]
            bub = [B() for _ in range(NU_)]
            UT = [sb("UT%d" % i, [128, 16, 128], BF16) for i in range(2)]
            bUT = [B(), B()]
            Vb = [sb("Vb%d" % i, [128, GI, D], BF16) for i in range(2)]
            bVb = [[B() for _ in range(GI)] for _ in range(2)]
            gst = [sb("gst%d" % i, [128, TP], BF16) for i in range(NU_)]
            bgst = [B() for _ in range(NU_)]
            ga = [sb("ga%d" % i, [128, 512], BF16) for i in range(2)]
            bga = [B(), B()]
            NW = GI + 1
            Wg = sb("Wg", [128, NW, TP], BF16)
            bWg = [B() for _ in range(NW)]
            gate2 = sb("gate2", [128, D], F32)
            fgb = sb("fgb", [128, D], F32)
            bgf = B()
            xb_ = [sb("xbf%d" % i, [128, D], F32) for i in range(2)]
            bxb_ = [B(), B()]
            ssf = [sb("ssf%d" % i, [128, 1], F32) for i in range(2)]
            bssf = [B(), B()]
            dma("sp", gate2[:], MODB[:, 5 * D:6 * D], writes=[bgf])
            dma("sp", fgb[:], fg_d.partition_broadcast(128), writes=[bgf])
            pTu = ps("pTu", [128, 16 * 128], BF16)
            bpTu = B()
            pa = [ps("pa%d" % i, [128, 512], F32) for i in range(4)]
            bpa = [B() for _ in range(4)]
            py = [ps("py%d" % i, [128, 512], F32) for i in range(2)]
            bpy = [B(), B()]
            ycnt = [0]
            def final_tile(tb_, tt):
                s = tt % 2
                xb, bxb = xb_[s], bxb_[s]
                r0 = tb_ + tt * 128
                YB = byacc[tt]
                dma("sp", xb[:], X1d[r0:r0 + 128, :], writes=[bxb])
                op("dve", lambda e: e.tensor_tensor(out=yacc[:, tt, :], in0=yacc[:, tt, :], in1=gate2[:], op=ALU.mult), YB + [bgf], YB)
                op("dve", lambda e: e.tensor_tensor(out=yacc[:, tt, :], in0=yacc[:, tt, :], in1=xb[:], op=ALU.add), YB + [bxb], YB)
                op("act", lambda e: e.activation(out=xb[:], in_=yacc[:, tt, :], func=AF.Square, accum_out=ssf[s][:]), YB, [bxb, bssf[s]])
                op("act", lambda e: e.activation(out=ssf[s][:], in_=ssf[s][:], func=AF.Sqrt, scale=1.0 / D, bias=1e-6), [bssf[s]], [bssf[s]])
                op("dve", lambda e: e.reciprocal(out=ssf[s][:], in_=ssf[s][:]), [bssf[s]], [bssf[s]])
                op("dve", lambda e: e.scalar_tensor_tensor(out=xb[:], in0=yacc[:, tt, :], scalar=ssf[s][:], in1=fgb[:], op0=ALU.mult, op1=ALU.mult),
                   YB + [bssf[s], bgf], [bxb])
                dma("sp", out_d[r0:r0 + 128, :], xb[:], reads=[bxb])

            pending_final = []
            for pas in range(NT // TP):
                tbase = pas * TP
                dma("sp", h2T[:], H2Td[:, :, tbase:tbase + TP].rearrange("c p t -> p c t"), writes=[bh2T])

                def loadU(i):
                    dma("pool", ub[i % NU_][:], peer_u[i * 128:(i + 1) * 128, :], writes=[bub[i % NU_]])
                    dma("sp", gst[i % NU_][:], Gd[i, :, tbase:tbase + TP], writes=[bgst[i % NU_]])

                def loadV(grp):
                    for gi in range(GI):
                        i = grp * GI + gi
                        dma("pool", Vb[grp % 2][:, gi, :], peer_v[i * 128:(i + 1) * 128, :], writes=[bVb[grp % 2][gi]])

                def Tr(i):
                    u2 = i % 2
                    for dc in range(16):
                        op("pe", lambda e: e.transpose(out=pTu[:, dc * 128:(dc + 1) * 128], in_=ub[i % NU_][:, dc * 128:(dc + 1) * 128], identity=identb[:]),
                           [bub[i % NU_]] + CONST, [bpTu])
                    op("act", lambda e: e.copy(out=UT[u2][:].rearrange("p a j -> p (a j)"), in_=pTu[:]), [bpTu], [bUT[u2]])

                def Amm(i):
                    u2 = i % 2
                    ws = i % NW
                    for dc in range(16):
                        for tg in range(2):
                            pz = (i % 2) * 2 + tg
                            op("pe", lambda e: e.matmul(pa[pz][:], lhsT=UT[u2][:, dc, :], rhs=h2T[:, dc, tg * 512:(tg + 1) * 512],
                                                        start=(dc == 0), stop=(dc == 15)), [bUT[u2], bh2T], [bpa[pz]])
                    for tg in range(2):
                        pz = (i % 2) * 2 + tg
                        op("act", lambda e: e.activation(out=ga[tg][:], in_=pa[pz][:], func=AF.Gelu), [bpa[pz]], [bga[tg]])
                        op("dve", lambda e: e.tensor_tensor(out=Wg[:, ws, tg * 512:(tg + 1) * 512], in0=ga[tg][:], in1=gst[i % NU_][:, tg * 512:(tg + 1) * 512],
                                                            op=ALU.mult), [bga[tg], bgst[i % NU_]], [bWg[ws]])

                def Ymm(grp, final_tb=None):
                    vb_ = Vb[grp % 2]
                    bv_ = bVb[grp % 2]
                    for tt in range(NTT):
                        for qd in range(4):
                            z = ycnt[0] % 2
                            ycnt[0] += 1
                            for gi in range(GI):
                                ws = (grp * GI + gi) % NW
                                op("pe", lambda e: e.matmul(py[z][:], lhsT=Wg[:, ws, tt * 128:(tt + 1) * 128],
                                                            rhs=vb_[:, gi, qd * 512:(qd + 1) * 512], start=(gi == 0), stop=(gi == GI - 1)),
                                   [bWg[ws], bv_[gi]], [bpy[z]])
                            ysl = yacc[:, tt, qd * 512:(qd + 1) * 512]
                            if grp == 0:
                                op("dve", lambda e: e.tensor_copy(out=ysl, in_=py[z][:]), [bpy[z]], [byacc[tt][qd // 2]])
                            else:
                                op("dve", lambda e: e.tensor_tensor(out=ysl, in0=py[z][:], in1=ysl, op=ALU.add),
                                   [bpy[z], byacc[tt][qd // 2]], [byacc[tt][qd // 2]])
                        if final_tb is not None:
                            final_tile(final_tb, tt)

                loadU(0)
                loadU(1)
                loadU(2)
                loadV(0)
                loadV(1)
                Tr(0)
                for i in range(NCH):
                    if i + 1 < NCH:
                        Tr(i + 1)
                    if i + 3 < NCH:
                        loadU(i + 3)
                    Amm(i)
                    if pending_final and i < GI:
                        for _ in range(NTT // GI):
                            final_tile(*pending_final.pop(0))
                    if i % GI == 0 and i > 0:
                        g_ = i // GI - 1
                        Ymm(g_)
                        if g_ + 2 < NCH // GI:
                            loadV(g_ + 2)
                last_pass = (pas + 1 == NT // TP)
                Ymm(NCH // GI - 1, final_tb=(tbase if last_pass else None))
                if pas + 1 < NT // TP:
                    pending_final = [(tbase, tt) for tt in range(NTT)]
                else:
                    pass
            phase_end()
    return nc


def make_in_maps(x, c, positions, w_mod, b_mod, norm1_g, w_in, w_pool, pool_scale,
                 w_out, norm2_g, w_query, sub_keys, peer_u, peer_v, final_g):
    f = np.float32
    x = np.asarray(x, f)
    shared = {
        "w_mod": np.ascontiguousarray(np.asarray(w_mod, f)[0]),
        "b_mod": np.ascontiguousarray(np.asarray(b_mod, f)[0][None, :]),
        "norm1_g": np.ascontiguousarray(np.asarray(norm1_g, f)[0][None, :]),
        "w_in": np.ascontiguousarray(np.asarray(w_in, f)[0]),
        "w_pool": np.ascontiguousarray(np.asarray(w_pool, f)[0]),
        "psT": np.ascontiguousarray(np.asarray(pool_scale, f)[0].reshape(8, 128).T),
        "w_out": np.ascontiguousarray(np.asarray(w_out, f)[0]),
        "norm2_g": np.ascontiguousarray(np.asarray(norm2_g, f)[0][None, :]),
        "w_query": np.ascontiguousarray(np.asarray(w_query, f)[0]),
        "skT": np.ascontiguousarray(np.asarray(sub_keys, f)[0].reshape(16, 128, 128).transpose(2, 0, 1)),
        "peer_u": np.ascontiguousarray(np.asarray(peer_u, f)[0]),
        "peer_v": np.ascontiguousarray(np.asarray(peer_v, f)[0]),
        "final_g": np.ascontiguousarray(np.asarray(final_g, f)[None, :]),
        "ident": np.eye(128, dtype=f),
        "iota": np.arange(128, dtype=f)[None, :],
    }
    inv = 500000.0 ** (-np.arange(16, dtype=np.float64) * 2.0 / 32.0)
    shared["inv2"] = (inv / (2 * np.pi)).astype(f)[None, :]
    kq = np.arange(128)
    m_own = np.where(kq[None, :] >= kq[:, None], 0.0, NEG).astype(f)
    m_prev = np.where(kq[None, :] <= kq[:, None], 0.0, NEG).astype(f)
    m_none = np.full((128, 128), NEG, f)
    positions = np.asarray(positions, np.int32)
    c = np.asarray(c, f)
    maps = []
    for core in range(8):
        b, qt = divmod(core, 4)
        t0 = qt * NT
        m = dict(shared)
        m["x_own"] = np.ascontiguousarray(x[b, t0:t0 + NT])
        m["x_halo"] = np.ascontiguousarray(x[b, t0 - NT:t0]) if qt > 0 else np.zeros((NT, D), f)
        m["cT"] = np.ascontiguousarray(c[b].reshape(16, 128).T)
        pl = np.zeros(NTL, np.int32)
        pl[NT:] = positions[b, t0:t0 + NT]
        if qt > 0:
            pl[:NT] = positions[b, t0 - NT:t0]
        m["pos"] = np.ascontiguousarray(pl.reshape(32, 128).T)
        m["masks"] = np.ascontiguousarray(np.stack([m_own, m_prev, m_prev if qt > 0 else m_none], axis=1))
        tg = t0 + np.arange(NT)
        m["invcnt"] = np.stack([1.0 / np.minimum(tg + 1, p) for p in (2, 4, 8, 16)]).astype(f)
        m["flag"] = np.full((128, 1), 1.0 if qt > 0 else 0.0, f)
        maps.append(m)
    return maps


_NC = None


def kernel(**inputs):
    global _NC
    maps = make_in_maps(**inputs)
    nc = build()
    res = run_bass_kernel_spmd(nc, maps, core_ids=list(range(8)))
    out = np.zeros((2, 8192, D), np.float32)
    for core in range(8):
        b, qt = divmod(core, 4)
        out[b, qt * NT:(qt + 1) * NT] = res.results[core]["out"]
    return out
```

```python
import numpy as np
import concourse.bass as bass
import concourse.mybir as mybir
from concourse.bass_utils import run_bass_kernel_spmd
from contextlib import ExitStack

F32 = mybir.dt.float32
BF16 = mybir.dt.bfloat16
I32 = mybir.dt.int32
U32 = mybir.dt.uint32
AF = mybir.ActivationFunctionType
ALU = mybir.AluOpType
AX = mybir.AxisListType

NDS = 48
D = 2048
NT = 2048
NTL = 4096
NEG = -30000.0


class Buf:
    __slots__ = ("w", "r")

    def __init__(self):
        self.w = None
        self.r = []


def _compress(toks):
    best = {}
    for t in toks:
        key = (t[0], t[1])
        if key not in best or best[key][2] < t[2]:
            best[key] = t
    return list(best.values())


class KB:
    def __init__(self, nc):
        self.nc = nc
        self.engs = {"pe": nc.tensor, "dve": nc.vector, "act": nc.scalar,
                     "pool": nc.gpsimd, "sp": nc.sync}
        self.sem = {e: nc.alloc_semaphore(name="s_" + e) for e in self.engs}
        self.cnt = {e: 0 for e in self.engs}
        self.seen = {e: {} for e in self.engs}
        self.dsem = [nc.alloc_semaphore(name="d%d" % i) for i in range(NDS)]
        self.dcnt = [0] * NDS
        self.dnext = 0
        self.ninst = 0

    def _wait(self, eng, tok):
        kind, src, k = tok
        if kind == "e":
            if src == eng and eng == "pe":
                return
            key = src
            sem = self.sem[src]
        else:
            key = ("d", src)
            sem = self.dsem[src]
        if self.seen[eng].get(key, 0) >= k:
            return
        self.engs[eng].wait_ge(sem, k)
        self.seen[eng][key] = k

    def _deps(self, eng, reads, writes):
        deps = []
        for b in reads:
            if b.w is not None:
                deps.append(b.w)
        for b in writes:
            if b.w is not None:
                deps.append(b.w)
            deps.extend(b.r)
        for d in deps:
            self._wait(eng, d)

    def _mark(self, tok, reads, writes):
        for b in reads:
            b.r.append(tok)
            if len(b.r) > 48:
                b.r = _compress(b.r)
        for b in writes:
            b.w = tok
            b.r = []

    def op(self, eng, fn, reads=(), writes=()):
        self._deps(eng, reads, writes)
        ins = fn(self.engs[eng])
        self.cnt[eng] += 1
        ins.then_inc(self.sem[eng], 1)
        tok = ("e", eng, self.cnt[eng])
        self._mark(tok, reads, writes)
        self.ninst += 1
        return tok

    def dma(self, q, out, in_, reads=(), writes=(), **kw):
        self._deps(q, reads, writes)
        j = self.dnext
        self.dnext = (self.dnext + 1) % NDS
        if self.dcnt[j] > 0:
            self._wait(q, ("d", j, self.dcnt[j]))
        ins = self.engs[q].dma_start(out=out, in_=in_, **kw)
        self.dcnt[j] += 16
        ins.then_inc(self.dsem[j], 16)
        tok = ("d", j, self.dcnt[j])
        self._mark(tok, reads, writes)
        self.ninst += 1
        return tok

    def barrier(self, engines=None):
        engines = engines or list(self.engs)
        for e in engines:
            for s in self.engs:
                if self.cnt[s] > 0 and not (s == e and e == "pe"):
                    self._wait(e, ("e", s, self.cnt[s]))
            for j in range(NDS):
                if self.dcnt[j] > 0:
                    self._wait(e, ("d", j, self.dcnt[j]))


def build(debug=False, stop_after=None):
    nc = bass.Bass("TRN2", target_bir_lowering=False)

    def din(name, shape, dt=F32):
        return nc.dram_tensor(name, shape, dt, kind="ExternalInput").ap()

    def dscr(name, shape, dt=F32):
        return nc.dram_tensor(name, shape, dt, kind="Internal").ap()

    x_own = din("x_own", [NT, D])
    x_halo = din("x_halo", [NT, D])
    cT_d = din("cT", [128, 16])
    pos_d = din("pos", [128, 32], I32)
    w_mod = din("w_mod", [D, 6 * D])
    b_mod = din("b_mod", [1, 6 * D])
    g1_d = din("norm1_g", [1, D])
    w_in = din("w_in", [D, 4096])
    w_pool = din("w_pool", [4, 256, 256])
    psT_d = din("psT", [128, 8])
    w_out = din("w_out", [D, D])
    g2_d = din("norm2_g", [1, D])
    w_query = din("w_query", [D, D])
    skT_d = din("skT", [128, 16, 128])
    peer_u = din("peer_u", [16384, D])
    peer_v = din("peer_v", [16384, D])
    fg_d = din("final_g", [1, D])
    ident_d = din("ident", [128, 128])
    masks_d = din("masks", [128, 3, 128])
    inv2_d = din("inv2", [1, 16])
    invcnt_d = din("invcnt", [4, NT])
    flag_d = din("flag", [128, 1])
    iota_d = din("iota", [1, 128])
    out_d = nc.dram_tensor("out", [NT, D], F32, kind="ExternalOutput").ap()

    okind = "ExternalOutput" if debug else "Internal"
    MODB = dscr("MODB", [128, 6 * D])
    QTd = dscr("QTd", [8, 128, NT], BF16)
    KTd = dscr("KTd", [8, 128, NTL], BF16)
    Vd = dscr("Vd", [NTL, 1024], BF16)
    UTd = dscr("UTd", [8, 128, 128 + NT])
    X1d = nc.dram_tensor("X1d", [NT, D], F32, kind=okind).ap()
    H2Td = dscr("H2Td", [16, 128, NT], BF16)
    Q2Td = dscr("Q2Td", [16, 128, NT], BF16)
    Gd = dscr("Gd", [128, 128, NT], BF16)
    MIXd = dscr("MIXd", [16, 128, NT], BF16)

    kb = KB(nc)
    op = kb.op
    dma = kb.dma
    B = Buf

    def phase_end():
        kb.barrier()

    with ExitStack() as es0:
        def sb0(n, s, d):
            return es0.enter_context(nc.sbuf_tensor(n, s, d))

        identf = sb0("identf", [128, 128], F32)
        identb = sb0("identb", [128, 128], BF16)
        onesb = sb0("onesb", [128, 128], BF16)
        iota_f = sb0("iota_f", [128, 128], F32)
        flag = sb0("flag_s", [128, 1], F32)
        b_const = B()
        dma("sp", identf[:], ident_d[:, :], writes=[b_const])
        dma("sp", iota_f[:], iota_d.partition_broadcast(128), writes=[b_const])
        dma("sp", flag[:], flag_d[:, :], writes=[b_const])
        op("dve", lambda e: e.tensor_copy(out=identb[:], in_=identf[:]), [b_const], [b_const])
        op("dve", lambda e: e.memset(onesb[:], 1.0), [], [b_const])
        iota_b = sb0("iota_b", [128, 128], BF16)
        op("dve", lambda e: e.tensor_copy(out=iota_b[:], in_=iota_f[:]), [b_const], [b_const])
        CONST = [b_const]

        with ExitStack() as es:
            def sb(n, s, d):
                return es.enter_context(nc.sbuf_tensor(n, s, d))

            def ps(n, s, d):
                return es.enter_context(nc.psum_tensor(n, s, d))
            cT = sb("cT_s", [128, 16], F32)
            sc = sb("sc_s", [128, 16], F32)
            srep = sb("srep", [128, 16, 128], BF16)
            NWM = 3
            wm = [sb("wm%d" % i, [128, 16, 512], BF16) for i in range(NWM)]
            bwm = [B() for _ in range(NWM)]
            bmb = sb("bmb", [128, 6 * D], F32)
            bbmb = B()
            mo = [sb("mo%d" % i, [128, 512], F32) for i in range(2)]
            bmo = [B(), B()]
            pm = [ps("pm%d" % i, [128, 512], F32) for i in range(2)]
            bpm = [B(), B()]
            b0 = B()
            dma("sp", cT[:], cT_d[:, :], writes=[b0])
            dma("sp", bmb[:], b_mod.partition_broadcast(128), writes=[bbmb])
            op("act", lambda e: e.activation(out=sc[:], in_=cT[:], func=AF.Silu), [b0], [b0])
            op("dve", lambda e: e.tensor_copy(out=srep[:], in_=sc[:].unsqueeze(2).to_broadcast([128, 16, 128])), [b0], [b0])

            def lw(g):
                dma("pool", wm[g % NWM][:], w_mod[:, g * 512:(g + 1) * 512].rearrange("(c p) n -> p c n", p=128), writes=[bwm[g % NWM]])
            lw(0)
            lw(1)
            for g in range(24):
                s = g % 2
                w3 = g % NWM
                if g + 2 < 24:
                    lw(g + 2)
                for c in range(16):
                    op("pe", lambda e: e.matmul(pm[s][:], lhsT=srep[:, c, :], rhs=wm[w3][:, c, :], start=(c == 0), stop=(c == 15)),
                       [b0, bwm[w3]], [bpm[s]])
                op("dve", lambda e: e.tensor_tensor(out=mo[s][:], in0=pm[s][:], in1=bmb[:, g * 512:(g + 1) * 512], op=ALU.add),
                   [bpm[s], bbmb], [bmo[s]])
                dma("sp", MODB[:, g * 512:(g + 1) * 512], mo[s][:], reads=[bmo[s]])
            phase_end()
        if stop_after == 0:
            return nc

        with ExitStack() as esM:

            with ExitStack() as es:
                def sb(n, s, d):
                    return es.enter_context(nc.sbuf_tensor(n, s, d))

                def ps(n, s, d):
                    return es.enter_context(nc.psum_tensor(n, s, d))
                wib = sb("wib", [128, 16, 2048], BF16)
                bwib = B()

                def load_w(pas):
                    if pas == 0:
                        for c4 in range(4):
                            dma("pool", wib[:, c4 * 4:(c4 + 1) * 4, :],
                                w_in[c4 * 512:(c4 + 1) * 512, 1024:3072].rearrange("(c p) n -> p c n", p=128), writes=[bwib])
                    else:
                        for c4 in range(4):
                            dma("pool", wib[:, c4 * 4:(c4 + 1) * 4, 0:1024],
                                w_in[c4 * 512:(c4 + 1) * 512, 0:1024].rearrange("(c p) n -> p c n", p=128), writes=[bwib])
                            dma("pool", wib[:, c4 * 4:(c4 + 1) * 4, 1024:2048],
                                w_in[c4 * 512:(c4 + 1) * 512, 3072:4096].rearrange("(c p) n -> p c n", p=128), writes=[bwib])
                A1 = sb("A1", [128, D], F32)
                B1 = sb("B1", [128, D], F32)
                bA = B()
                g1b = sb("g1b", [128, D], F32)
                dma("sp", A1[:, 0:D], MODB[:, D:2 * D], writes=[bA])
                dma("sp", B1[:, 0:D], MODB[:, 0:D], writes=[bA])
                dma("sp", g1b[:], g1_d.partition_broadcast(128), writes=[bA])
                op("dve", lambda e: e.scalar_tensor_tensor(out=A1[:, 0:D], in0=A1[:, 0:D], scalar=1.0, in1=g1b[:],
                                                           op0=ALU.add, op1=ALU.mult), [bA], [bA])
                posi = sb("posi", [128, 32], I32)
                posf = sb("posf", [128, 32], F32)
                inv2 = sb("inv2s", [128, 16], F32)
                uu = sb("uu", [128, 32, 32], F32)
                ki = sb("ki", [128, 32, 32], I32)
                kf = sb("kf", [128, 32, 32], F32)
                tab = sb("tab", [128, 32, 32], F32)
                tabq = sb("tabq", [128, 32, 32], F32)
                bt = B()
                dma("sp", posi[:], pos_d[:, :], writes=[bt])
                dma("sp", inv2[:], inv2_d.partition_broadcast(128), writes=[bt])
                T = [bt]
                op("dve", lambda e: e.tensor_copy(out=posf[:], in_=posi[:]), T, T)
                op("dve", lambda e: e.tensor_tensor(out=uu[:, :, 0:16], in0=posf[:].unsqueeze(2).to_broadcast([128, 32, 16]),
                                                    in1=inv2[:].unsqueeze(1).to_broadcast([128, 32, 16]), op=ALU.mult), T, T)
                op("dve", lambda e: e.tensor_scalar(out=uu[:, :, 16:32], in0=uu[:, :, 0:16], scalar1=0.25, scalar2=None, op0=ALU.add), T, T)
                op("dve", lambda e: e.tensor_copy(out=ki[:], in_=uu[:]), T, T)
                op("dve", lambda e: e.tensor_copy(out=kf[:], in_=ki[:]), T, T)
                op("dve", lambda e: e.tensor_tensor(out=uu[:], in0=uu[:], in1=kf[:], op=ALU.subtract), T, T)
                op("dve", lambda e: e.tensor_single_scalar(out=kf[:], in_=uu[:], scalar=0.5, op=ALU.is_gt), T, T)
                op("dve", lambda e: e.tensor_tensor(out=uu[:], in0=uu[:], in1=kf[:], op=ALU.subtract), T, T)
                op("dve", lambda e: e.tensor_single_scalar(out=kf[:], in_=uu[:], scalar=-0.5, op=ALU.is_lt), T, T)
                op("dve", lambda e: e.tensor_tensor(out=uu[:], in0=uu[:], in1=kf[:], op=ALU.add), T, T)
                op("act", lambda e: e.activation(out=tab[:], in_=uu[:], func=AF.Sin, scale=2 * np.pi), T, T)
                op("dve", lambda e: e.tensor_scalar(out=tabq[:], in0=tab[:], scalar1=128 ** -0.5, scalar2=None, op0=ALU.mult), T, T)

                xt = [sb("xt%d" % i, [128, D], F32) for i in range(2)]
                bxt = [B(), B()]
                ss = [sb("ss%d" % i, [128, 1], F32) for i in range(2)]
                bss = [B(), B()]
                hf = [g1b, sb("hf1", [128, D], F32)]
                bhf = [bA, B()]
                hb = [sb("hb%d" % i, [128, D], BF16) for i in range(2)]
                bhb = [B(), B()]
                hT = [sb("hT%d" % i, [128, 16, 128], BF16) for i in range(2)]
                bhT = [B(), B()]
                zr = [sb("zr%d" % i, [128, 1024], F32) for i in range(2)]
                bzr = [B(), B()]
                zu = sb("zu", [128, 1024], F32)
                bzu = B()
                vb = [sb("vb%d" % i, [128, 1024], BF16) for i in range(2)]
                bvb = [B(), B()]
                rb = [sb("rb%d" % i, [128, 1024], BF16) for i in range(2)]
                brb = [B(), B()]
                rt = [sb("rt%d" % i, [128, 8, 16], F32) for i in range(4)]
                brt = B()
                rT = [sb("rTa%d" % i, [128, 8, 128], BF16) for i in range(2)]
                uT = [sb("uT%d" % i, [128, 8, 128], F32) for i in range(2)]
                brT, buT = [B(), B()], [B(), B()]
                pT = ps("pT", [128, 16 * 128], BF16)
                bpT = B()
                pz = [ps("pz%d" % i, [128, 512], F32) for i in range(3)]
                bpz = [B(), B(), B()]
                pqk = ps("pqk", [128, 8 * 128], BF16)
                bpqk = B()
                pu = ps("pu", [128, 8 * 128], F32)
                bpu = B()
                zc = [0]

                def Nn(pas, lt, s):
                    own = lt >= 16
                    src = x_own[(lt - 16) * 128:(lt - 15) * 128, :] if own else x_halo[lt * 128:(lt + 1) * 128, :]
                    dma("pool", xt[s][:], src, writes=[bxt[s]])
                    op("act", lambda e: e.activation(out=hf[s][:], in_=xt[s][:], func=AF.Square, accum_out=ss[s][:]),
                       [bxt[s]], [bhf[s], bss[s]])
                    op("act", lambda e: e.activation(out=ss[s][:], in_=ss[s][:], func=AF.Sqrt, scale=1.0 / D, bias=1e-6),
                       [bss[s]], [bss[s]])
                    op("dve", lambda e: e.reciprocal(out=ss[s][:], in_=ss[s][:]), [bss[s]], [bss[s]])
                    op("dve", lambda e: e.scalar_tensor_tensor(out=hf[s][:], in0=xt[s][:], scalar=ss[s][:], in1=A1[:, 0:D],
                                                               op0=ALU.mult, op1=ALU.mult), [bxt[s], bss[s], bA], [bhf[s]])
                    op("dve", lambda e: e.tensor_tensor(out=hb[s][:], in0=hf[s][:], in1=B1[:, 0:D], op=ALU.add), [bhf[s], bA], [bhb[s]])

                def TH(pas, lt, s):
                    for c in range(16):
                        op("pe", lambda e: e.transpose(out=pT[:, c * 128:(c + 1) * 128], in_=hb[s][:, c * 128:(c + 1) * 128], identity=identb[:]),
                           [bhb[s]] + CONST, [bpT])
                    op("act", lambda e: e.copy(out=hT[s][:].rearrange("p c t -> p (c t)"), in_=pT[:]), [bpT], [bhT[s]])

                def MM(pas, lt, s, mid=None):
                    own = lt >= 16
                    if pas == 0:
                        groups = [(0, "r"), (1, "r"), (2, "v"), (3, "v")]
                    else:
                        groups = [(0, "r"), (1, "r"), (2, "u"), (3, "u")] if own else [(2, "u"), (3, "u")]
                    for wc, kind in groups:
                        z = zc[0] % 3
                        zc[0] += 1
                        for c in range(16):
                            op("pe", lambda e: e.matmul(pz[z][:], lhsT=hT[s][:, c, :], rhs=wib[:, c, wc * 512:(wc + 1) * 512],
                                                        start=(c == 0), stop=(c == 15)), [bhT[s], bwib], [bpz[z]])
                        half = (wc % 2) * 512
                        if kind == "r":
                            op("act", lambda e: e.copy(out=zr[s][:, half:half + 512], in_=pz[z][:]), [bpz[z]], [bzr[s]])
                        elif kind == "v":
                            op("act", lambda e: e.copy(out=vb[s][:, half:half + 512], in_=pz[z][:]), [bpz[z]], [bvb[s]])
                        else:
                            op("act", lambda e: e.copy(out=zu[:, half:half + 512], in_=pz[z][:]), [bpz[z]], [bzu])
                        if kind == "r" and wc == 1:
                            rot(pas, lt, s)
                        if mid is not None and wc == 2:
                            mid()
                            mid = None
                    if mid is not None:
                        mid()
                    if pas == 0:
                        dma("sp", Vd[lt * 128:(lt + 1) * 128, :], vb[s][:], reads=[bvb[s]])

                def rot(pas, lt, s):
                    table, scale = (tab, 1.0) if pas == 0 else (tabq, 128 ** -0.5)
                    zv = zr[s][:].rearrange("p (h d) -> p h d", h=8)
                    dv = rb[s][:].rearrange("p (h d) -> p h d", h=8)
                    sn = table[:, lt, 0:16].unsqueeze(1).to_broadcast([128, 8, 16])
                    cs = table[:, lt, 16:32].unsqueeze(1).to_broadcast([128, 8, 16])
                    x1 = zv[:, :, 0:16]
                    x2 = zv[:, :, 16:32]
                    R = [bzr[s], bt, brt]
                    op("dve", lambda e: e.tensor_tensor(out=rt[0][:], in0=x1, in1=cs, op=ALU.mult), R, [brt])
                    op("dve", lambda e: e.tensor_tensor(out=rt[1][:], in0=x2, in1=sn, op=ALU.mult), R, [brt])
                    op("dve", lambda e: e.tensor_tensor(out=rt[2][:], in0=x2, in1=cs, op=ALU.mult), R, [brt])
                    op("dve", lambda e: e.tensor_tensor(out=rt[3][:], in0=x1, in1=sn, op=ALU.mult), R, [brt])
                    op("dve", lambda e: e.tensor_tensor(out=dv[:, :, 0:16], in0=rt[0][:], in1=rt[1][:], op=ALU.subtract), [brt], [brb[s]])
                    op("dve", lambda e: e.tensor_tensor(out=dv[:, :, 16:32], in0=rt[2][:], in1=rt[3][:], op=ALU.add), [brt], [brb[s]])
                    op("act", lambda e: e.activation(out=dv[:, :, 32:128], in_=zv[:, :, 32:128], func=AF.Copy, scale=scale), [bzr[s]], [brb[s]])

                def RO(pas, lt, s):
                    own = lt >= 16
                    if pas == 0 or own:
                        for h in range(8):
                            op("pe", lambda e: e.transpose(out=pqk[:, h * 128:(h + 1) * 128], in_=rb[s][:, h * 128:(h + 1) * 128], identity=identb[:]),
                               [brb[s]] + CONST, [bpqk])
                        op("act", lambda e: e.copy(out=rT[s][:].rearrange("p h t -> p (h t)"), in_=pqk[:]), [bpqk], [brT[s]])
                        if pas == 0:
                            dma("sp", KTd[:, :, lt * 128:(lt + 1) * 128].rearrange("h d t -> d h t"), rT[s][:], reads=[brT[s]])
                        else:
                            dma("sp", QTd[:, :, (lt - 16) * 128:(lt - 15) * 128].rearrange("h d t -> d h t"), rT[s][:], reads=[brT[s]])
                    if pas == 1:
                        for j in range(8):
                            op("pe", lambda e: e.transpose(out=pu[:, j * 128:(j + 1) * 128], in_=zu[:, j * 128:(j + 1) * 128], identity=identf[:]),
                               [bzu] + CONST, [bpu])
                        op("act", lambda e: e.copy(out=uT[s][:].rearrange("p c t -> p (c t)"), in_=pu[:]), [bpu], [buT[s]])
                        dma("sp", UTd[:, :, (lt - 15) * 128:(lt - 14) * 128].rearrange("c p t -> p c t"), uT[s][:], reads=[buT[s]])

                for pas in (0, 1):
                    load_w(pas)
                    tl_ = list(range(32)) if pas == 0 else list(range(15, 32))
                    n_ = len(tl_)
                    Nn(pas, tl_[0], 0)
                    TH(pas, tl_[0], 0)
                    if n_ > 1:
                        Nn(pas, tl_[1], 1)
                    for k in range(n_):
                        s = k % 2
                        mid_ = None
                        if k + 1 < n_:
                            mid_ = (lambda kk=k: TH(pas, tl_[kk + 1], (kk + 1) % 2))
                        MM(pas, tl_[k], s, mid=mid_)
                        if k + 2 < n_:
                            Nn(pas, tl_[k + 2], s)
                        RO(pas, tl_[k], s)
                phase_end()
            if stop_after == 1:
                return nc

            with ExitStack() as es:
                def sb(n, s, d):
                    return es.enter_context(nc.sbuf_tensor(n, s, d))

                def ps(n, s, d):
                    return es.enter_context(nc.psum_tensor(n, s, d))
                maskf = sb("maskf", [128, 3, 128], F32)
                maskb = sb("maskb", [128, 3, 128], BF16)
                bmk = B()
                dma("sp", maskf[:], masks_d[:, :, :], writes=[bmk])
                op("dve", lambda e: e.tensor_copy(out=maskb[:], in_=maskf[:]), [bmk], [bmk])
                QT = [sb("QT%d" % i, [128, NT], BF16) for i in range(2)]
                KT = [sb("KT%d" % i, [128, NTL], BF16) for i in range(2)]
                V1 = [sb("V1_%d" % i, [128, 17, 128], BF16) for i in range(2)]
                V4 = [sb("V4_%d" % i, [128, 4, 5, 128], BF16) for i in range(2)]
                V16 = [sb("V16_%d" % i, [128, 16, 2, 128], BF16) for i in range(2)]
                bld = [B(), B()]
                accs = [sb("acc%d" % i, [128, 2, NT], F32) for i in range(2)]
                baccs = [B(), B()]
                rec = sb("rec", [128, NT], F32)
                brec = B()
                mixh = [sb("mixh%d" % i, [128, NT], BF16) for i in range(2)]
                bmixh = [B(), B()]
                PT = [sb("PT%d" % i, [128, 2, 128], BF16) for i in range(2)]
                bPT = [B(), B()]
                pS = [ps("pS%d" % i, [128, 512], F32) for i in range(2)]
                bpS = [B(), B()]
                pO = [ps("pO%d" % i, [128, 512], F32) for i in range(2)]
                bpO = [B(), B()]
                def loads(h):
                    s = h % 2
                    W = [bld[s]]
                    dma("sp", QT[s][:], QTd[h, :, :], writes=W)
                    dma("sp", KT[s][:], KTd[h, :, :], writes=W)
                    hc = slice(h * 128, (h + 1) * 128)
                    dma("sp", V1[s][:], Vd[1920:4096, hc].rearrange("(b k) d -> k b d", k=128), writes=W)
                    v4src = Vd[1536:4096, hc].rearrange("(b k r) d -> r k b d", k=128, r=4)
                    for r in range(4):
                        dma("sp", V4[s][:, r, :, :], v4src[r], writes=W)
                    v16src = Vd[0:4096, hc].rearrange("(b k r) d -> r k b d", k=128, r=16)
                    for r in range(16):
                        dma("sp", V16[s][:, r, :, :], v16src[r], writes=W)

                def iters(h):
                    s = h % 2
                    out = []
                    for dil in (1, 4, 16):
                        nblk = NT // (128 * dil)
                        qv = QT[s][:].rearrange("d (n q r) -> d r n q", r=dil, q=128)
                        kv = KT[s][:].rearrange("d (b k r) -> d r b k", r=dil, k=128)
                        av = accs[s][:].rearrange("d s (n q r) -> d r n s q", r=dil, q=128)
                        for r in range(dil):
                            for n in range(nblk):
                                bo = nblk + n
                                bp = bo - 1
                                if dil == 1:
                                    vp, vo = V1[s][:, bp - 15, :], V1[s][:, bo - 15, :]
                                elif dil == 4:
                                    vp, vo = V4[s][:, r, bp - 3, :], V4[s][:, r, bo - 3, :]
                                else:
                                    vp, vo = V16[s][:, r, bp, :], V16[s][:, r, bo, :]
                                out.append(dict(s=s, q=qv[:, r, n, :], kp=kv[:, r, bp, :], ko=kv[:, r, bo, :], vp=vp, vo=vo,
                                                mprev=(maskb[:, 2, :] if n == 0 else maskb[:, 1, :]), acc=av[:, r, n, :, :], first=(dil == 1)))
                    return out

                def Sst(it, z):
                    s = it["s"]
                    R = [bld[s], bmk] + CONST
                    op("pe", lambda e: e.matmul(pS[z][:, 0:128], lhsT=it["kp"], rhs=it["q"], start=True, stop=False), R, [bpS[z]])
                    op("pe", lambda e: e.matmul(pS[z][:, 0:128], lhsT=identb[:], rhs=it["mprev"], start=False, stop=True), R, [bpS[z]])
                    op("pe", lambda e: e.matmul(pS[z][:, 128:256], lhsT=it["ko"], rhs=it["q"], start=True, stop=False), R, [bpS[z]])
                    op("pe", lambda e: e.matmul(pS[z][:, 128:256], lhsT=identb[:], rhs=maskb[:, 0, :], start=False, stop=True), R, [bpS[z]])
                    op("act", lambda e: e.activation(out=PT[z][:].rearrange("p a b -> p (a b)"), in_=pS[z][:, 0:256], func=AF.Exp),
                       [bpS[z]], [bPT[z]])

                def PVst(it, z):
                    s = it["s"]
                    R2 = [bld[s], bPT[z]] + CONST
                    op("pe", lambda e: e.matmul(pO[z][:, 0:128], lhsT=it["vp"], rhs=PT[z][:, 0, :], start=True, stop=False), R2, [bpO[z]])
                    op("pe", lambda e: e.matmul(pO[z][:, 0:128], lhsT=it["vo"], rhs=PT[z][:, 1, :], start=False, stop=True), R2, [bpO[z]])
                    op("pe", lambda e: e.matmul(pO[z][:, 128:256], lhsT=onesb[:], rhs=PT[z][:, 0, :], start=True, stop=False), R2, [bpO[z]])
                    op("pe", lambda e: e.matmul(pO[z][:, 128:256], lhsT=onesb[:], rhs=PT[z][:, 1, :], start=False, stop=True), R2, [bpO[z]])
                    po_v = pO[z][:, 0:256].rearrange("p (s q) -> p s q", s=2)
                    bacc = baccs[s]
                    if it["first"]:
                        op("dve", lambda e: e.tensor_copy(out=it["acc"], in_=po_v), [bpO[z]], [bacc])
                    else:
                        op("dve", lambda e: e.tensor_tensor(out=it["acc"], in0=po_v, in1=it["acc"], op=ALU.add), [bpO[z], bacc], [bacc])

                loads(0)
                for h in range(8):
                    s = h % 2
                    if h + 1 < 8:
                        loads(h + 1)
                    its = iters(h)
                    NI = len(its)
                    Sst(its[0], 0)
                    Sst(its[1], 1)
                    for i in range(NI):
                        PVst(its[i], i % 3 if False else i % 2)
                        if i + 2 < NI:
                            Sst(its[i + 2], i % 2)
                    op("act", lambda e: e.activation(out=rec[:], in_=accs[s][:, 1, :], func=AF.Ln), [baccs[s]], [brec])
                    op("act", lambda e: e.activation(out=rec[:], in_=rec[:], func=AF.Exp, scale=-1.0), [brec], [brec])
                    op("pool", lambda e: e.tensor_tensor(out=mixh[s][:], in0=accs[s][:, 0, :], in1=rec[:], op=ALU.mult),
                       [baccs[s], brec], [bmixh[s]])
                    dma("sp", MIXd[h, :, :], mixh[s][:], reads=[bmixh[s]])
                phase_end()
            if stop_after == 2:
                return nc

            with ExitStack() as esCD:
                wob = esCD.enter_context(nc.sbuf_tensor("wob", [128, 16, D], BF16))
                bwob = B()
                for c4 in range(4):
                    dma("pool", wob[:, c4 * 4:(c4 + 1) * 4, :], w_out[c4 * 512:(c4 + 1) * 512, :].rearrange("(c p) n -> p c n", p=128), writes=[bwob])
                mixT = esCD.enter_context(nc.sbuf_tensor("mixT", [128, 16, NT], BF16))
                bmix = [B() for _ in range(16)]
                bMIXd = [B() for _ in range(16)]
                for c in range(8):
                    dma("act", mixT[:, c, :], MIXd[c, :, :], writes=[bmix[c]])

                with ExitStack() as es:
                    def sb(n, s, d):
                        return es.enter_context(nc.sbuf_tensor(n, s, d))

                    def ps(n, s, d):
                        return es.enter_context(nc.psum_tensor(n, s, d))
                    NU = 128 + NT
                    psT = sb("psT_s", [128, 8], F32)
                    bps = B()
                    dma("sp", psT[:], psT_d[:, :], writes=[bps])
                    ut = [sb("ut%d" % i, [128, NU], F32) for i in range(2)]
                    but = [B(), B()]
                    sa = sb("sa", [128, NU], F32)
                    sbb = sb("sbb", [128, NU], F32)
                    bsa, bsb = B(), B()
                    icb = sb("icb", [128, NT], F32)
                    bic = B()
                    tmp = sb("ptmp", [128, NT], F32)
                    btmp = B()
                    rT = [sb("rT%d" % i, [128, NT], BF16) for i in range(2)]
                    brT = [B(), B()]
                    wpb = [sb("wpb%d" % i, [128, 2, 256], BF16) for i in range(2)]
                    bwpb = [B(), B()]
                    pp = [ps("pp%d" % i, [128, 512], F32) for i in range(2)]
                    bpp = [B(), B()]
                    mo_c = [sb("mo_c%d" % i, [128, NT], BF16) for i in range(2)]
                    bmo_c = [B(), B()]

                    def cloads(g):
                        dma("pool", wpb[g % 2][:], w_pool[g].rearrange("(cc p) e -> p cc e", p=128), writes=[bwpb[g % 2]])
                        dma("sp", icb[:], invcnt_d[g:g + 1, :].partition_broadcast(128), writes=[bic])
                        for cc in range(2):
                            dma("sp", ut[cc][:], UTd[2 * g + cc, :, :], writes=[but[cc]])
                    pit = 0
                    cloads(0)
                    for g in range(4):
                        p = (2, 4, 8, 16)[g]
                        for cc in range(2):
                            u = ut[cc]
                            op("dve", lambda e: e.tensor_scalar(out=u[:, 0:128], in0=u[:, 0:128], scalar1=flag[:], scalar2=None, op0=ALU.mult),
                               [but[cc]] + CONST, [but[cc]])
                            cur, bcur = u, but[cc]
                            nxt = [(sa, bsa), (sbb, bsb)]
                            step = 1
                            k = 0
                            while step < p:
                                lo = 2 * step - 1
                                dst, bdst = nxt[k % 2]
                                k += 1
                                op("dve", lambda e: e.tensor_tensor(out=dst[:, lo:NU], in0=cur[:, lo:NU], in1=cur[:, lo - step:NU - step], op=ALU.add),
                                   [bcur], [bdst])
                                cur, bcur = dst, bdst
                                step *= 2
                            op("dve", lambda e: e.tensor_tensor(out=tmp[:], in0=cur[:, 128:NU], in1=icb[:], op=ALU.mult), [bcur, bic], [btmp])
                            op("pool", lambda e: e.tensor_tensor(out=rT[cc][:], in0=tmp[:], in1=u[:, 128:NU], op=ALU.subtract),
                               [btmp, but[cc]], [brT[cc]])
                        if g + 1 < 4:
                            cloads(g + 1)
                        for ec in range(2):
                            for tg in range(4):
                                z = pit % 2
                                pit += 1
                                for cc in range(2):
                                    op("pe", lambda e: e.matmul(pp[z][:], lhsT=wpb[g % 2][:, cc, ec * 128:(ec + 1) * 128], rhs=rT[cc][:, tg * 512:(tg + 1) * 512],
                                                                start=(cc == 0), stop=(cc == 1)), [bwpb[g % 2], brT[cc]], [bpp[z]])
                                op("act", lambda e: e.activation(out=mo_c[ec][:, tg * 512:(tg + 1) * 512], in_=pp[z][:], func=AF.Copy,
                                                                 scale=psT[:, 2 * g + ec:2 * g + ec + 1]), [bpp[z], bps], [bmo_c[ec]])
                            chn = 8 + 2 * g + ec
                            dma("sp", MIXd[chn, :, :], mo_c[ec][:], reads=[bmo_c[ec]], writes=[bMIXd[chn]])
                            dma("sp", mixT[:, chn, :], MIXd[chn, :, :], reads=[bMIXd[chn]], writes=[bmix[chn]])
                    phase_end()
                if stop_after == 3:
                    return nc

                with ExitStack() as es:
                    def sb(n, s, d):
                        return es.enter_context(nc.sbuf_tensor(n, s, d))

                    def ps(n, s, d):
                        return es.enter_context(nc.psum_tensor(n, s, d))
                    gate1 = sb("gate1", [128, D], F32)
                    bg1 = B()
                    dma("sp", gate1[:], MODB[:, 2 * D:3 * D], writes=[bg1])
                    xt = [sb("xtd%d" % i, [128, D], F32) for i in range(2)]
                    bxt = [B(), B()]
                    t1 = [sb("t1d%d" % i, [128, D], F32) for i in range(2)]
                    bt1 = [B(), B()]
                    po = [ps("po%d" % i, [128, 512], F32) for i in range(4)]
                    bpo = [B() for _ in range(4)]
                    for tt in range(16):
                        s = tt % 2
                        dma("act", xt[s][:], x_own[tt * 128:(tt + 1) * 128, :], writes=[bxt[s]])
                        for ng in range(4):
                            for c in range(16):
                                op("pe", lambda e: e.matmul(po[ng][:], lhsT=mixT[:, c, tt * 128:(tt + 1) * 128], rhs=wob[:, c, ng * 512:(ng + 1) * 512],
                                                            start=(c == 0), stop=(c == 15)), [bmix[c], bwob], [bpo[ng]])
                            op("dve", lambda e: e.tensor_tensor(out=t1[s][:, ng * 512:(ng + 1) * 512], in0=po[ng][:], in1=gate1[:, ng * 512:(ng + 1) * 512],
                                                                op=ALU.mult), [bpo[ng], bg1], [bt1[s]])
                        op("pool", lambda e: e.tensor_tensor(out=t1[s][:], in0=t1[s][:], in1=xt[s][:], op=ALU.add), [bt1[s], bxt[s]], [bt1[s]])
                        dma("sp", X1d[tt * 128:(tt + 1) * 128, :], t1[s][:], reads=[bt1[s]])
                    phase_end()
        if stop_after == 4:
            return nc

        with ExitStack() as es:
            def sb(n, s, d):
                return es.enter_context(nc.sbuf_tensor(n, s, d))

            def ps(n, s, d):
                return es.enter_context(nc.psum_tensor(n, s, d))
            wqb = sb("wqb", [128, 16, D], BF16)
            bwqb = B()
            for c4 in range(4):
                dma("pool", wqb[:, c4 * 4:(c4 + 1) * 4, :], w_query[c4 * 512:(c4 + 1) * 512, :].rearrange("(c p) n -> p c n", p=128), writes=[bwqb])
            A2 = sb("A2", [128, D], F32)
            B2 = sb("B2", [128, D], F32)
            g2b = sb("g2b", [128, D], F32)
            bA = B()
            dma("sp", A2[:], MODB[:, 4 * D:5 * D], writes=[bA])
            dma("sp", B2[:], MODB[:, 3 * D:4 * D], writes=[bA])
            dma("sp", g2b[:], g2_d.partition_broadcast(128), writes=[bA])
            op("dve", lambda e: e.scalar_tensor_tensor(out=A2[:], in0=A2[:], scalar=1.0, in1=g2b[:], op0=ALU.add, op1=ALU.mult), [bA], [bA])
            xt = [sb("xte%d" % i, [128, D], F32) for i in range(2)]
            bxt = [B(), B()]
            ss = [sb("sse%d" % i, [128, 1], F32) for i in range(2)]
            bss = [B(), B()]
            hf = [g2b, sb("hfe1", [128, D], F32)]
            bhf = [bA, B()]
            hb = [sb("hbe%d" % i, [128, D], BF16) for i in range(2)]
            bhb = [B(), B()]
            hT4 = [sb("hT4_%d" % i, [128, 16, 512], BF16) for i in range(2)]
            bhT4 = [B(), B()]
            q2 = [sb("q2e%d" % i, [128, 4, 512], BF16) for i in range(2)]
            bq2 = [B(), B()]
            pT = [ps("pTe%d" % i, [128, 16 * 128], BF16) for i in range(2)]
            bpT = [B(), B()]
            pq = [ps("pqe%d" % i, [128, 512], F32) for i in range(4)]
            bpq = [B() for _ in range(4)]

            def Nn(tt):
                s = tt % 2
                dma("pool", xt[s][:], X1d[tt * 128:(tt + 1) * 128, :], writes=[bxt[s]])
                op("act", lambda e: e.activation(out=hf[s][:], in_=xt[s][:], func=AF.Square, accum_out=ss[s][:]), [bxt[s]], [bhf[s], bss[s]])
                op("act", lambda e: e.activation(out=ss[s][:], in_=ss[s][:], func=AF.Sqrt, scale=1.0 / D, bias=1e-6), [bss[s]], [bss[s]])
                op("dve", lambda e: e.reciprocal(out=ss[s][:], in_=ss[s][:]), [bss[s]], [bss[s]])
                op("dve", lambda e: e.scalar_tensor_tensor(out=hf[s][:], in0=xt[s][:], scalar=ss[s][:], in1=A2[:], op0=ALU.mult, op1=ALU.mult),
                   [bxt[s], bss[s], bA], [bhf[s]])
                op("pool", lambda e: e.tensor_tensor(out=hb[s][:], in0=hf[s][:], in1=B2[:], op=ALU.add), [bhf[s], bA], [bhb[s]])

            def TH(tt):
                s = tt % 2
                g = (tt // 4) % 2
                j = tt % 4
                for c in range(16):
                    op("pe", lambda e: e.transpose(out=pT[s][:, c * 128:(c + 1) * 128], in_=hb[s][:, c * 128:(c + 1) * 128], identity=identb[:]),
                       [bhb[s]] + CONST, [bpT[s]])
                op("act", lambda e: e.copy(out=hT4[g][:, :, j * 128:(j + 1) * 128], in_=pT[s][:].rearrange("p (c t) -> p c t", c=16)), [bpT[s]], [bhT4[g]])

            def QM(grp):
                g = grp % 2
                dma("sp", H2Td[:, :, grp * 512:(grp + 1) * 512].rearrange("c p t -> p c t"), hT4[g][:], reads=[bhT4[g]])
                for hp4 in range(4):
                    z = hp4 % 2
                    for j in range(4):
                        hp = hp4 * 4 + j
                        for c in range(16):
                            op("pe", lambda e: e.matmul(pq[j][:], lhsT=wqb[:, c, hp * 128:(hp + 1) * 128], rhs=hT4[g][:, c, :],
                                                        start=(c == 0), stop=(c == 15)), [bwqb, bhT4[g]], [bpq[j]])
                        op("act", lambda e: e.copy(out=q2[z][:, j, :], in_=pq[j][:]), [bpq[j]], [bq2[z]])
                    dma("sp", Q2Td[hp4 * 4:(hp4 + 1) * 4, :, grp * 512:(grp + 1) * 512].rearrange("c p t -> p c t"), q2[z][:], reads=[bq2[z]])

            Nn(0)
            Nn(1)
            for tt in range(16):
                TH(tt)
                if tt + 2 < 16:
                    Nn(tt + 2)
                if tt % 4 == 3:
                    QM(tt // 4)
            phase_end()
        if stop_after == 5:
            return nc

        with ExitStack() as es:
            def sb(n, s, d):
                return es.enter_context(nc.sbuf_tensor(n, s, d))

            def ps(n, s, d):
                return es.enter_context(nc.psum_tensor(n, s, d))
            skb = sb("skb", [128, 16, 128], BF16)
            bsk = B()
            dma("pool", skb[:], skT_d[:, :, :], writes=[bsk])
            q2 = [sb("q2b%d" % i, [128, 16, 128], BF16) for i in range(2)]
            bq2 = [B(), B()]
            S = [sb("S%d" % i, [128, 16, 128], F32) for i in range(2)]
            bSS = [B(), B()]
            S2 = sb("S2", [128, 16, 128], F32)
            V16 = sb("V16", [128, 16, 16], F32)
            I16 = sb("I16", [128, 16, 16], U32)
            I16f = sb("I16f", [128, 16, 16], F32)
            cand = sb("cand", [128, 8, 256], F32)
            cand2 = sb("cand2", [128, 8, 256], F32)
            T16 = sb("T16", [128, 8, 16], F32)
            P16 = sb("P16", [128, 8, 16], U32)
            Pa = sb("Pa", [128, 8, 16], U32)
            Pb = sb("Pb", [128, 8, 16], U32)
            Paf = sb("Paf", [128, 8, 16], F32)
            Pbf = sb("Pbf", [128, 8, 16], F32)
            negm = sb("negm", [128, 8], F32)
            Z = sb("Z", [128, 8], F32)
            E = sb("E", [128, 8, 16], F32)
            oh = sb("oh", [128, 8, 16, 16], F32)
            tok = sb("tok", [128, 3, 128], F32)
            tokT = [sb("tokT%d" % i, [128, 3, 128], F32) for i in range(2)]
            btokT = [B(), B()]
            bS = B()
            bH = [B() for _ in range(16)]
            bG2 = [B() for _ in range(8)]
            TB = 16
            NB = 4
            Ab = [sb("Ab%d" % i, [128, TB, 128], BF16) for i in range(NB)]
            Bb = [sb("Bb%d" % i, [128, TB, 128], BF16) for i in range(NB)]
            bAb = [[B() for _ in range(TB)] for _ in range(NB)]
            bBb = [B() for _ in range(NB)]
            Gs = [sb("Gs%d" % i, [128, 128, 128], BF16) for i in range(2)]
            bGs = [B(), B()]
            pG = [ps("pG%d" % i, [128, 1024], F32) for i in range(2)]
            bpG = [B(), B()]
            pSc = ps("pSc", [128, 1024], F32)
            bpSc = B()
            pX = ps("pXb", [128, 512], F32)
            bpX = B()
            iota_b16 = iota_f[:, 0:16]

            def scores(tt):
                s = tt % 2
                dma("sp", q2[s][:], Q2Td[:, :, tt * 128:(tt + 1) * 128].rearrange("c p t -> p c t"), writes=[bq2[s]])
                for rnd in range(2):
                    for h8 in range(8):
                        hp = rnd * 8 + h8
                        op("pe", lambda e: e.matmul(pSc[:, h8 * 128:(h8 + 1) * 128], lhsT=q2[s][:, hp, :], rhs=skb[:, hp, :], start=True, stop=True),
                           [bq2[s], bsk], [bpSc])
                    op("act", lambda e: e.copy(out=S[s][:, rnd * 8:(rnd + 1) * 8, :].rearrange("p a n -> p (a n)"), in_=pSc[:]), [bpSc], [bSS[s]])

            gcount = [0]

            def R1(tt):
                s = tt % 2
                Sc = S[s]
                R = [bS]
                HB = [[bH[hp]] for hp in range(16)]
                for hp in range(16):
                    op("dve", lambda e: e.max(out=V16[:, hp, 0:8], in_=Sc[:, hp, :]), [bSS[s]], HB[hp])
                for hp in range(16):
                    op("dve", lambda e: e.max_index(out=I16[:, hp, 0:8], in_max=V16[:, hp, 0:8], in_values=Sc[:, hp, :]), [bSS[s]] + HB[hp], HB[hp])
                for hp in range(16):
                    op("dve", lambda e: e.match_replace(out=S2[:, hp, :], in_to_replace=V16[:, hp, 0:8], in_values=Sc[:, hp, :], imm_value=-1e30),
                       [bSS[s]] + HB[hp], HB[hp])
                for hp in range(16):
                    op("dve", lambda e: e.max(out=V16[:, hp, 8:16], in_=S2[:, hp, :]), HB[hp], HB[hp])
                for hp in range(16):
                    op("dve", lambda e: e.max_index(out=I16[:, hp, 8:16], in_max=V16[:, hp, 8:16], in_values=S2[:, hp, :]), HB[hp], HB[hp])
                ALLH = [bH[hp] for hp in range(16)]
                op("dve", lambda e: e.tensor_copy(out=I16f[:], in_=I16[:]), ALLH, R)
                Vv = V16[:].rearrange("p (h two) k -> p h two k", two=2)
                cv = cand[:].rearrange("p h (a b) -> p h a b", a=16)
                op("dve", lambda e: e.tensor_tensor(out=cv, in0=Vv[:, :, 0, :].unsqueeze(3).to_broadcast([128, 8, 16, 16]),
                                                    in1=Vv[:, :, 1, :].unsqueeze(2).to_broadcast([128, 8, 16, 16]), op=ALU.add), ALLH + R, R)
                GB = [[bG2[h]] for h in range(8)]
                for h in range(8):
                    op("dve", lambda e: e.max(out=T16[:, h, 0:8], in_=cand[:, h, :]), R, GB[h])
                for h in range(8):
                    op("dve", lambda e: e.max_index(out=P16[:, h, 0:8], in_max=T16[:, h, 0:8], in_values=cand[:, h, :]), R + GB[h], GB[h])
                for h in range(8):
                    op("dve", lambda e: e.match_replace(out=cand2[:, h, :], in_to_replace=T16[:, h, 0:8], in_values=cand[:, h, :], imm_value=-1e30),
                       R + GB[h], GB[h])
                for h in range(8):
                    op("dve", lambda e: e.max(out=T16[:, h, 8:16], in_=cand2[:, h, :]), GB[h], GB[h])
                for h in range(8):
                    op("dve", lambda e: e.max_index(out=P16[:, h, 8:16], in_max=T16[:, h, 8:16], in_values=cand2[:, h, :]), GB[h], GB[h])
                ALLG = [bG2[h] for h in range(8)]
                op("dve", lambda e: e.tensor_scalar(out=negm[:], in0=T16[:, :, 0], scalar1=-1.0, scalar2=None, op0=ALU.mult), R + ALLG, R + ALLG)
                for h in range(8):
                    op("act", lambda e: e.activation(out=E[:, h, :], in_=T16[:, h, :], func=AF.Exp, bias=negm[:, h:h + 1], accum_out=Z[:, h:h + 1]),
                       R + [bG2[h]], R)

            def R2(tt):
                s = tt % 2
                R = [bS]
                Iv = I16f[:].rearrange("p (h two) k -> p h two k", two=2)
                op("dve", lambda e: e.reciprocal(out=Z[:], in_=Z[:]), R, R)
                gv = tok[:, 2, :].rearrange("p (h k) -> p h k", h=8)
                op("dve", lambda e: e.tensor_tensor(out=gv, in0=E[:], in1=Z[:].unsqueeze(2).to_broadcast([128, 8, 16]), op=ALU.mult), R, R)
                ALLG = [bG2[h] for h in range(8)]
                op("dve", lambda e: e.tensor_single_scalar(out=Pa[:], in_=P16[:], scalar=4, op=ALU.logical_shift_right), R + ALLG, R)
                op("dve", lambda e: e.tensor_single_scalar(out=Pb[:], in_=P16[:], scalar=15, op=ALU.bitwise_and), R + ALLG, R)
                op("dve", lambda e: e.tensor_copy(out=Paf[:], in_=Pa[:]), R, R)
                op("dve", lambda e: e.tensor_copy(out=Pbf[:], in_=Pb[:]), R, R)
                io4 = iota_b16.unsqueeze(1).unsqueeze(1).to_broadcast([128, 8, 16, 16])
                for which, Pf in ((0, Paf), (1, Pbf)):
                    op("dve", lambda e: e.tensor_tensor(out=oh[:], in0=Pf[:].unsqueeze(3).to_broadcast([128, 8, 16, 16]), in1=io4, op=ALU.is_equal), R + CONST, R)
                    op("dve", lambda e: e.tensor_tensor(out=oh[:], in0=oh[:], in1=Iv[:, :, which, :].unsqueeze(2).to_broadcast([128, 8, 16, 16]),
                                                        op=ALU.mult), R, R)
                    op("dve", lambda e: e.tensor_reduce(out=tok[:, which, :].rearrange("p (h k) -> p h k", h=8), in_=oh[:], axis=AX.X, op=ALU.add), R, R)
                for j in range(3):
                    op("pe", lambda e: e.transpose(out=pX[:, j * 128:(j + 1) * 128], in_=tok[:, j, :], identity=identf[:]), R + CONST, [bpX])
                op("act", lambda e: e.copy(out=tokT[s][:].rearrange("p a t -> p (a t)"), in_=pX[:, 0:384]), [bpX], [btokT[s]])

            def OH(tt, sb_lo, sb_hi):
                s = tt % 2
                g = tt % 2
                tT = tokT[s]
                RT = [btokT[s]]
                for sbi in range(sb_lo, sb_hi):
                    a = sbi % NB
                    t0 = sbi * TB
                    iob = iota_f[:].unsqueeze(1).to_broadcast([128, TB, 128])
                    op("dve", lambda e: e.tensor_tensor(out=Bb[a][:], in0=iob, in1=tT[:, 1, t0:t0 + TB].unsqueeze(2).to_broadcast([128, TB, 128]),
                                                        op=ALU.is_equal), RT + CONST, [bBb[a]])
                    for tl in range(TB):
                        op("dve", lambda e: e.tensor_scalar(out=Ab[a][:, tl, :], in0=iota_b[:], scalar1=tT[:, 0, t0 + tl:t0 + tl + 1],
                                                            scalar2=tT[:, 2, t0 + tl:t0 + tl + 1], op0=ALU.is_equal, op1=ALU.mult),
                           RT + CONST, [bAb[a][tl]])
                    for q8 in range(TB // 8):
                        pz = gcount[0] % 2
                        gcount[0] += 1
                        for tl in range(8):
                            tloc = q8 * 8 + tl
                            bank = tl // 4
                            oap = pG[pz][:, bank * 512:(bank + 1) * 512].rearrange("j (i t) -> j t i", t=4)[:, tl % 4, :]
                            op("pe", lambda e: e.matmul(oap, lhsT=Bb[a][:, tloc, :], rhs=Ab[a][:, tloc, :], start=True, stop=True),
                               [bBb[a], bAb[a][tloc]], [bpG[pz]])
                        tg0 = t0 + q8 * 8
                        op("act", lambda e: e.copy(out=Gs[g][:, :, tg0:tg0 + 8].rearrange("j i (b t) -> j b i t", b=2),
                                                   in_=pG[pz][:].rearrange("j (b i t) -> j b i t", b=2, t=4)), [bpG[pz]], [bGs[g]])

            def Gst(tt):
                g = tt % 2
                for i8 in range(8):
                    dma("sp" if i8 % 2 else "act", Gd[i8 * 16:(i8 + 1) * 16, :, tt * 128:(tt + 1) * 128].rearrange("i j t -> j i t"),
                        Gs[g][:, i8 * 16:(i8 + 1) * 16, :], reads=[bGs[g]])

            scores(0)
            R1(0)
            R2(0)
            scores(1)
            NSB = 128 // TB
            for tt in range(16):
                if tt + 1 < 16:
                    R1(tt + 1)
                OH(tt, 0, NSB - 2)
                if tt + 1 < 16:
                    R2(tt + 1)
                if tt + 2 < 16:
                    scores(tt + 2)
                OH(tt, NSB - 2, NSB)
                Gst(tt)
            phase_end()
        if stop_after == 6:
            return nc

        with ExitStack() as es:
            def sb(n, s, d):
                return es.enter_context(nc.sbuf_tensor(n, s, d))

            def ps(n, s, d):
                return es.enter_context(nc.psum_tensor(n, s, d))
            TP = 1024
            NTT = TP // 128
            GI = 4
            NCH = 128
            yacc = sb("yacc", [128, NTT, D], F32)
            byacc = [[B(), B()] for _ in range(NTT)]
            h2T = sb("h2T", [128, 16, TP], BF16)
            bh2T = B()
            NU_ = 4
            ub = [sb("ub%d" % i, [128, D], BF16) for i in range(NU_)]
            bub = [B() for _ in range(NU_)]
            UT = [sb("UT%d" % i, [128, 16, 128], BF16) for i in range(2)]
            bUT = [B(), B()]
            Vb = [sb("Vb%d" % i, [128, GI, D], BF16) for i in range(2)]
            bVb = [[B() for _ in range(GI)] for _ in range(2)]
            gst = [sb("gst%d" % i, [128, TP], BF16) for i in range(NU_)]
            bgst = [B() for _ in range(NU_)]
            ga = [sb("ga%d" % i, [128, 512], BF16) for i in range(2)]
            bga = [B(), B()]
            NW = GI + 1
            Wg = sb("Wg", [128, NW, TP], BF16)
            bWg = [B() for _ in range(NW)]
            gate2 = sb("gate2", [128, D], F32)
            fgb = sb("fgb", [128, D], F32)
            bgf = B()
            xb_ = [sb("xbf%d" % i, [128, D], F32) for i in range(2)]
            bxb_ = [B(), B()]
            ssf = [sb("ssf%d" % i, [128, 1], F32) for i in range(2)]
            bssf = [B(), B()]
            dma("sp", gate2[:], MODB[:, 5 * D:6 * D], writes=[bgf])
            dma("sp", fgb[:], fg_d.partition_broadcast(128), writes=[bgf])
            pTu = ps("pTu", [128, 16 * 128], BF16)
            bpTu = B()
            pa = [ps("pa%d" % i, [128, 512], F32) for i in range(4)]
            bpa = [B() for _ in range(4)]
            py = [ps("py%d" % i, [128, 512], F32) for i in range(2)]
            bpy = [B(), B()]
            ycnt = [0]
            def final_tile(tb_, tt):
                s = tt % 2
                xb, bxb = xb_[s], bxb_[s]
                r0 = tb_ + tt * 128
                YB = byacc[tt]
                dma("sp", xb[:], X1d[r0:r0 + 128, :], writes=[bxb])
                op("dve", lambda e: e.tensor_tensor(out=yacc[:, tt, :], in0=yacc[:, tt, :], in1=gate2[:], op=ALU.mult), YB + [bgf], YB)
                op("dve", lambda e: e.tensor_tensor(out=yacc[:, tt, :], in0=yacc[:, tt, :], in1=xb[:], op=ALU.add), YB + [bxb], YB)
                op("act", lambda e: e.activation(out=xb[:], in_=yacc[:, tt, :], func=AF.Square, accum_out=ssf[s][:]), YB, [bxb, bssf[s]])
                op("act", lambda e: e.activation(out=ssf[s][:], in_=ssf[s][:], func=AF.Sqrt, scale=1.0 / D, bias=1e-6), [bssf[s]], [bssf[s]])
                op("dve", lambda e: e.reciprocal(out=ssf[s][:], in_=ssf[s][:]), [bssf[s]], [bssf[s]])
                op("dve", lambda e: e.scalar_tensor_tensor(out=xb[:], in0=yacc[:, tt, :], scalar=ssf[s][:], in1=fgb[:], op0=ALU.mult, op1=ALU.mult),
                   YB + [bssf[s], bgf], [bxb])
                dma("sp", out_d[r0:r0 + 128, :], xb[:], reads=[bxb])

            pending_final = []
            for pas in range(NT // TP):
                tbase = pas * TP
                dma("sp", h2T[:], H2Td[:, :, tbase:tbase + TP].rearrange("c p t -> p c t"), writes=[bh2T])

                def loadU(i):
                    dma("pool", ub[i % NU_][:], peer_u[i * 128:(i + 1) * 128, :], writes=[bub[i % NU_]])
                    dma("sp", gst[i % NU_][:], Gd[i, :, tbase:tbase + TP], writes=[bgst[i % NU_]])

                def loadV(grp):
                    for gi in range(GI):
                        i = grp * GI + gi
                        dma("pool", Vb[grp % 2][:, gi, :], peer_v[i * 128:(i + 1) * 128, :], writes=[bVb[grp % 2][gi]])

                def Tr(i):
                    u2 = i % 2
                    for dc in range(16):
                        op("pe", lambda e: e.transpose(out=pTu[:, dc * 128:(dc + 1) * 128], in_=ub[i % NU_][:, dc * 128:(dc + 1) * 128], identity=identb[:]),
                           [bub[i % NU_]] + CONST, [bpTu])
                    op("act", lambda e: e.copy(out=UT[u2][:].rearrange("p a j -> p (a j)"), in_=pTu[:]), [bpTu], [bUT[u2]])

                def Amm(i):
                    u2 = i % 2
                    ws = i % NW
                    for dc in range(16):
                        for tg in range(2):
                            pz = (i % 2) * 2 + tg
                            op("pe", lambda e: e.matmul(pa[pz][:], lhsT=UT[u2][:, dc, :], rhs=h2T[:, dc, tg * 512:(tg + 1) * 512],
                                                        start=(dc == 0), stop=(dc == 15)), [bUT[u2], bh2T], [bpa[pz]])
                    for tg in range(2):
                        pz = (i % 2) * 2 + tg
                        op("act", lambda e: e.activation(out=ga[tg][:], in_=pa[pz][:], func=AF.Gelu), [bpa[pz]], [bga[tg]])
                        op("dve", lambda e: e.tensor_tensor(out=Wg[:, ws, tg * 512:(tg + 1) * 512], in0=ga[tg][:], in1=gst[i % NU_][:, tg * 512:(tg + 1) * 512],
                                                            op=ALU.mult), [bga[tg], bgst[i % NU_]], [bWg[ws]])

                def Ymm(grp, final_tb=None):
                    vb_ = Vb[grp % 2]
                    bv_ = bVb[grp % 2]
                    for tt in range(NTT):
                        for qd in range(4):
                            z = ycnt[0] % 2
                            ycnt[0] += 1
                            for gi in range(GI):
                                ws = (grp * GI + gi) % NW
                                op("pe", lambda e: e.matmul(py[z][:], lhsT=Wg[:, ws, tt * 128:(tt + 1) * 128],
                                                            rhs=vb_[:, gi, qd * 512:(qd + 1) * 512], start=(gi == 0), stop=(gi == GI - 1)),
                                   [bWg[ws], bv_[gi]], [bpy[z]])
                            ysl = yacc[:, tt, qd * 512:(qd + 1) * 512]
                            if grp == 0:
                                op("dve", lambda e: e.tensor_copy(out=ysl, in_=py[z][:]), [bpy[z]], [byacc[tt][qd // 2]])
                            else:
                                op("dve", lambda e: e.tensor_tensor(out=ysl, in0=py[z][:], in1=ysl, op=ALU.add),
                                   [bpy[z], byacc[tt][qd // 2]], [byacc[tt][qd // 2]])
                        if final_tb is not None:
                            final_tile(final_tb, tt)

                loadU(0)
                loadU(1)
                loadU(2)
                loadV(0)
                loadV(1)
                Tr(0)
                for i in range(NCH):
                    if i + 1 < NCH:
                        Tr(i + 1)
                    if i + 3 < NCH:
                        loadU(i + 3)
                    Amm(i)
                    if pending_final and i < GI:
                        for _ in range(NTT // GI):
                            final_tile(*pending_final.pop(0))
                    if i % GI == 0 and i > 0:
                        g_ = i // GI - 1
                        Ymm(g_)
                        if g_ + 2 < NCH // GI:
                            loadV(g_ + 2)
                last_pass = (pas + 1 == NT // TP)
                Ymm(NCH // GI - 1, final_tb=(tbase if last_pass else None))
                if pas + 1 < NT // TP:
                    pending_final = [(tbase, tt) for tt in range(NTT)]
                else:
                    pass
            phase_end()
    return nc


def make_in_maps(x, c, positions, w_mod, b_mod, norm1_g, w_in, w_pool, pool_scale,
                 w_out, norm2_g, w_query, sub_keys, peer_u, peer_v, final_g):
    f = np.float32
    x = np.asarray(x, f)
    shared = {
        "w_mod": np.ascontiguousarray(np.asarray(w_mod, f)[0]),
        "b_mod": np.ascontiguousarray(np.asarray(b_mod, f)[0][None, :]),
        "norm1_g": np.ascontiguousarray(np.asarray(norm1_g, f)[0][None, :]),
        "w_in": np.ascontiguousarray(np.asarray(w_in, f)[0]),
        "w_pool": np.ascontiguousarray(np.asarray(w_pool, f)[0]),
        "psT": np.ascontiguousarray(np.asarray(pool_scale, f)[0].reshape(8, 128).T),
        "w_out": np.ascontiguousarray(np.asarray(w_out, f)[0]),
        "norm2_g": np.ascontiguousarray(np.asarray(norm2_g, f)[0][None, :]),
        "w_query": np.ascontiguousarray(np.asarray(w_query, f)[0]),
        "skT": np.ascontiguousarray(np.asarray(sub_keys, f)[0].reshape(16, 128, 128).transpose(2, 0, 1)),
        "peer_u": np.ascontiguousarray(np.asarray(peer_u, f)[0]),
        "peer_v": np.ascontiguousarray(np.asarray(peer_v, f)[0]),
        "final_g": np.ascontiguousarray(np.asarray(final_g, f)[None, :]),
        "ident": np.eye(128, dtype=f),
        "iota": np.arange(128, dtype=f)[None, :],
    }
    inv = 500000.0 ** (-np.arange(16, dtype=np.float64) * 2.0 / 32.0)
    shared["inv2"] = (inv / (2 * np.pi)).astype(f)[None, :]
    kq = np.arange(128)
    m_own = np.where(kq[None, :] >= kq[:, None], 0.0, NEG).astype(f)
    m_prev = np.where(kq[None, :] <= kq[:, None], 0.0, NEG).astype(f)
    m_none = np.full((128, 128), NEG, f)
    positions = np.asarray(positions, np.int32)
    c = np.asarray(c, f)
    maps = []
    for core in range(8):
        b, qt = divmod(core, 4)
        t0 = qt * NT
        m = dict(shared)
        m["x_own"] = np.ascontiguousarray(x[b, t0:t0 + NT])
        m["x_halo"] = np.ascontiguousarray(x[b, t0 - NT:t0]) if qt > 0 else np.zeros((NT, D), f)
        m["cT"] = np.ascontiguousarray(c[b].reshape(16, 128).T)
        pl = np.zeros(NTL, np.int32)
        pl[NT:] = positions[b, t0:t0 + NT]
        if qt > 0:
            pl[:NT] = positions[b, t0 - NT:t0]
        m["pos"] = np.ascontiguousarray(pl.reshape(32, 128).T)
        m["masks"] = np.ascontiguousarray(np.stack([m_own, m_prev, m_prev if qt > 0 else m_none], axis=1))
        tg = t0 + np.arange(NT)
        m["invcnt"] = np.stack([1.0 / np.minimum(tg + 1, p) for p in (2, 4, 8, 16)]).astype(f)
        m["flag"] = np.full((128, 1), 1.0 if qt > 0 else 0.0, f)
        maps.append(m)
    return maps


_NC = None


def kernel(**inputs):
    global _NC
    maps = make_in_maps(**inputs)
    nc = build()
    res = run_bass_kernel_spmd(nc, maps, core_ids=list(range(8)))
    out = np.zeros((2, 8192, D), np.float32)
    for core in range(8):
        b, qt = divmod(core, 4)
        out[b, qt * NT:(qt + 1) * NT] = res.results[core]["out"]
    return out
```

```python
import numpy as np
import concourse.bass as bass
import concourse.mybir as mybir
from concourse.bass_utils import run_bass_kernel_spmd
from contextlib import ExitStack

F32 = mybir.dt.float32
BF16 = mybir.dt.bfloat16
I32 = mybir.dt.int32
U32 = mybir.dt.uint32
AF = mybir.ActivationFunctionType
ALU = mybir.AluOpType
AX = mybir.AxisListType

NDS = 48
D = 2048
NT = 2048
NTL = 4096
NEG = -30000.0


class Buf:
    __slots__ = ("w", "r")

    def __init__(self):
        self.w = None
        self.r = []


def _compress(toks):
    best = {}
    for t in toks:
        key = (t[0], t[1])
        if key not in best or best[key][2] < t[2]:
            best[key] = t
    return list(best.values())


class KB:
    def __init__(self, nc):
        self.nc = nc
        self.engs = {"pe": nc.tensor, "dve": nc.vector, "act": nc.scalar,
                     "pool": nc.gpsimd, "sp": nc.sync}
        self.sem = {e: nc.alloc_semaphore(name="s_" + e) for e in self.engs}
        self.cnt = {e: 0 for e in self.engs}
        self.seen = {e: {} for e in self.engs}
        self.dsem = [nc.alloc_semaphore(name="d%d" % i) for i in range(NDS)]
        self.dcnt = [0] * NDS
        self.dnext = 0
        self.ninst = 0

    def _wait(self, eng, tok):
        kind, src, k = tok
        if kind == "e":
            if src == eng and eng == "pe":
                return
            key = src
            sem = self.sem[src]
        else:
            key = ("d", src)
            sem = self.dsem[src]
        if self.seen[eng].get(key, 0) >= k:
            return
        self.engs[eng].wait_ge(sem, k)
        self.seen[eng][key] = k

    def _deps(self, eng, reads, writes):
        deps = []
        for b in reads:
            if b.w is not None:
                deps.append(b.w)
        for b in writes:
            if b.w is not None:
                deps.append(b.w)
            deps.extend(b.r)
        for d in deps:
            self._wait(eng, d)

    def _mark(self, tok, reads, writes):
        for b in reads:
            b.r.append(tok)
            if len(b.r) > 48:
                b.r = _compress(b.r)
        for b in writes:
            b.w = tok
            b.r = []

    def op(self, eng, fn, reads=(), writes=()):
        self._deps(eng, reads, writes)
        ins = fn(self.engs[eng])
        self.cnt[eng] += 1
        ins.then_inc(self.sem[eng], 1)
        tok = ("e", eng, self.cnt[eng])
        self._mark(tok, reads, writes)
        self.ninst += 1
        return tok

    def dma(self, q, out, in_, reads=(), writes=(), **kw):
        self._deps(q, reads, writes)
        j = self.dnext
        self.dnext = (self.dnext + 1) % NDS
        if self.dcnt[j] > 0:
            self._wait(q, ("d", j, self.dcnt[j]))
        ins = self.engs[q].dma_start(out=out, in_=in_, **kw)
        self.dcnt[j] += 16
        ins.then_inc(self.dsem[j], 16)
        tok = ("d", j, self.dcnt[j])
        self._mark(tok, reads, writes)
        self.ninst += 1
        return tok

    def barrier(self, engines=None):
        engines = engines or list(self.engs)
        for e in engines:
            for s in self.engs:
                if self.cnt[s] > 0 and not (s == e and e == "pe"):
                    self._wait(e, ("e", s, self.cnt[s]))
            for j in range(NDS):
                if self.dcnt[j] > 0:
                    self._wait(e, ("d", j, self.dcnt[j]))


def build(debug=False, stop_after=None):
    nc = bass.Bass("TRN2", target_bir_lowering=False)

    def din(name, shape, dt=F32):
        return nc.dram_tensor(name, shape, dt, kind="ExternalInput").ap()

    def dscr(name, shape, dt=F32):
        return nc.dram_tensor(name, shape, dt, kind="Internal").ap()

    x_own = din("x_own", [NT, D])
    x_halo = din("x_halo", [NT, D])
    cT_d = din("cT", [128, 16])
    pos_d = din("pos", [128, 32], I32)
    w_mod = din("w_mod", [D, 6 * D])
    b_mod = din("b_mod", [1, 6 * D])
    g1_d = din("norm1_g", [1, D])
    w_in = din("w_in", [D, 4096])
    w_pool = din("w_pool", [4, 256, 256])
    psT_d = din("psT", [128, 8])
    w_out = din("w_out", [D, D])
    g2_d = din("norm2_g", [1, D])
    w_query = din("w_query", [D, D])
    skT_d = din("skT", [128, 16, 128])
    peer_u = din("peer_u", [16384, D])
    peer_v = din("peer_v", [16384, D])
    fg_d = din("final_g", [1, D])
    ident_d = din("ident", [128, 128])
    masks_d = din("masks", [128, 3, 128])
    inv2_d = din("inv2", [1, 16])
    invcnt_d = din("invcnt", [4, NT])
    flag_d = din("flag", [128, 1])
    iota_d = din("iota", [1, 128])
    out_d = nc.dram_tensor("out", [NT, D], F32, kind="ExternalOutput").ap()

    okind = "ExternalOutput" if debug else "Internal"
    MODB = dscr("MODB", [128, 6 * D])
    QTd = dscr("QTd", [8, 128, NT], BF16)
    KTd = dscr("KTd", [8, 128, NTL], BF16)
    Vd = dscr("Vd", [NTL, 1024], BF16)
    UTd = dscr("UTd", [8, 128, 128 + NT])
    X1d = nc.dram_tensor("X1d", [NT, D], F32, kind=okind).ap()
    H2Td = dscr("H2Td", [16, 128, NT], BF16)
    Q2Td = dscr("Q2Td", [16, 128, NT], BF16)
    Gd = dscr("Gd", [128, 128, NT], BF16)
    MIXd = dscr("MIXd", [16, 128, NT], BF16)

    kb = KB(nc)
    op = kb.op
    dma = kb.dma
    B = Buf

    def phase_end():
        kb.barrier()

    with ExitStack() as es0:
        def sb0(n, s, d):
            return es0.enter_context(nc.sbuf_tensor(n, s, d))

        identf = sb0("identf", [128, 128], F32)
        identb = sb0("identb", [128, 128], BF16)
        onesb = sb0("onesb", [128, 128], BF16)
        iota_f = sb0("iota_f", [128, 128], F32)
        flag = sb0("flag_s", [128, 1], F32)
        b_const = B()
        dma("sp", identf[:], ident_d[:, :], writes=[b_const])
        dma("sp", iota_f[:], iota_d.partition_broadcast(128), writes=[b_const])
        dma("sp", flag[:], flag_d[:, :], writes=[b_const])
        op("dve", lambda e: e.tensor_copy(out=identb[:], in_=identf[:]), [b_const], [b_const])
        op("dve", lambda e: e.memset(onesb[:], 1.0), [], [b_const])
        iota_b = sb0("iota_b", [128, 128], BF16)
        op("dve", lambda e: e.tensor_copy(out=iota_b[:], in_=iota_f[:]), [b_const], [b_const])
        CONST = [b_const]

        with ExitStack() as es:
            def sb(n, s, d):
                return es.enter_context(nc.sbuf_tensor(n, s, d))

            def ps(n, s, d):
                return es.enter_context(nc.psum_tensor(n, s, d))
            cT = sb("cT_s", [128, 16], F32)
            sc = sb("sc_s", [128, 16], F32)
            srep = sb("srep", [128, 16, 128], BF16)
            NWM = 3
            wm = [sb("wm%d" % i, [128, 16, 512], BF16) for i in range(NWM)]
            bwm = [B() for _ in range(NWM)]
            bmb = sb("bmb", [128, 6 * D], F32)
            bbmb = B()
            mo = [sb("mo%d" % i, [128, 512], F32) for i in range(2)]
            bmo = [B(), B()]
            pm = [ps("pm%d" % i, [128, 512], F32) for i in range(2)]
            bpm = [B(), B()]
            b0 = B()
            dma("sp", cT[:], cT_d[:, :], writes=[b0])
            dma("sp", bmb[:], b_mod.partition_broadcast(128), writes=[bbmb])
            op("act", lambda e: e.activation(out=sc[:], in_=cT[:], func=AF.Silu), [b0], [b0])
            op("dve", lambda e: e.tensor_copy(out=srep[:], in_=sc[:].unsqueeze(2).to_broadcast([128, 16, 128])), [b0], [b0])

            def lw(g):
                dma("pool", wm[g % NWM][:], w_mod[:, g * 512:(g + 1) * 512].rearrange("(c p) n -> p c n", p=128), writes=[bwm[g % NWM]])
            lw(0)
            lw(1)
            for g in range(24):
                s = g % 2
                w3 = g % NWM
                if g + 2 < 24:
                    lw(g + 2)
                for c in range(16):
                    op("pe", lambda e: e.matmul(pm[s][:], lhsT=srep[:, c, :], rhs=wm[w3][:, c, :], start=(c == 0), stop=(c == 15)),
                       [b0, bwm[w3]], [bpm[s]])
                op("dve", lambda e: e.tensor_tensor(out=mo[s][:], in0=pm[s][:], in1=bmb[:, g * 512:(g + 1) * 512], op=ALU.add),
                   [bpm[s], bbmb], [bmo[s]])
                dma("sp", MODB[:, g * 512:(g + 1) * 512], mo[s][:], reads=[bmo[s]])
            phase_end()
        if stop_after == 0:
            return nc

        with ExitStack() as esM:

            with ExitStack() as es:
                def sb(n, s, d):
                    return es.enter_context(nc.sbuf_tensor(n, s, d))

                def ps(n, s, d):
                    return es.enter_context(nc.psum_tensor(n, s, d))
                wib = sb("wib", [128, 16, 2048], BF16)
                bwibg = [B() for _ in range(4)]

                def load_w(pas):
                    cols = [1024, 1536, 2048, 2560] if pas == 0 else [0, 512, 3072, 3584]
                    for wc in range(4):
                        c0 = cols[wc]
                        dma("pool", wib[:, :, wc * 512:(wc + 1) * 512],
                            w_in[:, c0:c0 + 512].rearrange("(c p) n -> p c n", p=128), writes=[bwibg[wc]])
                A1 = sb("A1", [128, D], F32)
                B1 = sb("B1", [128, D], F32)
                bA = B()
                g1b = sb("g1b", [128, D], F32)
                dma("sp", A1[:, 0:D], MODB[:, D:2 * D], writes=[bA])
                dma("sp", B1[:, 0:D], MODB[:, 0:D], writes=[bA])
                dma("sp", g1b[:], g1_d.partition_broadcast(128), writes=[bA])
                op("dve", lambda e: e.scalar_tensor_tensor(out=A1[:, 0:D], in0=A1[:, 0:D], scalar=1.0, in1=g1b[:],
                                                           op0=ALU.add, op1=ALU.mult), [bA], [bA])
                posi = sb("posi", [128, 32], I32)
                posf = sb("posf", [128, 32], F32)
                inv2 = sb("inv2s", [128, 16], F32)
                uu = sb("uu", [128, 32, 32], F32)
                ki = sb("ki", [128, 32, 32], I32)
                kf = sb("kf", [128, 32, 32], F32)
                tab = sb("tab", [128, 32, 32], F32)
                tabq = sb("tabq", [128, 32, 32], F32)
                bt = B()
                dma("sp", posi[:], pos_d[:, :], writes=[bt])
                dma("sp", inv2[:], inv2_d.partition_broadcast(128), writes=[bt])
                T = [bt]
                op("dve", lambda e: e.tensor_copy(out=posf[:], in_=posi[:]), T, T)
                op("dve", lambda e: e.tensor_tensor(out=uu[:, :, 0:16], in0=posf[:].unsqueeze(2).to_broadcast([128, 32, 16]),
                                                    in1=inv2[:].unsqueeze(1).to_broadcast([128, 32, 16]), op=ALU.mult), T, T)
                op("dve", lambda e: e.tensor_scalar(out=uu[:, :, 16:32], in0=uu[:, :, 0:16], scalar1=0.25, scalar2=None, op0=ALU.add), T, T)
                op("dve", lambda e: e.tensor_copy(out=ki[:], in_=uu[:]), T, T)
                op("dve", lambda e: e.tensor_copy(out=kf[:], in_=ki[:]), T, T)
                op("dve", lambda e: e.tensor_tensor(out=uu[:], in0=uu[:], in1=kf[:], op=ALU.subtract), T, T)
                op("dve", lambda e: e.tensor_single_scalar(out=kf[:], in_=uu[:], scalar=0.5, op=ALU.is_gt), T, T)
                op("dve", lambda e: e.tensor_tensor(out=uu[:], in0=uu[:], in1=kf[:], op=ALU.subtract), T, T)
                op("dve", lambda e: e.tensor_single_scalar(out=kf[:], in_=uu[:], scalar=-0.5, op=ALU.is_lt), T, T)
                op("dve", lambda e: e.tensor_tensor(out=uu[:], in0=uu[:], in1=kf[:], op=ALU.add), T, T)
                op("act", lambda e: e.activation(out=tab[:], in_=uu[:], func=AF.Sin, scale=2 * np.pi), T, T)
                op("dve", lambda e: e.tensor_scalar(out=tabq[:], in0=tab[:], scalar1=128 ** -0.5, scalar2=None, op0=ALU.mult), T, T)

                xt = [sb("xt%d" % i, [128, D], F32) for i in range(2)]
                bxt = [B(), B()]
                ss = [sb("ss%d" % i, [128, 1], F32) for i in range(2)]
                bss = [B(), B()]
                hf = [g1b, sb("hf1", [128, D], F32)]
                bhf = [bA, B()]
                hb = [sb("hb%d" % i, [128, D], BF16) for i in range(2)]
                bhb = [B(), B()]
                hT = [sb("hT%d" % i, [128, 16, 128], BF16) for i in range(2)]
                bhT = [B(), B()]
                zr = [sb("zr%d" % i, [128, 1024], F32) for i in range(2)]
                bzr = [B(), B()]
                zu = sb("zu", [128, 1024], F32)
                bzu = B()
                vb = [sb("vb%d" % i, [128, 1024], BF16) for i in range(2)]
                bvb = [B(), B()]
                rb = [sb("rb%d" % i, [128, 1024], BF16) for i in range(2)]
                brb = [B(), B()]
                rt = [sb("rt%d" % i, [128, 8, 16], F32) for i in range(4)]
                brt = B()
                rT = [sb("rTa%d" % i, [128, 8, 128], BF16) for i in range(2)]
                uT = [sb("uT%d" % i, [128, 8, 128], F32) for i in range(2)]
                brT, buT = [B(), B()], [B(), B()]
                pT = ps("pT", [128, 16 * 128], BF16)
                bpT = B()
                pz = [ps("pz%d" % i, [128, 512], F32) for i in range(3)]
                bpz = [B(), B(), B()]
                pqk = ps("pqk", [128, 8 * 128], BF16)
                bpqk = B()
                pu = ps("pu", [128, 8 * 128], F32)
                bpu = B()
                zc = [0]

                def Nn(pas, lt, s):
                    own = lt >= 16
                    src = x_own[(lt - 16) * 128:(lt - 15) * 128, :] if own else x_halo[lt * 128:(lt + 1) * 128, :]
                    dma("pool", xt[s][:], src, writes=[bxt[s]])
                    op("act", lambda e: e.activation(out=hf[s][:], in_=xt[s][:], func=AF.Square, accum_out=ss[s][:]),
                       [bxt[s]], [bhf[s], bss[s]])
                    op("act", lambda e: e.activation(out=ss[s][:], in_=ss[s][:], func=AF.Sqrt, scale=1.0 / D, bias=1e-6),
                       [bss[s]], [bss[s]])
                    op("dve", lambda e: e.reciprocal(out=ss[s][:], in_=ss[s][:]), [bss[s]], [bss[s]])
                    op("dve", lambda e: e.scalar_tensor_tensor(out=hf[s][:], in0=xt[s][:], scalar=ss[s][:], in1=A1[:, 0:D],
                                                               op0=ALU.mult, op1=ALU.mult), [bxt[s], bss[s], bA], [bhf[s]])
                    op("dve", lambda e: e.tensor_tensor(out=hb[s][:], in0=hf[s][:], in1=B1[:, 0:D], op=ALU.add), [bhf[s], bA], [bhb[s]])

                def TH(pas, lt, s):
                    for c in range(16):
                        op("pe", lambda e: e.transpose(out=pT[:, c * 128:(c + 1) * 128], in_=hb[s][:, c * 128:(c + 1) * 128], identity=identb[:]),
                           [bhb[s]] + CONST, [bpT])
                    op("act", lambda e: e.copy(out=hT[s][:].rearrange("p c t -> p (c t)"), in_=pT[:]), [bpT], [bhT[s]])

                def MM(pas, lt, s, mid=None):
                    own = lt >= 16
                    if pas == 0:
                        groups = [(0, "r"), (1, "r"), (2, "v"), (3, "v")]
                    else:
                        groups = [(0, "r"), (1, "r"), (2, "u"), (3, "u")] if own else [(2, "u"), (3, "u")]
                    for wc, kind in groups:
                        z = zc[0] % 3
                        zc[0] += 1
                        for c in range(16):
                            op("pe", lambda e: e.matmul(pz[z][:], lhsT=hT[s][:, c, :], rhs=wib[:, c, wc * 512:(wc + 1) * 512],
                                                        start=(c == 0), stop=(c == 15)), [bhT[s], bwibg[wc]], [bpz[z]])
                        half = (wc % 2) * 512
                        if kind == "r":
                            op("act", lambda e: e.copy(out=zr[s][:, half:half + 512], in_=pz[z][:]), [bpz[z]], [bzr[s]])
                        elif kind == "v":
                            op("act", lambda e: e.copy(out=vb[s][:, half:half + 512], in_=pz[z][:]), [bpz[z]], [bvb[s]])
                        else:
                            op("act", lambda e: e.copy(out=zu[:, half:half + 512], in_=pz[z][:]), [bpz[z]], [bzu])
                        if kind == "r" and wc == 1:
                            rot(pas, lt, s)
                        if mid is not None and wc == 2:
                            mid()
                            mid = None
                    if mid is not None:
                        mid()
                    if pas == 0:
                        dma("sp", Vd[lt * 128:(lt + 1) * 128, :], vb[s][:], reads=[bvb[s]])

                def rot(pas, lt, s):
                    table, scale = (tab, 1.0) if pas == 0 else (tabq, 128 ** -0.5)
                    zv = zr[s][:].rearrange("p (h d) -> p h d", h=8)
                    dv = rb[s][:].rearrange("p (h d) -> p h d", h=8)
                    sn = table[:, lt, 0:16].unsqueeze(1).to_broadcast([128, 8, 16])
                    cs = table[:, lt, 16:32].unsqueeze(1).to_broadcast([128, 8, 16])
                    x1 = zv[:, :, 0:16]
                    x2 = zv[:, :, 16:32]
                    R = [bzr[s], bt, brt]
                    op("dve", lambda e: e.tensor_tensor(out=rt[0][:], in0=x1, in1=cs, op=ALU.mult), R, [brt])
                    op("dve", lambda e: e.tensor_tensor(out=rt[1][:], in0=x2, in1=sn, op=ALU.mult), R, [brt])
                    op("dve", lambda e: e.tensor_tensor(out=rt[2][:], in0=x2, in1=cs, op=ALU.mult), R, [brt])
                    op("dve", lambda e: e.tensor_tensor(out=rt[3][:], in0=x1, in1=sn, op=ALU.mult), R, [brt])
                    op("dve", lambda e: e.tensor_tensor(out=dv[:, :, 0:16], in0=rt[0][:], in1=rt[1][:], op=ALU.subtract), [brt], [brb[s]])
                    op("dve", lambda e: e.tensor_tensor(out=dv[:, :, 16:32], in0=rt[2][:], in1=rt[3][:], op=ALU.add), [brt], [brb[s]])
                    op("act", lambda e: e.activation(out=dv[:, :, 32:128], in_=zv[:, :, 32:128], func=AF.Copy, scale=scale), [bzr[s]], [brb[s]])

                def RO(pas, lt, s):
                    own = lt >= 16
                    if pas == 0 or own:
                        for h in range(8):
                            op("pe", lambda e: e.transpose(out=pqk[:, h * 128:(h + 1) * 128], in_=rb[s][:, h * 128:(h + 1) * 128], identity=identb[:]),
                               [brb[s]] + CONST, [bpqk])
                        op("act", lambda e: e.copy(out=rT[s][:].rearrange("p h t -> p (h t)"), in_=pqk[:]), [bpqk], [brT[s]])
                        if pas == 0:
                            dma("sp", KTd[:, :, lt * 128:(lt + 1) * 128].rearrange("h d t -> d h t"), rT[s][:], reads=[brT[s]])
                        else:
                            dma("sp", QTd[:, :, (lt - 16) * 128:(lt - 15) * 128].rearrange("h d t -> d h t"), rT[s][:], reads=[brT[s]])
                    if pas == 1:
                        for j in range(8):
                            op("pe", lambda e: e.transpose(out=pu[:, j * 128:(j + 1) * 128], in_=zu[:, j * 128:(j + 1) * 128], identity=identf[:]),
                               [bzu] + CONST, [bpu])
                        op("act", lambda e: e.copy(out=uT[s][:].rearrange("p c t -> p (c t)"), in_=pu[:]), [bpu], [buT[s]])
                        dma("sp", UTd[:, :, (lt - 15) * 128:(lt - 14) * 128].rearrange("c p t -> p c t"), uT[s][:], reads=[buT[s]])

                for pas in (0, 1):
                    load_w(pas)
                    tl_ = list(range(32)) if pas == 0 else list(range(15, 32))
                    n_ = len(tl_)
                    Nn(pas, tl_[0], 0)
                    TH(pas, tl_[0], 0)
                    if n_ > 1:
                        Nn(pas, tl_[1], 1)
                    for k in range(n_):
                        s = k % 2
                        mid_ = None
                        if k + 1 < n_:
                            mid_ = (lambda kk=k: TH(pas, tl_[kk + 1], (kk + 1) % 2))
                        MM(pas, tl_[k], s, mid=mid_)
                        if k + 2 < n_:
                            Nn(pas, tl_[k + 2], s)
                        RO(pas, tl_[k], s)
                phase_end()
            if stop_after == 1:
                return nc

            with ExitStack() as es:
                def sb(n, s, d):
                    return es.enter_context(nc.sbuf_tensor(n, s, d))

                def ps(n, s, d):
                    return es.enter_context(nc.psum_tensor(n, s, d))
                maskf = sb("maskf", [128, 3, 128], F32)
                maskb = sb("maskb", [128, 3, 128], BF16)
                bmk = B()
                dma("sp", maskf[:], masks_d[:, :, :], writes=[bmk])
                op("dve", lambda e: e.tensor_copy(out=maskb[:], in_=maskf[:]), [bmk], [bmk])
                QT = [sb("QT%d" % i, [128, NT], BF16) for i in range(2)]
                KT = [sb("KT%d" % i, [128, NTL], BF16) for i in range(2)]
                V1 = [sb("V1_%d" % i, [128, 17, 128], BF16) for i in range(2)]
                V4 = [sb("V4_%d" % i, [128, 4, 5, 128], BF16) for i in range(2)]
                V16 = [sb("V16_%d" % i, [128, 16, 2, 128], BF16) for i in range(2)]
                bld = [B(), B()]
                accs = [sb("acc%d" % i, [128, 2, NT], F32) for i in range(2)]
                baccs = [B(), B()]
                rec = sb("rec", [128, NT], F32)
                brec = B()
                mixh = [sb("mixh%d" % i, [128, NT], BF16) for i in range(2)]
                bmixh = [B(), B()]
                PT = [sb("PT%d" % i, [128, 2, 128], BF16) for i in range(2)]
                bPT = [B(), B()]
                pS = [ps("pS%d" % i, [128, 512], F32) for i in range(2)]
                bpS = [B(), B()]
                pO = [ps("pO%d" % i, [128, 512], F32) for i in range(2)]
                bpO = [B(), B()]
                def loads(h):
                    s = h % 2
                    W = [bld[s]]
                    dma("sp", QT[s][:], QTd[h, :, :], writes=W)
                    dma("sp", KT[s][:], KTd[h, :, :], writes=W)
                    hc = slice(h * 128, (h + 1) * 128)
                    dma("sp", V1[s][:], Vd[1920:4096, hc].rearrange("(b k) d -> k b d", k=128), writes=W)
                    v4src = Vd[1536:4096, hc].rearrange("(b k r) d -> r k b d", k=128, r=4)
                    for r in range(4):
                        dma("sp", V4[s][:, r, :, :], v4src[r], writes=W)
                    v16src = Vd[0:4096, hc].rearrange("(b k r) d -> r k b d", k=128, r=16)
                    for r in range(16):
                        dma("sp", V16[s][:, r, :, :], v16src[r], writes=W)

                def iters(h):
                    s = h % 2
                    out = []
                    for dil in (1, 4, 16):
                        nblk = NT // (128 * dil)
                        qv = QT[s][:].rearrange("d (n q r) -> d r n q", r=dil, q=128)
                        kv = KT[s][:].rearrange("d (b k r) -> d r b k", r=dil, k=128)
                        av = accs[s][:].rearrange("d s (n q r) -> d r n s q", r=dil, q=128)
                        for r in range(dil):
                            for n in range(nblk):
                                bo = nblk + n
                                bp = bo - 1
                                if dil == 1:
                                    vp, vo = V1[s][:, bp - 15, :], V1[s][:, bo - 15, :]
                                elif dil == 4:
                                    vp, vo = V4[s][:, r, bp - 3, :], V4[s][:, r, bo - 3, :]
                                else:
                                    vp, vo = V16[s][:, r, bp, :], V16[s][:, r, bo, :]
                                out.append(dict(s=s, q=qv[:, r, n, :], kp=kv[:, r, bp, :], ko=kv[:, r, bo, :], vp=vp, vo=vo,
                                                mprev=(maskb[:, 2, :] if n == 0 else maskb[:, 1, :]), acc=av[:, r, n, :, :], first=(dil == 1)))
                    return out

                def Sst(it, z):
                    s = it["s"]
                    R = [bld[s], bmk] + CONST
                    op("pe", lambda e: e.matmul(pS[z][:, 0:128], lhsT=it["kp"], rhs=it["q"], start=True, stop=False), R, [bpS[z]])
                    op("pe", lambda e: e.matmul(pS[z][:, 0:128], lhsT=identb[:], rhs=it["mprev"], start=False, stop=True), R, [bpS[z]])
                    op("pe", lambda e: e.matmul(pS[z][:, 128:256], lhsT=it["ko"], rhs=it["q"], start=True, stop=False), R, [bpS[z]])
                    op("pe", lambda e: e.matmul(pS[z][:, 128:256], lhsT=identb[:], rhs=maskb[:, 0, :], start=False, stop=True), R, [bpS[z]])
                    op("act", lambda e: e.activation(out=PT[z][:].rearrange("p a b -> p (a b)"), in_=pS[z][:, 0:256], func=AF.Exp),
                       [bpS[z]], [bPT[z]])

                def PVst(it, z):
                    s = it["s"]
                    R2 = [bld[s], bPT[z]] + CONST
                    op("pe", lambda e: e.matmul(pO[z][:, 0:128], lhsT=it["vp"], rhs=PT[z][:, 0, :], start=True, stop=False), R2, [bpO[z]])
                    op("pe", lambda e: e.matmul(pO[z][:, 0:128], lhsT=it["vo"], rhs=PT[z][:, 1, :], start=False, stop=True), R2, [bpO[z]])
                    op("pe", lambda e: e.matmul(pO[z][:, 128:256], lhsT=onesb[:], rhs=PT[z][:, 0, :], start=True, stop=False), R2, [bpO[z]])
                    op("pe", lambda e: e.matmul(pO[z][:, 128:256], lhsT=onesb[:], rhs=PT[z][:, 1, :], start=False, stop=True), R2, [bpO[z]])
                    po_v = pO[z][:, 0:256].rearrange("p (s q) -> p s q", s=2)
                    bacc = baccs[s]
                    if it["first"]:
                        op("dve", lambda e: e.tensor_copy(out=it["acc"], in_=po_v), [bpO[z]], [bacc])
                    else:
                        op("dve", lambda e: e.tensor_tensor(out=it["acc"], in0=po_v, in1=it["acc"], op=ALU.add), [bpO[z], bacc], [bacc])

                loads(0)
                for h in range(8):
                    s = h % 2
                    if h + 1 < 8:
                        loads(h + 1)
                    its = iters(h)
                    NI = len(its)
                    Sst(its[0], 0)
                    Sst(its[1], 1)
                    for i in range(NI):
                        PVst(its[i], i % 3 if False else i % 2)
                        if i + 2 < NI:
                            Sst(its[i + 2], i % 2)
                    op("act", lambda e: e.activation(out=rec[:], in_=accs[s][:, 1, :], func=AF.Ln), [baccs[s]], [brec])
                    op("act", lambda e: e.activation(out=rec[:], in_=rec[:], func=AF.Exp, scale=-1.0), [brec], [brec])
                    op("pool", lambda e: e.tensor_tensor(out=mixh[s][:], in0=accs[s][:, 0, :], in1=rec[:], op=ALU.mult),
                       [baccs[s], brec], [bmixh[s]])
                    dma("sp", MIXd[h, :, :], mixh[s][:], reads=[bmixh[s]])
                phase_end()
            if stop_after == 2:
                return nc

            with ExitStack() as esCD:
                wob = esCD.enter_context(nc.sbuf_tensor("wob", [128, 16, D], BF16))
                bwob = B()
                for c4 in range(4):
                    dma("pool", wob[:, c4 * 4:(c4 + 1) * 4, :], w_out[c4 * 512:(c4 + 1) * 512, :].rearrange("(c p) n -> p c n", p=128), writes=[bwob])
                mixT = esCD.enter_context(nc.sbuf_tensor("mixT", [128, 16, NT], BF16))
                bmix = [B() for _ in range(16)]
                bMIXd = [B() for _ in range(16)]
                for c in range(8):
                    dma("act", mixT[:, c, :], MIXd[c, :, :], writes=[bmix[c]])

                with ExitStack() as es:
                    def sb(n, s, d):
                        return es.enter_context(nc.sbuf_tensor(n, s, d))

                    def ps(n, s, d):
                        return es.enter_context(nc.psum_tensor(n, s, d))
                    NU = 128 + NT
                    psT = sb("psT_s", [128, 8], F32)
                    bps = B()
                    dma("sp", psT[:], psT_d[:, :], writes=[bps])
                    ut = [sb("ut%d" % i, [128, NU], F32) for i in range(2)]
                    but = [B(), B()]
                    sa = sb("sa", [128, NU], F32)
                    sbb = sb("sbb", [128, NU], F32)
                    bsa, bsb = B(), B()
                    icb = sb("icb", [128, NT], F32)
                    bic = B()
                    tmp = sb("ptmp", [128, NT], F32)
                    btmp = B()
                    rT = [sb("rT%d" % i, [128, NT], BF16) for i in range(2)]
                    brT = [B(), B()]
                    wpb = [sb("wpb%d" % i, [128, 2, 256], BF16) for i in range(2)]
                    bwpb = [B(), B()]
                    pp = [ps("pp%d" % i, [128, 512], F32) for i in range(2)]
                    bpp = [B(), B()]
                    mo_c = [sb("mo_c%d" % i, [128, NT], BF16) for i in range(2)]
                    bmo_c = [B(), B()]

                    def cloads(g):
                        dma("pool", wpb[g % 2][:], w_pool[g].rearrange("(cc p) e -> p cc e", p=128), writes=[bwpb[g % 2]])
                        dma("sp", icb[:], invcnt_d[g:g + 1, :].partition_broadcast(128), writes=[bic])
                        for cc in range(2):
                            dma("sp", ut[cc][:], UTd[2 * g + cc, :, :], writes=[but[cc]])
                    pit = 0
                    cloads(0)
                    for g in range(4):
                        p = (2, 4, 8, 16)[g]
                        for cc in range(2):
                            u = ut[cc]
                            op("dve", lambda e: e.tensor_scalar(out=u[:, 0:128], in0=u[:, 0:128], scalar1=flag[:], scalar2=None, op0=ALU.mult),
                               [but[cc]] + CONST, [but[cc]])
                            cur, bcur = u, but[cc]
                            nxt = [(sa, bsa), (sbb, bsb)]
                            step = 1
                            k = 0
                            while step < p:
                                lo = 2 * step - 1
                                dst, bdst = nxt[k % 2]
                                k += 1
                                op("dve", lambda e: e.tensor_tensor(out=dst[:, lo:NU], in0=cur[:, lo:NU], in1=cur[:, lo - step:NU - step], op=ALU.add),
                                   [bcur], [bdst])
                                cur, bcur = dst, bdst
                                step *= 2
                            op("dve", lambda e: e.tensor_tensor(out=tmp[:], in0=cur[:, 128:NU], in1=icb[:], op=ALU.mult), [bcur, bic], [btmp])
                            op("pool", lambda e: e.tensor_tensor(out=rT[cc][:], in0=tmp[:], in1=u[:, 128:NU], op=ALU.subtract),
                               [btmp, but[cc]], [brT[cc]])
                        if g + 1 < 4:
                            cloads(g + 1)
                        for ec in range(2):
                            for tg in range(4):
                                z = pit % 2
                                pit += 1
                                for cc in range(2):
                                    op("pe", lambda e: e.matmul(pp[z][:], lhsT=wpb[g % 2][:, cc, ec * 128:(ec + 1) * 128], rhs=rT[cc][:, tg * 512:(tg + 1) * 512],
                                                                start=(cc == 0), stop=(cc == 1)), [bwpb[g % 2], brT[cc]], [bpp[z]])
                                op("act", lambda e: e.activation(out=mo_c[ec][:, tg * 512:(tg + 1) * 512], in_=pp[z][:], func=AF.Copy,
                                                                 scale=psT[:, 2 * g + ec:2 * g + ec + 1]), [bpp[z], bps], [bmo_c[ec]])
                            chn = 8 + 2 * g + ec
                            dma("sp", MIXd[chn, :, :], mo_c[ec][:], reads=[bmo_c[ec]], writes=[bMIXd[chn]])
                            dma("sp", mixT[:, chn, :], MIXd[chn, :, :], reads=[bMIXd[chn]], writes=[bmix[chn]])
                    phase_end()
                if stop_after == 3:
                    return nc

                with ExitStack() as es:
                    def sb(n, s, d):
                        return es.enter_context(nc.sbuf_tensor(n, s, d))

                    def ps(n, s, d):
                        return es.enter_context(nc.psum_tensor(n, s, d))
                    gate1 = sb("gate1", [128, D], F32)
                    bg1 = B()
                    dma("sp", gate1[:], MODB[:, 2 * D:3 * D], writes=[bg1])
                    xt = [sb("xtd%d" % i, [128, D], F32) for i in range(2)]
                    bxt = [B(), B()]
                    t1 = [sb("t1d%d" % i, [128, D], F32) for i in range(2)]
                    bt1 = [B(), B()]
                    po = [ps("po%d" % i, [128, 512], F32) for i in range(4)]
                    bpo = [B() for _ in range(4)]
                    for tt in range(16):
                        s = tt % 2
                        dma("act", xt[s][:], x_own[tt * 128:(tt + 1) * 128, :], writes=[bxt[s]])
                        for ng in range(4):
                            for c in range(16):
                                op("pe", lambda e: e.matmul(po[ng][:], lhsT=mixT[:, c, tt * 128:(tt + 1) * 128], rhs=wob[:, c, ng * 512:(ng + 1) * 512],
                                                            start=(c == 0), stop=(c == 15)), [bmix[c], bwob], [bpo[ng]])
                            op("dve", lambda e: e.tensor_tensor(out=t1[s][:, ng * 512:(ng + 1) * 512], in0=po[ng][:], in1=gate1[:, ng * 512:(ng + 1) * 512],
                                                                op=ALU.mult), [bpo[ng], bg1], [bt1[s]])
                        op("pool", lambda e: e.tensor_tensor(out=t1[s][:], in0=t1[s][:], in1=xt[s][:], op=ALU.add), [bt1[s], bxt[s]], [bt1[s]])
                        dma("sp", X1d[tt * 128:(tt + 1) * 128, :], t1[s][:], reads=[bt1[s]])
                    phase_end()
        if stop_after == 4:
            return nc

        with ExitStack() as es:
            def sb(n, s, d):
                return es.enter_context(nc.sbuf_tensor(n, s, d))

            def ps(n, s, d):
                return es.enter_context(nc.psum_tensor(n, s, d))
            wqb = sb("wqb", [128, 16, D], BF16)
            bwqbg = [B() for _ in range(4)]
            for c4 in range(4):
                dma("pool", wqb[:, :, c4 * 512:(c4 + 1) * 512], w_query[:, c4 * 512:(c4 + 1) * 512].rearrange("(c p) n -> p c n", p=128), writes=[bwqbg[c4]])
            A2 = sb("A2", [128, D], F32)
            B2 = sb("B2", [128, D], F32)
            g2b = sb("g2b", [128, D], F32)
            bA = B()
            dma("sp", A2[:], MODB[:, 4 * D:5 * D], writes=[bA])
            dma("sp", B2[:], MODB[:, 3 * D:4 * D], writes=[bA])
            dma("sp", g2b[:], g2_d.partition_broadcast(128), writes=[bA])
            op("dve", lambda e: e.scalar_tensor_tensor(out=A2[:], in0=A2[:], scalar=1.0, in1=g2b[:], op0=ALU.add, op1=ALU.mult), [bA], [bA])
            NR = 4
            xt = [sb("xte%d" % i, [128, D], F32) for i in range(NR)]
            bxt = [B() for _ in range(NR)]
            ss = [sb("sse%d" % i, [128, 1], F32) for i in range(NR)]
            bss = [B() for _ in range(NR)]
            hf = [g2b] + [sb("hfe%d" % i, [128, D], F32) for i in range(1, NR)]
            bhf = [bA] + [B() for _ in range(1, NR)]
            hb = [sb("hbe%d" % i, [128, D], BF16) for i in range(NR)]
            bhb = [B() for _ in range(NR)]
            hT4 = [sb("hT4_%d" % i, [128, 16, 512], BF16) for i in range(2)]
            bhT4 = [B(), B()]
            q2 = [sb("q2e%d" % i, [128, 4, 512], BF16) for i in range(2)]
            bq2 = [B(), B()]
            pT = [ps("pTe%d" % i, [128, 16 * 128], BF16) for i in range(2)]
            bpT = [B(), B()]
            pq = [ps("pqe%d" % i, [128, 512], F32) for i in range(4)]
            bpq = [B() for _ in range(4)]

            def Nn(tt):
                s = tt % NR
                dma("pool", xt[s][:], X1d[tt * 128:(tt + 1) * 128, :], writes=[bxt[s]])
                op("act", lambda e: e.activation(out=hf[s][:], in_=xt[s][:], func=AF.Square, accum_out=ss[s][:]), [bxt[s]], [bhf[s], bss[s]])
                op("act", lambda e: e.activation(out=ss[s][:], in_=ss[s][:], func=AF.Sqrt, scale=1.0 / D, bias=1e-6), [bss[s]], [bss[s]])
                op("dve", lambda e: e.reciprocal(out=ss[s][:], in_=ss[s][:]), [bss[s]], [bss[s]])
                op("dve", lambda e: e.scalar_tensor_tensor(out=hf[s][:], in0=xt[s][:], scalar=ss[s][:], in1=A2[:], op0=ALU.mult, op1=ALU.mult),
                   [bxt[s], bss[s], bA], [bhf[s]])
                op("dve", lambda e: e.tensor_tensor(out=hb[s][:], in0=hf[s][:], in1=B2[:], op=ALU.add), [bhf[s], bA], [bhb[s]])

            def TH(tt):
                s = tt % NR
                g = (tt // 4) % 2
                j = tt % 4
                z = tt % 2
                for c in range(16):
                    op("pe", lambda e: e.transpose(out=pT[z][:, c * 128:(c + 1) * 128], in_=hb[s][:, c * 128:(c + 1) * 128], identity=identb[:]),
                       [bhb[s]] + CONST, [bpT[z]])
                op("act", lambda e: e.copy(out=hT4[g][:, :, j * 128:(j + 1) * 128], in_=pT[z][:].rearrange("p (c t) -> p c t", c=16)), [bpT[z]], [bhT4[g]])

            def QM(grp):
                g = grp % 2
                dma("sp", H2Td[:, :, grp * 512:(grp + 1) * 512].rearrange("c p t -> p c t"), hT4[g][:], reads=[bhT4[g]])
                for hp4 in range(4):
                    z = hp4 % 2
                    for j in range(4):
                        hp = hp4 * 4 + j
                        for c in range(16):
                            op("pe", lambda e: e.matmul(pq[j][:], lhsT=wqb[:, c, hp * 128:(hp + 1) * 128], rhs=hT4[g][:, c, :],
                                                        start=(c == 0), stop=(c == 15)), [bwqbg[hp4], bhT4[g]], [bpq[j]])
                        op("act", lambda e: e.copy(out=q2[z][:, j, :], in_=pq[j][:]), [bpq[j]], [bq2[z]])
                    dma("sp", Q2Td[hp4 * 4:(hp4 + 1) * 4, :, grp * 512:(grp + 1) * 512].rearrange("c p t -> p c t"), q2[z][:], reads=[bq2[z]])

            for t_ in range(4):
                Nn(t_)
            for tt in range(16):
                TH(tt)
                if tt % 4 == 3:
                    for t_ in range(tt + 1, min(tt + 5, 16)):
                        Nn(t_)
                    QM(tt // 4)
            phase_end()
        if stop_after == 5:
            return nc

        with ExitStack() as es:
            def sb(n, s, d):
                return es.enter_context(nc.sbuf_tensor(n, s, d))

            def ps(n, s, d):
                return es.enter_context(nc.psum_tensor(n, s, d))
            skb = sb("skb", [128, 16, 128], BF16)
            bsk = B()
            dma("pool", skb[:], skT_d[:, :, :], writes=[bsk])
            q2 = [sb("q2b%d" % i, [128, 16, 128], BF16) for i in range(2)]
            bq2 = [B(), B()]
            S = [sb("S%d" % i, [128, 16, 128], F32) for i in range(2)]
            bSS = [B(), B()]
            S2 = sb("S2", [128, 16, 128], F32)
            V16 = sb("V16", [128, 16, 16], F32)
            I16 = sb("I16", [128, 16, 16], U32)
            I16f = sb("I16f", [128, 16, 16], F32)
            cand = sb("cand", [128, 8, 256], F32)
            cand2 = sb("cand2", [128, 8, 256], F32)
            T16 = sb("T16", [128, 8, 16], F32)
            P16 = sb("P16", [128, 8, 16], U32)
            Pa = sb("Pa", [128, 8, 16], U32)
            Pb = sb("Pb", [128, 8, 16], U32)
            Paf = sb("Paf", [128, 8, 16], F32)
            Pbf = sb("Pbf", [128, 8, 16], F32)
            negm = sb("negm", [128, 8], F32)
            Z = sb("Z", [128, 8], F32)
            E = sb("E", [128, 8, 16], F32)
            oh = sb("oh", [128, 8, 16, 16], F32)
            tok = sb("tok", [128, 3, 128], F32)
            tokT = [sb("tokT%d" % i, [128, 3, 128], F32) for i in range(2)]
            btokT = [B(), B()]
            bS = B()
            bH = [B() for _ in range(16)]
            bG2 = [B() for _ in range(8)]
            TB = 16
            NB = 4
            Ab = [sb("Ab%d" % i, [128, TB, 128], BF16) for i in range(NB)]
            Bb = [sb("Bb%d" % i, [128, TB, 128], BF16) for i in range(NB)]
            bAb = [[B() for _ in range(TB)] for _ in range(NB)]
            bBb = [B() for _ in range(NB)]
            Gs = [sb("Gs%d" % i, [128, 128, 128], BF16) for i in range(2)]
            bGs = [B(), B()]
            pG = [ps("pG%d" % i, [128, 1024], F32) for i in range(2)]
            bpG = [B(), B()]
            pSc = ps("pSc", [128, 1024], F32)
            bpSc = B()
            pX = ps("pXb", [128, 512], F32)
            bpX = B()
            iota_b16 = iota_f[:, 0:16]

            def scores(tt):
                s = tt % 2
                dma("sp", q2[s][:], Q2Td[:, :, tt * 128:(tt + 1) * 128].rearrange("c p t -> p c t"), writes=[bq2[s]])
                for rnd in range(2):
                    for h8 in range(8):
                        hp = rnd * 8 + h8
                        op("pe", lambda e: e.matmul(pSc[:, h8 * 128:(h8 + 1) * 128], lhsT=q2[s][:, hp, :], rhs=skb[:, hp, :], start=True, stop=True),
                           [bq2[s], bsk], [bpSc])
                    op("act", lambda e: e.copy(out=S[s][:, rnd * 8:(rnd + 1) * 8, :].rearrange("p a n -> p (a n)"), in_=pSc[:]), [bpSc], [bSS[s]])

            gcount = [0]

            def R1(tt):
                s = tt % 2
                Sc = S[s]
                R = [bS]
                HB = [[bH[hp]] for hp in range(16)]
                for hp in range(16):
                    op("dve", lambda e: e.max(out=V16[:, hp, 0:8], in_=Sc[:, hp, :]), [bSS[s]], HB[hp])
                for hp in range(16):
                    op("dve", lambda e: e.max_index(out=I16[:, hp, 0:8], in_max=V16[:, hp, 0:8], in_values=Sc[:, hp, :]), [bSS[s]] + HB[hp], HB[hp])
                for hp in range(16):
                    op("dve", lambda e: e.match_replace(out=S2[:, hp, :], in_to_replace=V16[:, hp, 0:8], in_values=Sc[:, hp, :], imm_value=-1e30),
                       [bSS[s]] + HB[hp], HB[hp])
                for hp in range(16):
                    op("dve", lambda e: e.max(out=V16[:, hp, 8:16], in_=S2[:, hp, :]), HB[hp], HB[hp])
                for hp in range(16):
                    op("dve", lambda e: e.max_index(out=I16[:, hp, 8:16], in_max=V16[:, hp, 8:16], in_values=S2[:, hp, :]), HB[hp], HB[hp])
                ALLH = [bH[hp] for hp in range(16)]
                op("dve", lambda e: e.tensor_copy(out=I16f[:], in_=I16[:]), ALLH, R)
                Vv = V16[:].rearrange("p (h two) k -> p h two k", two=2)
                cv = cand[:].rearrange("p h (a b) -> p h a b", a=16)
                op("dve", lambda e: e.tensor_tensor(out=cv, in0=Vv[:, :, 0, :].unsqueeze(3).to_broadcast([128, 8, 16, 16]),
                                                    in1=Vv[:, :, 1, :].unsqueeze(2).to_broadcast([128, 8, 16, 16]), op=ALU.add), ALLH + R, R)
                GB = [[bG2[h]] for h in range(8)]
                for h in range(8):
                    op("dve", lambda e: e.max(out=T16[:, h, 0:8], in_=cand[:, h, :]), R, GB[h])
                for h in range(8):
                    op("dve", lambda e: e.max_index(out=P16[:, h, 0:8], in_max=T16[:, h, 0:8], in_values=cand[:, h, :]), R + GB[h], GB[h])
                for h in range(8):
                    op("dve", lambda e: e.match_replace(out=cand2[:, h, :], in_to_replace=T16[:, h, 0:8], in_values=cand[:, h, :], imm_value=-1e30),
                       R + GB[h], GB[h])
                for h in range(8):
                    op("dve", lambda e: e.max(out=T16[:, h, 8:16], in_=cand2[:, h, :]), GB[h], GB[h])
                for h in range(8):
                    op("dve", lambda e: e.max_index(out=P16[:, h, 8:16], in_max=T16[:, h, 8:16], in_values=cand2[:, h, :]), GB[h], GB[h])
                ALLG = [bG2[h] for h in range(8)]
                op("dve", lambda e: e.tensor_scalar(out=negm[:], in0=T16[:, :, 0], scalar1=-1.0, scalar2=None, op0=ALU.mult), R + ALLG, R + ALLG)
                for h in range(8):
                    op("act", lambda e: e.activation(out=E[:, h, :], in_=T16[:, h, :], func=AF.Exp, bias=negm[:, h:h + 1], accum_out=Z[:, h:h + 1]),
                       R + [bG2[h]], R)

            def R2(tt):
                s = tt % 2
                R = [bS]
                Iv = I16f[:].rearrange("p (h two) k -> p h two k", two=2)
                op("dve", lambda e: e.reciprocal(out=Z[:], in_=Z[:]), R, R)
                gv = tok[:, 2, :].rearrange("p (h k) -> p h k", h=8)
                op("dve", lambda e: e.tensor_tensor(out=gv, in0=E[:], in1=Z[:].unsqueeze(2).to_broadcast([128, 8, 16]), op=ALU.mult), R, R)
                ALLG = [bG2[h] for h in range(8)]
                op("dve", lambda e: e.tensor_single_scalar(out=Pa[:], in_=P16[:], scalar=4, op=ALU.logical_shift_right), R + ALLG, R)
                op("dve", lambda e: e.tensor_single_scalar(out=Pb[:], in_=P16[:], scalar=15, op=ALU.bitwise_and), R + ALLG, R)
                op("dve", lambda e: e.tensor_copy(out=Paf[:], in_=Pa[:]), R, R)
                op("dve", lambda e: e.tensor_copy(out=Pbf[:], in_=Pb[:]), R, R)
                io4 = iota_b16.unsqueeze(1).unsqueeze(1).to_broadcast([128, 8, 16, 16])
                for which, Pf in ((0, Paf), (1, Pbf)):
                    op("dve", lambda e: e.tensor_tensor(out=oh[:], in0=Pf[:].unsqueeze(3).to_broadcast([128, 8, 16, 16]), in1=io4, op=ALU.is_equal), R + CONST, R)
                    op("dve", lambda e: e.tensor_tensor(out=oh[:], in0=oh[:], in1=Iv[:, :, which, :].unsqueeze(2).to_broadcast([128, 8, 16, 16]),
                                                        op=ALU.mult), R, R)
                    op("dve", lambda e: e.tensor_reduce(out=tok[:, which, :].rearrange("p (h k) -> p h k", h=8), in_=oh[:], axis=AX.X, op=ALU.add), R, R)
                for j in range(3):
                    op("pe", lambda e: e.transpose(out=pX[:, j * 128:(j + 1) * 128], in_=tok[:, j, :], identity=identf[:]), R + CONST, [bpX])
                op("act", lambda e: e.copy(out=tokT[s][:].rearrange("p a t -> p (a t)"), in_=pX[:, 0:384]), [bpX], [btokT[s]])

            def OH(tt, sb_lo, sb_hi):
                s = tt % 2
                g = tt % 2
                tT = tokT[s]
                RT = [btokT[s]]
                for sbi in range(sb_lo, sb_hi):
                    a = sbi % NB
                    t0 = sbi * TB
                    iob = iota_f[:].unsqueeze(1).to_broadcast([128, TB, 128])
                    op("dve", lambda e: e.tensor_tensor(out=Bb[a][:], in0=iob, in1=tT[:, 1, t0:t0 + TB].unsqueeze(2).to_broadcast([128, TB, 128]),
                                                        op=ALU.is_equal), RT + CONST, [bBb[a]])
                    for tl in range(TB):
                        op("dve", lambda e: e.tensor_scalar(out=Ab[a][:, tl, :], in0=iota_b[:], scalar1=tT[:, 0, t0 + tl:t0 + tl + 1],
                                                            scalar2=tT[:, 2, t0 + tl:t0 + tl + 1], op0=ALU.is_equal, op1=ALU.mult),
                           RT + CONST, [bAb[a][tl]])
                    for q8 in range(TB // 8):
                        pz = gcount[0] % 2
                        gcount[0] += 1
                        for tl in range(8):
                            tloc = q8 * 8 + tl
                            bank = tl // 4
                            oap = pG[pz][:, bank * 512:(bank + 1) * 512].rearrange("j (i t) -> j t i", t=4)[:, tl % 4, :]
                            op("pe", lambda e: e.matmul(oap, lhsT=Bb[a][:, tloc, :], rhs=Ab[a][:, tloc, :], start=True, stop=True),
                               [bBb[a], bAb[a][tloc]], [bpG[pz]])
                        tg0 = t0 + q8 * 8
                        op("act", lambda e: e.copy(out=Gs[g][:, :, tg0:tg0 + 8].rearrange("j i (b t) -> j b i t", b=2),
                                                   in_=pG[pz][:].rearrange("j (b i t) -> j b i t", b=2, t=4)), [bpG[pz]], [bGs[g]])

            def Gst(tt):
                g = tt % 2
                for i8 in range(8):
                    dma("sp" if i8 % 2 else "act", Gd[i8 * 16:(i8 + 1) * 16, :, tt * 128:(tt + 1) * 128].rearrange("i j t -> j i t"),
                        Gs[g][:, i8 * 16:(i8 + 1) * 16, :], reads=[bGs[g]])

            scores(0)
            R1(0)
            R2(0)
            scores(1)
            NSB = 128 // TB
            for tt in range(16):
                if tt + 1 < 16:
                    R1(tt + 1)
                OH(tt, 0, NSB - 2)
                if tt + 1 < 16:
                    R2(tt + 1)
                if tt + 2 < 16:
                    scores(tt + 2)
                OH(tt, NSB - 2, NSB)
                Gst(tt)
            phase_end()
        if stop_after == 6:
            return nc

        with ExitStack() as es:
            def sb(n, s, d):
                return es.enter_context(nc.sbuf_tensor(n, s, d))

            def ps(n, s, d):
                return es.enter_context(nc.psum_tensor(n, s, d))
            TP = 1024
            NTT = TP // 128
            GI = 4
            NCH = 128
            yacc = sb("yacc", [128, NTT, D], F32)
            byacc = [[B(), B()] for _ in range(NTT)]
            h2T = sb("h2T", [128, 16, TP], BF16)
            bh2T = B()
            NU_ = 4
            ub = [sb("ub%d" % i, [128, D], BF16) for i in range(NU_)]
            bub = [B() for _ in range(NU_)]
            UT = [sb("UT%d" % i, [128, 16, 128], BF16) for i in range(2)]
            bUT = [B(), B()]
            Vb = [sb("Vb%d" % i, [128, GI, D], BF16) for i in range(2)]
            bVb = [[B() for _ in range(GI)] for _ in range(2)]
            gst = [sb("gst%d" % i, [128, TP], BF16) for i in range(NU_)]
            bgst = [B() for _ in range(NU_)]
            ga = [sb("ga%d" % i, [128, 512], BF16) for i in range(2)]
            bga = [B(), B()]
            NW = GI + 1
            Wg = sb("Wg", [128, NW, TP], BF16)
            bWg = [B() for _ in range(NW)]
            gate2 = sb("gate2", [128, D], F32)
            fgb = sb("fgb", [128, D], F32)
            bgf = B()
            xb_ = [sb("xbf%d" % i, [128, D], F32) for i in range(2)]
            bxb_ = [B(), B()]
            ssf = [sb("ssf%d" % i, [128, 1], F32) for i in range(2)]
            bssf = [B(), B()]
            dma("sp", gate2[:], MODB[:, 5 * D:6 * D], writes=[bgf])
            dma("sp", fgb[:], fg_d.partition_broadcast(128), writes=[bgf])
            pTu = ps("pTu", [128, 16 * 128], BF16)
            bpTu = B()
            pa = [ps("pa%d" % i, [128, 512], F32) for i in range(4)]
            bpa = [B() for _ in range(4)]
            py = [ps("py%d" % i, [128, 512], F32) for i in range(2)]
            bpy = [B(), B()]
            ycnt = [0]
            def final_tile(tb_, tt):
                s = tt % 2
                xb, bxb = xb_[s], bxb_[s]
                r0 = tb_ + tt * 128
                YB = byacc[tt]
                dma("sp", xb[:], X1d[r0:r0 + 128, :], writes=[bxb])
                op("dve", lambda e: e.tensor_tensor(out=yacc[:, tt, :], in0=yacc[:, tt, :], in1=gate2[:], op=ALU.mult), YB + [bgf], YB)
                op("dve", lambda e: e.tensor_tensor(out=yacc[:, tt, :], in0=yacc[:, tt, :], in1=xb[:], op=ALU.add), YB + [bxb], YB)
                op("act", lambda e: e.activation(out=xb[:], in_=yacc[:, tt, :], func=AF.Square, accum_out=ssf[s][:]), YB, [bxb, bssf[s]])
                op("act", lambda e: e.activation(out=ssf[s][:], in_=ssf[s][:], func=AF.Sqrt, scale=1.0 / D, bias=1e-6), [bssf[s]], [bssf[s]])
                op("dve", lambda e: e.reciprocal(out=ssf[s][:], in_=ssf[s][:]), [bssf[s]], [bssf[s]])
                op("dve", lambda e: e.scalar_tensor_tensor(out=xb[:], in0=yacc[:, tt, :], scalar=ssf[s][:], in1=fgb[:], op0=ALU.mult, op1=ALU.mult),
                   YB + [bssf[s], bgf], [bxb])
                dma("sp", out_d[r0:r0 + 128, :], xb[:], reads=[bxb])

            pending_final = []
            for pas in range(NT // TP):
                tbase = pas * TP
                dma("sp", h2T[:], H2Td[:, :, tbase:tbase + TP].rearrange("c p t -> p c t"), writes=[bh2T])

                def loadU(i):
                    dma("pool", ub[i % NU_][:], peer_u[i * 128:(i + 1) * 128, :], writes=[bub[i % NU_]])
                    dma("sp", gst[i % NU_][:], Gd[i, :, tbase:tbase + TP], writes=[bgst[i % NU_]])

                def loadV(grp):
                    for gi in range(GI):
                        i = grp * GI + gi
                        dma("pool", Vb[grp % 2][:, gi, :], peer_v[i * 128:(i + 1) * 128, :], writes=[bVb[grp % 2][gi]])

                def Tr(i):
                    u2 = i % 2
                    for dc in range(16):
                        op("pe", lambda e: e.transpose(out=pTu[:, dc * 128:(dc + 1) * 128], in_=ub[i % NU_][:, dc * 128:(dc + 1) * 128], identity=identb[:]),
                           [bub[i % NU_]] + CONST, [bpTu])
                    op("act", lambda e: e.copy(out=UT[u2][:].rearrange("p a j -> p (a j)"), in_=pTu[:]), [bpTu], [bUT[u2]])

                def Amm(i):
                    u2 = i % 2
                    ws = i % NW
                    for dc in range(16):
                        for tg in range(2):
                            pz = (i % 2) * 2 + tg
                            op("pe", lambda e: e.matmul(pa[pz][:], lhsT=UT[u2][:, dc, :], rhs=h2T[:, dc, tg * 512:(tg + 1) * 512],
                                                        start=(dc == 0), stop=(dc == 15)), [bUT[u2], bh2T], [bpa[pz]])
                    for tg in range(2):
                        pz = (i % 2) * 2 + tg
                        op("act", lambda e: e.activation(out=ga[tg][:], in_=pa[pz][:], func=AF.Gelu), [bpa[pz]], [bga[tg]])
                        op("dve", lambda e: e.tensor_tensor(out=Wg[:, ws, tg * 512:(tg + 1) * 512], in0=ga[tg][:], in1=gst[i % NU_][:, tg * 512:(tg + 1) * 512],
                                                            op=ALU.mult), [bga[tg], bgst[i % NU_]], [bWg[ws]])

                def Ymm(grp, final_tb=None):
                    vb_ = Vb[grp % 2]
                    bv_ = bVb[grp % 2]
                    for tt in range(NTT):
                        for qd in range(4):
                            z = ycnt[0] % 2
                            ycnt[0] += 1
                            for gi in range(GI):
                                ws = (grp * GI + gi) % NW
                                op("pe", lambda e: e.matmul(py[z][:], lhsT=Wg[:, ws, tt * 128:(tt + 1) * 128],
                                                            rhs=vb_[:, gi, qd * 512:(qd + 1) * 512], start=(gi == 0), stop=(gi == GI - 1)),
                                   [bWg[ws], bv_[gi]], [bpy[z]])
                            ysl = yacc[:, tt, qd * 512:(qd + 1) * 512]
                            if grp == 0:
                                op("dve", lambda e: e.tensor_copy(out=ysl, in_=py[z][:]), [bpy[z]], [byacc[tt][qd // 2]])
                            else:
                                op("dve", lambda e: e.tensor_tensor(out=ysl, in0=py[z][:], in1=ysl, op=ALU.add),
                                   [bpy[z], byacc[tt][qd // 2]], [byacc[tt][qd // 2]])
                        if final_tb is not None:
                            final_tile(final_tb, tt)

                loadU(0)
                loadU(1)
                loadU(2)
                loadV(0)
                loadV(1)
                Tr(0)
                for i in range(NCH):
                    if i + 1 < NCH:
                        Tr(i + 1)
                    if i + 3 < NCH:
                        loadU(i + 3)
                    Amm(i)
                    if pending_final and i < GI:
                        for _ in range(NTT // GI):
                            final_tile(*pending_final.pop(0))
                    if i % GI == 0 and i > 0:
                        g_ = i // GI - 1
                        Ymm(g_)
                        if g_ + 2 < NCH // GI:
                            loadV(g_ + 2)
                last_pass = (pas + 1 == NT // TP)
                Ymm(NCH // GI - 1, final_tb=(tbase if last_pass else None))
                if pas + 1 < NT // TP:
                    pending_final = [(tbase, tt) for tt in range(NTT)]
                else:
                    pass
            phase_end()
    return nc


def make_in_maps(x, c, positions, w_mod, b_mod, norm1_g, w_in, w_pool, pool_scale,
                 w_out, norm2_g, w_query, sub_keys, peer_u, peer_v, final_g):
    f = np.float32
    x = np.asarray(x, f)
    shared = {
        "w_mod": np.ascontiguousarray(np.asarray(w_mod, f)[0]),
        "b_mod": np.ascontiguousarray(np.asarray(b_mod, f)[0][None, :]),
        "norm1_g": np.ascontiguousarray(np.asarray(norm1_g, f)[0][None, :]),
        "w_in": np.ascontiguousarray(np.asarray(w_in, f)[0]),
        "w_pool": np.ascontiguousarray(np.asarray(w_pool, f)[0]),
        "psT": np.ascontiguousarray(np.asarray(pool_scale, f)[0].reshape(8, 128).T),
        "w_out": np.ascontiguousarray(np.asarray(w_out, f)[0]),
        "norm2_g": np.ascontiguousarray(np.asarray(norm2_g, f)[0][None, :]),
        "w_query": np.ascontiguousarray(np.asarray(w_query, f)[0]),
        "skT": np.ascontiguousarray(np.asarray(sub_keys, f)[0].reshape(16, 128, 128).transpose(2, 0, 1)),
        "peer_u": np.ascontiguousarray(np.asarray(peer_u, f)[0]),
        "peer_v": np.ascontiguousarray(np.asarray(peer_v, f)[0]),
        "final_g": np.ascontiguousarray(np.asarray(final_g, f)[None, :]),
        "ident": np.eye(128, dtype=f),
        "iota": np.arange(128, dtype=f)[None, :],
    }
    inv = 500000.0 ** (-np.arange(16, dtype=np.float64) * 2.0 / 32.0)
    shared["inv2"] = (inv / (2 * np.pi)).astype(f)[None, :]
    kq = np.arange(128)
    m_own = np.where(kq[None, :] >= kq[:, None], 0.0, NEG).astype(f)
    m_prev = np.where(kq[None, :] <= kq[:, None], 0.0, NEG).astype(f)
    m_none = np.full((128, 128), NEG, f)
    positions = np.asarray(positions, np.int32)
    c = np.asarray(c, f)
    maps = []
    for core in range(8):
        b, qt = divmod(core, 4)
        t0 = qt * NT
        m = dict(shared)
        m["x_own"] = np.ascontiguousarray(x[b, t0:t0 + NT])
        m["x_halo"] = np.ascontiguousarray(x[b, t0 - NT:t0]) if qt > 0 else np.zeros((NT, D), f)
        m["cT"] = np.ascontiguousarray(c[b].reshape(16, 128).T)
        pl = np.zeros(NTL, np.int32)
        pl[NT:] = positions[b, t0:t0 + NT]
        if qt > 0:
            pl[:NT] = positions[b, t0 - NT:t0]
        m["pos"] = np.ascontiguousarray(pl.reshape(32, 128).T)
        m["masks"] = np.ascontiguousarray(np.stack([m_own, m_prev, m_prev if qt > 0 else m_none], axis=1))
        tg = t0 + np.arange(NT)
        m["invcnt"] = np.stack([1.0 / np.minimum(tg + 1, p) for p in (2, 4, 8, 16)]).astype(f)
        m["flag"] = np.full((128, 1), 1.0 if qt > 0 else 0.0, f)
        maps.append(m)
    return maps


_NC = None


def kernel(**inputs):
    global _NC
    maps = make_in_maps(**inputs)
    nc = build()
    res = run_bass_kernel_spmd(nc, maps, core_ids=list(range(8)))
    out = np.zeros((2, 8192, D), np.float32)
    for core in range(8):
        b, qt = divmod(core, 4)
        out[b, qt * NT:(qt + 1) * NT] = res.results[core]["out"]
    return out
```

```python
import numpy as np
import concourse.bass as bass
import concourse.mybir as mybir
from concourse.bass_utils import run_bass_kernel_spmd
from contextlib import ExitStack

F32 = mybir.dt.float32
BF16 = mybir.dt.bfloat16
I32 = mybir.dt.int32
U32 = mybir.dt.uint32
AF = mybir.ActivationFunctionType
ALU = mybir.AluOpType
AX = mybir.AxisListType

NDS = 48
D = 2048
NT = 2048
NTL = 4096
NEG = -30000.0


class Buf:
    __slots__ = ("w", "r")

    def __init__(self):
        self.w = None
        self.r = []


def _compress(toks):
    best = {}
    for t in toks:
        key = (t[0], t[1])
        if key not in best or best[key][2] < t[2]:
            best[key] = t
    return list(best.values())


class KB:
    def __init__(self, nc):
        self.nc = nc
        self.engs = {"pe": nc.tensor, "dve": nc.vector, "act": nc.scalar,
                     "pool": nc.gpsimd, "sp": nc.sync}
        self.sem = {e: nc.alloc_semaphore(name="s_" + e) for e in self.engs}
        self.cnt = {e: 0 for e in self.engs}
        self.seen = {e: {} for e in self.engs}
        self.dsem = [nc.alloc_semaphore(name="d%d" % i) for i in range(NDS)]
        self.dcnt = [0] * NDS
        self.dnext = 0
        self.ninst = 0

    def _wait(self, eng, tok):
        kind, src, k = tok
        if kind == "e":
            if src == eng and eng == "pe":
                return
            key = src
            sem = self.sem[src]
        else:
            key = ("d", src)
            sem = self.dsem[src]
        if self.seen[eng].get(key, 0) >= k:
            return
        self.engs[eng].wait_ge(sem, k)
        self.seen[eng][key] = k

    def _deps(self, eng, reads, writes):
        deps = []
        for b in reads:
            if b.w is not None:
                deps.append(b.w)
        for b in writes:
            if b.w is not None:
                deps.append(b.w)
            deps.extend(b.r)
        for d in deps:
            self._wait(eng, d)

    def _mark(self, tok, reads, writes):
        for b in reads:
            b.r.append(tok)
            if len(b.r) > 48:
                b.r = _compress(b.r)
        for b in writes:
            b.w = tok
            b.r = []

    def op(self, eng, fn, reads=(), writes=()):
        self._deps(eng, reads, writes)
        ins = fn(self.engs[eng])
        self.cnt[eng] += 1
        ins.then_inc(self.sem[eng], 1)
        tok = ("e", eng, self.cnt[eng])
        self._mark(tok, reads, writes)
        self.ninst += 1
        return tok

    def dma(self, q, out, in_, reads=(), writes=(), **kw):
        self._deps(q, reads, writes)
        j = self.dnext
        self.dnext = (self.dnext + 1) % NDS
        if self.dcnt[j] > 0:
            self._wait(q, ("d", j, self.dcnt[j]))
        ins = self.engs[q].dma_start(out=out, in_=in_, **kw)
        self.dcnt[j] += 16
        ins.then_inc(self.dsem[j], 16)
        tok = ("d", j, self.dcnt[j])
        self._mark(tok, reads, writes)
        self.ninst += 1
        return tok

    def barrier(self, engines=None):
        engines = engines or list(self.engs)
        for e in engines:
            for s in self.engs:
                if self.cnt[s] > 0 and not (s == e and e == "pe"):
                    self._wait(e, ("e", s, self.cnt[s]))
            for j in range(NDS):
                if self.dcnt[j] > 0:
                    self._wait(e, ("d", j, self.dcnt[j]))


def build(debug=False, stop_after=None):
    nc = bass.Bass("TRN2", target_bir_lowering=False)

    def din(name, shape, dt=F32):
        return nc.dram_tensor(name, shape, dt, kind="ExternalInput").ap()

    def dscr(name, shape, dt=F32):
        return nc.dram_tensor(name, shape, dt, kind="Internal").ap()

    x_own = din("x_own", [NT, D])
    x_halo = din("x_halo", [NT, D])
    cT_d = din("cT", [128, 16])
    pos_d = din("pos", [128, 32], I32)
    w_mod = din("w_mod", [D, 6 * D])
    b_mod = din("b_mod", [1, 6 * D])
    g1_d = din("norm1_g", [1, D])
    w_in = din("w_in", [D, 4096])
    w_pool = din("w_pool", [4, 256, 256])
    psT_d = din("psT", [128, 8])
    w_out = din("w_out", [D, D])
    g2_d = din("norm2_g", [1, D])
    w_query = din("w_query", [D, D])
    skT_d = din("skT", [128, 16, 128])
    peer_u = din("peer_u", [16384, D])
    peer_v = din("peer_v", [16384, D])
    fg_d = din("final_g", [1, D])
    ident_d = din("ident", [128, 128])
    masks_d = din("masks", [128, 3, 128])
    inv2_d = din("inv2", [1, 16])
    invcnt_d = din("invcnt", [4, NT])
    flag_d = din("flag", [128, 1])
    iota_d = din("iota", [1, 128])
    out_d = nc.dram_tensor("out", [NT, D], F32, kind="ExternalOutput").ap()

    okind = "ExternalOutput" if debug else "Internal"
    MODB = dscr("MODB", [128, 6 * D])
    QTd = dscr("QTd", [8, 128, NT], BF16)
    KTd = dscr("KTd", [8, 128, NTL], BF16)
    Vd = dscr("Vd", [NTL, 1024], BF16)
    UTd = dscr("UTd", [8, 128, 128 + NT])
    X1d = nc.dram_tensor("X1d", [NT, D], F32, kind=okind).ap()
    H2Td = dscr("H2Td", [16, 128, NT], BF16)
    Q2Td = dscr("Q2Td", [16, 128, NT], BF16)
    Gd = dscr("Gd", [128, 128, NT], BF16)
    MIXd = dscr("MIXd", [16, 128, NT], BF16)

    kb = KB(nc)
    op = kb.op
    dma = kb.dma
    B = Buf

    def phase_end():
        kb.barrier()

    with ExitStack() as es0:
        def sb0(n, s, d):
            return es0.enter_context(nc.sbuf_tensor(n, s, d))

        identf = sb0("identf", [128, 128], F32)
        identb = sb0("identb", [128, 128], BF16)
        onesb = sb0("onesb", [128, 128], BF16)
        iota_f = sb0("iota_f", [128, 128], F32)
        flag = sb0("flag_s", [128, 1], F32)
        b_const = B()
        dma("sp", identf[:], ident_d[:, :], writes=[b_const])
        dma("sp", iota_f[:], iota_d.partition_broadcast(128), writes=[b_const])
        dma("sp", flag[:], flag_d[:, :], writes=[b_const])
        op("dve", lambda e: e.tensor_copy(out=identb[:], in_=identf[:]), [b_const], [b_const])
        op("dve", lambda e: e.memset(onesb[:], 1.0), [], [b_const])
        iota_b = sb0("iota_b", [128, 128], BF16)
        op("dve", lambda e: e.tensor_copy(out=iota_b[:], in_=iota_f[:]), [b_const], [b_const])
        CONST = [b_const]

        with ExitStack() as es:
            def sb(n, s, d):
                return es.enter_context(nc.sbuf_tensor(n, s, d))

            def ps(n, s, d):
                return es.enter_context(nc.psum_tensor(n, s, d))
            cT = sb("cT_s", [128, 16], F32)
            sc = sb("sc_s", [128, 16], F32)
            srep = sb("srep", [128, 16, 128], BF16)
            NWM = 3
            wm = [sb("wm%d" % i, [128, 16, 512], BF16) for i in range(NWM)]
            bwm = [B() for _ in range(NWM)]
            bmb = sb("bmb", [128, 6 * D], F32)
            bbmb = B()
            mo = [sb("mo%d" % i, [128, 512], F32) for i in range(2)]
            bmo = [B(), B()]
            pm = [ps("pm%d" % i, [128, 512], F32) for i in range(2)]
            bpm = [B(), B()]
            b0 = B()
            dma("sp", cT[:], cT_d[:, :], writes=[b0])
            dma("sp", bmb[:], b_mod.partition_broadcast(128), writes=[bbmb])
            op("act", lambda e: e.activation(out=sc[:], in_=cT[:], func=AF.Silu), [b0], [b0])
            op("dve", lambda e: e.tensor_copy(out=srep[:], in_=sc[:].unsqueeze(2).to_broadcast([128, 16, 128])), [b0], [b0])

            def lw(g):
                dma("pool", wm[g % NWM][:], w_mod[:, g * 512:(g + 1) * 512].rearrange("(c p) n -> p c n", p=128), writes=[bwm[g % NWM]])
            lw(0)
            lw(1)
            for g in range(24):
                s = g % 2
                w3 = g % NWM
                if g + 2 < 24:
                    lw(g + 2)
                for c in range(16):
                    op("pe", lambda e: e.matmul(pm[s][:], lhsT=srep[:, c, :], rhs=wm[w3][:, c, :], start=(c == 0), stop=(c == 15)),
                       [b0, bwm[w3]], [bpm[s]])
                op("dve", lambda e: e.tensor_tensor(out=mo[s][:], in0=pm[s][:], in1=bmb[:, g * 512:(g + 1) * 512], op=ALU.add),
                   [bpm[s], bbmb], [bmo[s]])
                dma("sp", MODB[:, g * 512:(g + 1) * 512], mo[s][:], reads=[bmo[s]])
            phase_end()
        if stop_after == 0:
            return nc

        with ExitStack() as esM:

            with ExitStack() as es:
                def sb(n, s, d):
                    return es.enter_context(nc.sbuf_tensor(n, s, d))

                def ps(n, s, d):
                    return es.enter_context(nc.psum_tensor(n, s, d))
                wib = sb("wib", [128, 16, 2048], BF16)
                bwibg = [B() for _ in range(4)]

                def load_w(pas):
                    cols = [1024, 1536, 2048, 2560] if pas == 0 else [0, 512, 3072, 3584]
                    for wc in range(4):
                        c0 = cols[wc]
                        dma("pool", wib[:, :, wc * 512:(wc + 1) * 512],
                            w_in[:, c0:c0 + 512].rearrange("(c p) n -> p c n", p=128), writes=[bwibg[wc]])
                A1 = sb("A1", [128, D], F32)
                B1 = sb("B1", [128, D], F32)
                bA = B()
                g1b = sb("g1b", [128, D], F32)
                dma("sp", A1[:, 0:D], MODB[:, D:2 * D], writes=[bA])
                dma("sp", B1[:, 0:D], MODB[:, 0:D], writes=[bA])
                dma("sp", g1b[:], g1_d.partition_broadcast(128), writes=[bA])
                op("dve", lambda e: e.scalar_tensor_tensor(out=A1[:, 0:D], in0=A1[:, 0:D], scalar=1.0, in1=g1b[:],
                                                           op0=ALU.add, op1=ALU.mult), [bA], [bA])
                posi = sb("posi", [128, 32], I32)
                posf = sb("posf", [128, 32], F32)
                inv2 = sb("inv2s", [128, 16], F32)
                uu = sb("uu", [128, 32, 32], F32)
                ki = sb("ki", [128, 32, 32], I32)
                kf = sb("kf", [128, 32, 32], F32)
                tab = sb("tab", [128, 32, 32], F32)
                tabq = sb("tabq", [128, 32, 32], F32)
                bt = B()
                dma("sp", posi[:], pos_d[:, :], writes=[bt])
                dma("sp", inv2[:], inv2_d.partition_broadcast(128), writes=[bt])
                T = [bt]
                op("dve", lambda e: e.tensor_copy(out=posf[:], in_=posi[:]), T, T)
                op("dve", lambda e: e.tensor_tensor(out=uu[:, :, 0:16], in0=posf[:].unsqueeze(2).to_broadcast([128, 32, 16]),
                                                    in1=inv2[:].unsqueeze(1).to_broadcast([128, 32, 16]), op=ALU.mult), T, T)
                op("dve", lambda e: e.tensor_scalar(out=uu[:, :, 16:32], in0=uu[:, :, 0:16], scalar1=0.25, scalar2=None, op0=ALU.add), T, T)
                op("dve", lambda e: e.tensor_copy(out=ki[:], in_=uu[:]), T, T)
                op("dve", lambda e: e.tensor_copy(out=kf[:], in_=ki[:]), T, T)
                op("dve", lambda e: e.tensor_tensor(out=uu[:], in0=uu[:], in1=kf[:], op=ALU.subtract), T, T)
                op("dve", lambda e: e.tensor_single_scalar(out=kf[:], in_=uu[:], scalar=0.5, op=ALU.is_gt), T, T)
                op("dve", lambda e: e.tensor_tensor(out=uu[:], in0=uu[:], in1=kf[:], op=ALU.subtract), T, T)
                op("dve", lambda e: e.tensor_single_scalar(out=kf[:], in_=uu[:], scalar=-0.5, op=ALU.is_lt), T, T)
                op("dve", lambda e: e.tensor_tensor(out=uu[:], in0=uu[:], in1=kf[:], op=ALU.add), T, T)
                op("act", lambda e: e.activation(out=tab[:], in_=uu[:], func=AF.Sin, scale=2 * np.pi), T, T)
                op("dve", lambda e: e.tensor_scalar(out=tabq[:], in0=tab[:], scalar1=128 ** -0.5, scalar2=None, op0=ALU.mult), T, T)

                xt = [sb("xt%d" % i, [128, D], F32) for i in range(2)]
                bxt = [B(), B()]
                ss = [sb("ss%d" % i, [128, 1], F32) for i in range(2)]
                bss = [B(), B()]
                hf = [g1b, sb("hf1", [128, D], F32)]
                bhf = [bA, B()]
                hb = [sb("hb%d" % i, [128, D], BF16) for i in range(2)]
                bhb = [B(), B()]
                hT = [sb("hT%d" % i, [128, 16, 128], BF16) for i in range(2)]
                bhT = [B(), B()]
                zr = [sb("zr%d" % i, [128, 1024], F32) for i in range(2)]
                bzr = [B(), B()]
                zu = sb("zu", [128, 1024], F32)
                bzu = B()
                vb = [sb("vb%d" % i, [128, 1024], BF16) for i in range(2)]
                bvb = [B(), B()]
                rb = [sb("rb%d" % i, [128, 1024], BF16) for i in range(2)]
                brb = [B(), B()]
                rt = [sb("rt%d" % i, [128, 8, 16], F32) for i in range(4)]
                brt = B()
                rT = [sb("rTa%d" % i, [128, 8, 128], BF16) for i in range(2)]
                uT = [sb("uT%d" % i, [128, 8, 128], F32) for i in range(2)]
                brT, buT = [B(), B()], [B(), B()]
                pT = ps("pT", [128, 16 * 128], BF16)
                bpT = B()
                pz = [ps("pz%d" % i, [128, 512], F32) for i in range(3)]
                bpz = [B(), B(), B()]
                pqk = ps("pqk", [128, 8 * 128], BF16)
                bpqk = B()
                pu = ps("pu", [128, 8 * 128], F32)
                bpu = B()
                zc = [0]

                def Nn(pas, lt, s):
                    own = lt >= 16
                    src = x_own[(lt - 16) * 128:(lt - 15) * 128, :] if own else x_halo[lt * 128:(lt + 1) * 128, :]
                    dma("pool", xt[s][:], src, writes=[bxt[s]])
                    op("act", lambda e: e.activation(out=hf[s][:], in_=xt[s][:], func=AF.Square, accum_out=ss[s][:]),
                       [bxt[s]], [bhf[s], bss[s]])
                    op("act", lambda e: e.activation(out=ss[s][:], in_=ss[s][:], func=AF.Sqrt, scale=1.0 / D, bias=1e-6),
                       [bss[s]], [bss[s]])
                    op("dve", lambda e: e.reciprocal(out=ss[s][:], in_=ss[s][:]), [bss[s]], [bss[s]])
                    op("dve", lambda e: e.scalar_tensor_tensor(out=hf[s][:], in0=xt[s][:], scalar=ss[s][:], in1=A1[:, 0:D],
                                                               op0=ALU.mult, op1=ALU.mult), [bxt[s], bss[s], bA], [bhf[s]])
                    op("dve", lambda e: e.tensor_tensor(out=hb[s][:], in0=hf[s][:], in1=B1[:, 0:D], op=ALU.add), [bhf[s], bA], [bhb[s]])

                def TH(pas, lt, s):
                    for c in range(16):
                        op("pe", lambda e: e.transpose(out=pT[:, c * 128:(c + 1) * 128], in_=hb[s][:, c * 128:(c + 1) * 128], identity=identb[:]),
                           [bhb[s]] + CONST, [bpT])
                    op("act", lambda e: e.copy(out=hT[s][:].rearrange("p c t -> p (c t)"), in_=pT[:]), [bpT], [bhT[s]])

                def MM(pas, lt, s, mid=None):
                    own = lt >= 16
                    if pas == 0:
                        groups = [(0, "r"), (1, "r"), (2, "v"), (3, "v")]
                    else:
                        groups = [(0, "r"), (1, "r"), (2, "u"), (3, "u")] if own else [(2, "u"), (3, "u")]
                    for wc, kind in groups:
                        z = zc[0] % 3
                        zc[0] += 1
                        for c in range(16):
                            op("pe", lambda e: e.matmul(pz[z][:], lhsT=hT[s][:, c, :], rhs=wib[:, c, wc * 512:(wc + 1) * 512],
                                                        start=(c == 0), stop=(c == 15)), [bhT[s], bwibg[wc]], [bpz[z]])
                        half = (wc % 2) * 512
                        if kind == "r":
                            op("act", lambda e: e.copy(out=zr[s][:, half:half + 512], in_=pz[z][:]), [bpz[z]], [bzr[s]])
                        elif kind == "v":
                            op("act", lambda e: e.copy(out=vb[s][:, half:half + 512], in_=pz[z][:]), [bpz[z]], [bvb[s]])
                        else:
                            op("act", lambda e: e.copy(out=zu[:, half:half + 512], in_=pz[z][:]), [bpz[z]], [bzu])
                        if kind == "r" and wc == 1:
                            rot(pas, lt, s)
                        if mid is not None and wc == 2:
                            mid()
                            mid = None
                    if mid is not None:
                        mid()
                    if pas == 0:
                        dma("sp", Vd[lt * 128:(lt + 1) * 128, :], vb[s][:], reads=[bvb[s]])

                def rot(pas, lt, s):
                    table, scale = (tab, 1.0) if pas == 0 else (tabq, 128 ** -0.5)
                    zv = zr[s][:].rearrange("p (h d) -> p h d", h=8)
                    dv = rb[s][:].rearrange("p (h d) -> p h d", h=8)
                    sn = table[:, lt, 0:16].unsqueeze(1).to_broadcast([128, 8, 16])
                    cs = table[:, lt, 16:32].unsqueeze(1).to_broadcast([128, 8, 16])
                    x1 = zv[:, :, 0:16]
                    x2 = zv[:, :, 16:32]
                    R = [bzr[s], bt, brt]
                    op("dve", lambda e: e.tensor_tensor(out=rt[0][:], in0=x1, in1=cs, op=ALU.mult), R, [brt])
                    op("dve", lambda e: e.tensor_tensor(out=rt[1][:], in0=x2, in1=sn, op=ALU.mult), R, [brt])
                    op("dve", lambda e: e.tensor_tensor(out=rt[2][:], in0=x2, in1=cs, op=ALU.mult), R, [brt])
                    op("dve", lambda e: e.tensor_tensor(out=rt[3][:], in0=x1, in1=sn, op=ALU.mult), R, [brt])
                    op("dve", lambda e: e.tensor_tensor(out=dv[:, :, 0:16], in0=rt[0][:], in1=rt[1][:], op=ALU.subtract), [brt], [brb[s]])
                    op("dve", lambda e: e.tensor_tensor(out=dv[:, :, 16:32], in0=rt[2][:], in1=rt[3][:], op=ALU.add), [brt], [brb[s]])
                    op("act", lambda e: e.activation(out=dv[:, :, 32:128], in_=zv[:, :, 32:128], func=AF.Copy, scale=scale), [bzr[s]], [brb[s]])

                def RO(pas, lt, s):
                    own = lt >= 16
                    if pas == 0 or own:
                        for h in range(8):
                            op("pe", lambda e: e.transpose(out=pqk[:, h * 128:(h + 1) * 128], in_=rb[s][:, h * 128:(h + 1) * 128], identity=identb[:]),
                               [brb[s]] + CONST, [bpqk])
                        op("act", lambda e: e.copy(out=rT[s][:].rearrange("p h t -> p (h t)"), in_=pqk[:]), [bpqk], [brT[s]])
                        if pas == 0:
                            dma("sp", KTd[:, :, lt * 128:(lt + 1) * 128].rearrange("h d t -> d h t"), rT[s][:], reads=[brT[s]])
                        else:
                            dma("sp", QTd[:, :, (lt - 16) * 128:(lt - 15) * 128].rearrange("h d t -> d h t"), rT[s][:], reads=[brT[s]])
                    if pas == 1:
                        for j in range(8):
                            op("pe", lambda e: e.transpose(out=pu[:, j * 128:(j + 1) * 128], in_=zu[:, j * 128:(j + 1) * 128], identity=identf[:]),
                               [bzu] + CONST, [bpu])
                        op("act", lambda e: e.copy(out=uT[s][:].rearrange("p c t -> p (c t)"), in_=pu[:]), [bpu], [buT[s]])
                        dma("sp", UTd[:, :, (lt - 15) * 128:(lt - 14) * 128].rearrange("c p t -> p c t"), uT[s][:], reads=[buT[s]])

                for pas in (0, 1):
                    load_w(pas)
                    tl_ = list(range(32)) if pas == 0 else list(range(15, 32))
                    n_ = len(tl_)
                    Nn(pas, tl_[0], 0)
                    TH(pas, tl_[0], 0)
                    if n_ > 1:
                        Nn(pas, tl_[1], 1)
                    for k in range(n_):
                        s = k % 2
                        mid_ = None
                        if k + 1 < n_:
                            mid_ = (lambda kk=k: TH(pas, tl_[kk + 1], (kk + 1) % 2))
                        MM(pas, tl_[k], s, mid=mid_)
                        if k + 2 < n_:
                            Nn(pas, tl_[k + 2], s)
                        RO(pas, tl_[k], s)
                phase_end()
            if stop_after == 1:
                return nc

            with ExitStack() as es:
                def sb(n, s, d):
                    return es.enter_context(nc.sbuf_tensor(n, s, d))

                def ps(n, s, d):
                    return es.enter_context(nc.psum_tensor(n, s, d))
                maskf = sb("maskf", [128, 3, 128], F32)
                maskb = sb("maskb", [128, 3, 128], BF16)
                bmk = B()
                dma("sp", maskf[:], masks_d[:, :, :], writes=[bmk])
                op("dve", lambda e: e.tensor_copy(out=maskb[:], in_=maskf[:]), [bmk], [bmk])
                QT = [sb("QT%d" % i, [128, NT], BF16) for i in range(2)]
                KT = [sb("KT%d" % i, [128, NTL], BF16) for i in range(2)]
                V1 = [sb("V1_%d" % i, [128, 17, 128], BF16) for i in range(2)]
                V4 = [sb("V4_%d" % i, [128, 4, 5, 128], BF16) for i in range(2)]
                V16 = [sb("V16_%d" % i, [128, 16, 2, 128], BF16) for i in range(2)]
                bld = [B(), B()]
                accs = [sb("acc%d" % i, [128, 2, NT], F32) for i in range(2)]
                baccs = [B(), B()]
                rec = sb("rec", [128, NT], F32)
                brec = B()
                mixh = [sb("mixh%d" % i, [128, NT], BF16) for i in range(2)]
                bmixh = [B(), B()]
                PT = [sb("PT%d" % i, [128, 2, 128], BF16) for i in range(2)]
                bPT = [B(), B()]
                pS = [ps("pS%d" % i, [128, 512], F32) for i in range(2)]
                bpS = [B(), B()]
                pO = [ps("pO%d" % i, [128, 512], F32) for i in range(2)]
                bpO = [B(), B()]
                def loads(h):
                    s = h % 2
                    W = [bld[s]]
                    dma("sp", QT[s][:], QTd[h, :, :], writes=W)
                    dma("sp", KT[s][:], KTd[h, :, :], writes=W)
                    hc = slice(h * 128, (h + 1) * 128)
                    dma("sp", V1[s][:], Vd[1920:4096, hc].rearrange("(b k) d -> k b d", k=128), writes=W)
                    v4src = Vd[1536:4096, hc].rearrange("(b k r) d -> r k b d", k=128, r=4)
                    for r in range(4):
                        dma("sp", V4[s][:, r, :, :], v4src[r], writes=W)
                    v16src = Vd[0:4096, hc].rearrange("(b k r) d -> r k b d", k=128, r=16)
                    for r in range(16):
                        dma("sp", V16[s][:, r, :, :], v16src[r], writes=W)

                def iters(h):
                    s = h % 2
                    out = []
                    for dil in (1, 4, 16):
                        nblk = NT // (128 * dil)
                        qv = QT[s][:].rearrange("d (n q r) -> d r n q", r=dil, q=128)
                        kv = KT[s][:].rearrange("d (b k r) -> d r b k", r=dil, k=128)
                        av = accs[s][:].rearrange("d s (n q r) -> d r n s q", r=dil, q=128)
                        for r in range(dil):
                            for n in range(nblk):
                                bo = nblk + n
                                bp = bo - 1
                                if dil == 1:
                                    vp, vo = V1[s][:, bp - 15, :], V1[s][:, bo - 15, :]
                                elif dil == 4:
                                    vp, vo = V4[s][:, r, bp - 3, :], V4[s][:, r, bo - 3, :]
                                else:
                                    vp, vo = V16[s][:, r, bp, :], V16[s][:, r, bo, :]
                                out.append(dict(s=s, q=qv[:, r, n, :], kp=kv[:, r, bp, :], ko=kv[:, r, bo, :], vp=vp, vo=vo,
                                                mprev=(maskb[:, 2, :] if n == 0 else maskb[:, 1, :]), acc=av[:, r, n, :, :], first=(dil == 1)))
                    return out

                def Sst(it, z):
                    s = it["s"]
                    R = [bld[s], bmk] + CONST
                    op("pe", lambda e: e.matmul(pS[z][:, 0:128], lhsT=it["kp"], rhs=it["q"], start=True, stop=False), R, [bpS[z]])
                    op("pe", lambda e: e.matmul(pS[z][:, 0:128], lhsT=identb[:], rhs=it["mprev"], start=False, stop=True), R, [bpS[z]])
                    op("pe", lambda e: e.matmul(pS[z][:, 128:256], lhsT=it["ko"], rhs=it["q"], start=True, stop=False), R, [bpS[z]])
                    op("pe", lambda e: e.matmul(pS[z][:, 128:256], lhsT=identb[:], rhs=maskb[:, 0, :], start=False, stop=True), R, [bpS[z]])
                    op("act", lambda e: e.activation(out=PT[z][:].rearrange("p a b -> p (a b)"), in_=pS[z][:, 0:256], func=AF.Exp),
                       [bpS[z]], [bPT[z]])

                def PVst(it, z):
                    s = it["s"]
                    R2 = [bld[s], bPT[z]] + CONST
                    op("pe", lambda e: e.matmul(pO[z][:, 0:128], lhsT=it["vp"], rhs=PT[z][:, 0, :], start=True, stop=False), R2, [bpO[z]])
                    op("pe", lambda e: e.matmul(pO[z][:, 0:128], lhsT=it["vo"], rhs=PT[z][:, 1, :], start=False, stop=True), R2, [bpO[z]])
                    op("pe", lambda e: e.matmul(pO[z][:, 128:256], lhsT=onesb[:], rhs=PT[z][:, 0, :], start=True, stop=False), R2, [bpO[z]])
                    op("pe", lambda e: e.matmul(pO[z][:, 128:256], lhsT=onesb[:], rhs=PT[z][:, 1, :], start=False, stop=True), R2, [bpO[z]])
                    po_v = pO[z][:, 0:256].rearrange("p (s q) -> p s q", s=2)
                    bacc = baccs[s]
                    if it["first"]:
                        op("dve", lambda e: e.tensor_copy(out=it["acc"], in_=po_v), [bpO[z]], [bacc])
                    else:
                        op("dve", lambda e: e.tensor_tensor(out=it["acc"], in0=po_v, in1=it["acc"], op=ALU.add), [bpO[z], bacc], [bacc])

                loads(0)
                for h in range(8):
                    s = h % 2
                    if h + 1 < 8:
                        loads(h + 1)
                    its = iters(h)
                    NI = len(its)
                    Sst(its[0], 0)
                    Sst(its[1], 1)
                    for i in range(NI):
                        PVst(its[i], i % 3 if False else i % 2)
                        if i + 2 < NI:
                            Sst(its[i + 2], i % 2)
                    op("act", lambda e: e.activation(out=rec[:], in_=accs[s][:, 1, :], func=AF.Ln), [baccs[s]], [brec])
                    op("act", lambda e: e.activation(out=rec[:], in_=rec[:], func=AF.Exp, scale=-1.0), [brec], [brec])
                    op("pool", lambda e: e.tensor_tensor(out=mixh[s][:], in0=accs[s][:, 0, :], in1=rec[:], op=ALU.mult),
                       [baccs[s], brec], [bmixh[s]])
                    dma("sp", MIXd[h, :, :], mixh[s][:], reads=[bmixh[s]])
                phase_end()
            if stop_after == 2:
                return nc

            with ExitStack() as esCD:
                wob = esCD.enter_context(nc.sbuf_tensor("wob", [128, 16, D], BF16))
                bwob = B()
                for c4 in range(4):
                    dma("pool", wob[:, c4 * 4:(c4 + 1) * 4, :], w_out[c4 * 512:(c4 + 1) * 512, :].rearrange("(c p) n -> p c n", p=128), writes=[bwob])
                mixT = esCD.enter_context(nc.sbuf_tensor("mixT", [128, 16, NT], BF16))
                bmix = [B() for _ in range(16)]
                bMIXd = [B() for _ in range(16)]
                for c in range(8):
                    dma("act", mixT[:, c, :], MIXd[c, :, :], writes=[bmix[c]])

                with ExitStack() as es:
                    def sb(n, s, d):
                        return es.enter_context(nc.sbuf_tensor(n, s, d))

                    def ps(n, s, d):
                        return es.enter_context(nc.psum_tensor(n, s, d))
                    NU = 128 + NT
                    psT = sb("psT_s", [128, 8], F32)
                    bps = B()
                    dma("sp", psT[:], psT_d[:, :], writes=[bps])
                    ut = [sb("ut%d" % i, [128, NU], F32) for i in range(2)]
                    but = [B(), B()]
                    sa = sb("sa", [128, NU], F32)
                    sbb = sb("sbb", [128, NU], F32)
                    bsa, bsb = B(), B()
                    icb = sb("icb", [128, NT], F32)
                    bic = B()
                    tmp = sb("ptmp", [128, NT], F32)
                    btmp = B()
                    rT = [sb("rT%d" % i, [128, NT], BF16) for i in range(2)]
                    brT = [B(), B()]
                    wpb = [sb("wpb%d" % i, [128, 2, 256], BF16) for i in range(2)]
                    bwpb = [B(), B()]
                    pp = [ps("pp%d" % i, [128, 512], F32) for i in range(2)]
                    bpp = [B(), B()]
                    mo_c = [sb("mo_c%d" % i, [128, NT], BF16) for i in range(2)]
                    bmo_c = [B(), B()]

                    def cloads(g):
                        dma("pool", wpb[g % 2][:], w_pool[g].rearrange("(cc p) e -> p cc e", p=128), writes=[bwpb[g % 2]])
                        dma("sp", icb[:], invcnt_d[g:g + 1, :].partition_broadcast(128), writes=[bic])
                        for cc in range(2):
                            dma("sp", ut[cc][:], UTd[2 * g + cc, :, :], writes=[but[cc]])
                    pit = 0
                    cloads(0)
                    for g in range(4):
                        p = (2, 4, 8, 16)[g]
                        for cc in range(2):
                            u = ut[cc]
                            op("dve", lambda e: e.tensor_scalar(out=u[:, 0:128], in0=u[:, 0:128], scalar1=flag[:], scalar2=None, op0=ALU.mult),
                               [but[cc]] + CONST, [but[cc]])
                            cur, bcur = u, but[cc]
                            nxt = [(sa, bsa), (sbb, bsb)]
                            step = 1
                            k = 0
                            while step < p:
                                lo = 2 * step - 1
                                dst, bdst = nxt[k % 2]
                                k += 1
                                op("dve", lambda e: e.tensor_tensor(out=dst[:, lo:NU], in0=cur[:, lo:NU], in1=cur[:, lo - step:NU - step], op=ALU.add),
                                   [bcur], [bdst])
                                cur, bcur = dst, bdst
                                step *= 2
                            op("dve", lambda e: e.tensor_tensor(out=tmp[:], in0=cur[:, 128:NU], in1=icb[:], op=ALU.mult), [bcur, bic], [btmp])
                            op("pool", lambda e: e.tensor_tensor(out=rT[cc][:], in0=tmp[:], in1=u[:, 128:NU], op=ALU.subtract),
                               [btmp, but[cc]], [brT[cc]])
                        if g + 1 < 4:
                            cloads(g + 1)
                        for ec in range(2):
                            for tg in range(4):
                                z = pit % 2
                                pit += 1
                                for cc in range(2):
                                    op("pe", lambda e: e.matmul(pp[z][:], lhsT=wpb[g % 2][:, cc, ec * 128:(ec + 1) * 128], rhs=rT[cc][:, tg * 512:(tg + 1) * 512],
                                                                start=(cc == 0), stop=(cc == 1)), [bwpb[g % 2], brT[cc]], [bpp[z]])
                                op("act", lambda e: e.activation(out=mo_c[ec][:, tg * 512:(tg + 1) * 512], in_=pp[z][:], func=AF.Copy,
                                                                 scale=psT[:, 2 * g + ec:2 * g + ec + 1]), [bpp[z], bps], [bmo_c[ec]])
                            chn = 8 + 2 * g + ec
                            dma("sp", MIXd[chn, :, :], mo_c[ec][:], reads=[bmo_c[ec]], writes=[bMIXd[chn]])
                            dma("sp", mixT[:, chn, :], MIXd[chn, :, :], reads=[bMIXd[chn]], writes=[bmix[chn]])
                    phase_end()
                if stop_after == 3:
                    return nc

                with ExitStack() as es:
                    def sb(n, s, d):
                        return es.enter_context(nc.sbuf_tensor(n, s, d))

                    def ps(n, s, d):
                        return es.enter_context(nc.psum_tensor(n, s, d))
                    gate1 = sb("gate1", [128, D], F32)
                    bg1 = B()
                    dma("sp", gate1[:], MODB[:, 2 * D:3 * D], writes=[bg1])
                    xt = [sb("xtd%d" % i, [128, D], F32) for i in range(2)]
                    bxt = [B(), B()]
                    t1 = [sb("t1d%d" % i, [128, D], F32) for i in range(2)]
                    bt1 = [B(), B()]
                    po = [ps("po%d" % i, [128, 512], F32) for i in range(4)]
                    bpo = [B() for _ in range(4)]
                    for tt in range(16):
                        s = tt % 2
                        dma("act", xt[s][:], x_own[tt * 128:(tt + 1) * 128, :], writes=[bxt[s]])
                        for ng in range(4):
                            for c in range(16):
                                op("pe", lambda e: e.matmul(po[ng][:], lhsT=mixT[:, c, tt * 128:(tt + 1) * 128], rhs=wob[:, c, ng * 512:(ng + 1) * 512],
                                                            start=(c == 0), stop=(c == 15)), [bmix[c], bwob], [bpo[ng]])
                            op("dve", lambda e: e.tensor_tensor(out=t1[s][:, ng * 512:(ng + 1) * 512], in0=po[ng][:], in1=gate1[:, ng * 512:(ng + 1) * 512],
                                                                op=ALU.mult), [bpo[ng], bg1], [bt1[s]])
                        op("pool", lambda e: e.tensor_tensor(out=t1[s][:], in0=t1[s][:], in1=xt[s][:], op=ALU.add), [bt1[s], bxt[s]], [bt1[s]])
                        dma("sp", X1d[tt * 128:(tt + 1) * 128, :], t1[s][:], reads=[bt1[s]])
                    phase_end()
        if stop_after == 4:
            return nc

        with ExitStack() as es:
            def sb(n, s, d):
                return es.enter_context(nc.sbuf_tensor(n, s, d))

            def ps(n, s, d):
                return es.enter_context(nc.psum_tensor(n, s, d))
            wqb = sb("wqb", [128, 16, D], BF16)
            bwqbg = [B() for _ in range(4)]
            for c4 in range(4):
                dma("pool", wqb[:, :, c4 * 512:(c4 + 1) * 512], w_query[:, c4 * 512:(c4 + 1) * 512].rearrange("(c p) n -> p c n", p=128), writes=[bwqbg[c4]])
            A2 = sb("A2", [128, D], F32)
            B2 = sb("B2", [128, D], F32)
            g2b = sb("g2b", [128, D], F32)
            bA = B()
            dma("sp", A2[:], MODB[:, 4 * D:5 * D], writes=[bA])
            dma("sp", B2[:], MODB[:, 3 * D:4 * D], writes=[bA])
            dma("sp", g2b[:], g2_d.partition_broadcast(128), writes=[bA])
            op("dve", lambda e: e.scalar_tensor_tensor(out=A2[:], in0=A2[:], scalar=1.0, in1=g2b[:], op0=ALU.add, op1=ALU.mult), [bA], [bA])
            NR = 4
            xt = [sb("xte%d" % i, [128, D], F32) for i in range(NR)]
            bxt = [B() for _ in range(NR)]
            ss = [sb("sse%d" % i, [128, 1], F32) for i in range(NR)]
            bss = [B() for _ in range(NR)]
            hf = [g2b] + [sb("hfe%d" % i, [128, D], F32) for i in range(1, NR)]
            bhf = [bA] + [B() for _ in range(1, NR)]
            hb = [sb("hbe%d" % i, [128, D], BF16) for i in range(NR)]
            bhb = [B() for _ in range(NR)]
            hT4 = [sb("hT4_%d" % i, [128, 16, 512], BF16) for i in range(2)]
            bhT4 = [B(), B()]
            q2 = [sb("q2e%d" % i, [128, 4, 512], BF16) for i in range(2)]
            bq2 = [B(), B()]
            pT = [ps("pTe%d" % i, [128, 16 * 128], BF16) for i in range(2)]
            bpT = [B(), B()]
            pq = [ps("pqe%d" % i, [128, 512], F32) for i in range(4)]
            bpq = [B() for _ in range(4)]

            def Nn(tt):
                s = tt % NR
                dma("pool", xt[s][:], X1d[tt * 128:(tt + 1) * 128, :], writes=[bxt[s]])
                op("act", lambda e: e.activation(out=hf[s][:], in_=xt[s][:], func=AF.Square, accum_out=ss[s][:]), [bxt[s]], [bhf[s], bss[s]])
                op("act", lambda e: e.activation(out=ss[s][:], in_=ss[s][:], func=AF.Sqrt, scale=1.0 / D, bias=1e-6), [bss[s]], [bss[s]])
                op("dve", lambda e: e.reciprocal(out=ss[s][:], in_=ss[s][:]), [bss[s]], [bss[s]])
                op("dve", lambda e: e.scalar_tensor_tensor(out=hf[s][:], in0=xt[s][:], scalar=ss[s][:], in1=A2[:], op0=ALU.mult, op1=ALU.mult),
                   [bxt[s], bss[s], bA], [bhf[s]])
                op("dve", lambda e: e.tensor_tensor(out=hb[s][:], in0=hf[s][:], in1=B2[:], op=ALU.add), [bhf[s], bA], [bhb[s]])

            def TH(tt):
                s = tt % NR
                g = (tt // 4) % 2
                j = tt % 4
                z = tt % 2
                for c in range(16):
                    op("pe", lambda e: e.transpose(out=pT[z][:, c * 128:(c + 1) * 128], in_=hb[s][:, c * 128:(c + 1) * 128], identity=identb[:]),
                       [bhb[s]] + CONST, [bpT[z]])
                op("act", lambda e: e.copy(out=hT4[g][:, :, j * 128:(j + 1) * 128], in_=pT[z][:].rearrange("p (c t) -> p c t", c=16)), [bpT[z]], [bhT4[g]])

            def QM(grp):
                g = grp % 2
                dma("sp", H2Td[:, :, grp * 512:(grp + 1) * 512].rearrange("c p t -> p c t"), hT4[g][:], reads=[bhT4[g]])
                for hp4 in range(4):
                    z = hp4 % 2
                    for j in range(4):
                        hp = hp4 * 4 + j
                        for c in range(16):
                            op("pe", lambda e: e.matmul(pq[j][:], lhsT=wqb[:, c, hp * 128:(hp + 1) * 128], rhs=hT4[g][:, c, :],
                                                        start=(c == 0), stop=(c == 15)), [bwqbg[hp4], bhT4[g]], [bpq[j]])
                        op("act", lambda e: e.copy(out=q2[z][:, j, :], in_=pq[j][:]), [bpq[j]], [bq2[z]])
                    dma("sp", Q2Td[hp4 * 4:(hp4 + 1) * 4, :, grp * 512:(grp + 1) * 512].rearrange("c p t -> p c t"), q2[z][:], reads=[bq2[z]])

            for t_ in range(4):
                Nn(t_)
            for tt in range(16):
                TH(tt)
                if tt % 4 == 3:
                    for t_ in range(tt + 1, min(tt + 5, 16)):
                        Nn(t_)
                    QM(tt // 4)
            phase_end()
        if stop_after == 5:
            return nc

        with ExitStack() as es:
            def sb(n, s, d):
                return es.enter_context(nc.sbuf_tensor(n, s, d))

            def ps(n, s, d):
                return es.enter_context(nc.psum_tensor(n, s, d))
            skb = sb("skb", [128, 16, 128], BF16)
            bsk = B()
            dma("pool", skb[:], skT_d[:, :, :], writes=[bsk])
            q2 = [sb("q2b%d" % i, [128, 16, 128], BF16) for i in range(2)]
            bq2 = [B(), B()]
            S = [sb("S%d" % i, [128, 16, 128], F32) for i in range(2)]
            bSS = [B(), B()]
            S2 = sb("S2", [128, 16, 128], F32)
            V16 = sb("V16", [128, 16, 16], F32)
            I16 = sb("I16", [128, 16, 16], U32)
            I16f = sb("I16f", [128, 16, 16], F32)
            cand = sb("cand", [128, 8, 256], F32)
            cand2 = sb("cand2", [128, 8, 256], F32)
            T16 = sb("T16", [128, 8, 16], F32)
            P16 = sb("P16", [128, 8, 16], U32)
            Pa = sb("Pa", [128, 8, 16], U32)
            Pb = sb("Pb", [128, 8, 16], U32)
            Paf = sb("Paf", [128, 8, 16], F32)
            Pbf = sb("Pbf", [128, 8, 16], F32)
            negm = sb("negm", [128, 8], F32)
            Z = sb("Z", [128, 8], F32)
            E = sb("E", [128, 8, 16], F32)
            oh = sb("oh", [128, 8, 16, 16], F32)
            tok = sb("tok", [128, 3, 128], F32)
            tokT = [sb("tokT%d" % i, [128, 3, 128], F32) for i in range(2)]
            btokT = [B(), B()]
            bS = B()
            bH = [B() for _ in range(16)]
            bG2 = [B() for _ in range(8)]
            TB = 16
            NB = 4
            Ab = [sb("Ab%d" % i, [128, TB, 128], BF16) for i in range(NB)]
            Bb = [sb("Bb%d" % i, [128, TB, 128], BF16) for i in range(NB)]
            bAb = [[B() for _ in range(TB)] for _ in range(NB)]
            bBb = [B() for _ in range(NB)]
            Gs = [sb("Gs%d" % i, [128, 128, 128], BF16) for i in range(2)]
            bGs = [B(), B()]
            pG = [ps("pG%d" % i, [128, 1024], F32) for i in range(2)]
            bpG = [B(), B()]
            pSc = ps("pSc", [128, 1024], F32)
            bpSc = B()
            pX = ps("pXb", [128, 512], F32)
            bpX = B()
            iota_b16 = iota_f[:, 0:16]

            def scores(tt):
                s = tt % 2
                dma("sp", q2[s][:], Q2Td[:, :, tt * 128:(tt + 1) * 128].rearrange("c p t -> p c t"), writes=[bq2[s]])
                for rnd in range(2):
                    for h8 in range(8):
                        hp = rnd * 8 + h8
                        op("pe", lambda e: e.matmul(pSc[:, h8 * 128:(h8 + 1) * 128], lhsT=q2[s][:, hp, :], rhs=skb[:, hp, :], start=True, stop=True),
                           [bq2[s], bsk], [bpSc])
                    op("act", lambda e: e.copy(out=S[s][:, rnd * 8:(rnd + 1) * 8, :].rearrange("p a n -> p (a n)"), in_=pSc[:]), [bpSc], [bSS[s]])

            gcount = [0]

            def R1(tt):
                s = tt % 2
                Sc = S[s]
                R = [bS]
                HB = [[bH[hp]] for hp in range(16)]
                for hp in range(16):
                    op("dve", lambda e: e.max(out=V16[:, hp, 0:8], in_=Sc[:, hp, :]), [bSS[s]], HB[hp])
                for hp in range(16):
                    op("dve", lambda e: e.max_index(out=I16[:, hp, 0:8], in_max=V16[:, hp, 0:8], in_values=Sc[:, hp, :]), [bSS[s]] + HB[hp], HB[hp])
                for hp in range(16):
                    op("dve", lambda e: e.match_replace(out=S2[:, hp, :], in_to_replace=V16[:, hp, 0:8], in_values=Sc[:, hp, :], imm_value=-1e30),
                       [bSS[s]] + HB[hp], HB[hp])
                for hp in range(16):
                    op("dve", lambda e: e.max(out=V16[:, hp, 8:16], in_=S2[:, hp, :]), HB[hp], HB[hp])
                for hp in range(16):
                    op("dve", lambda e: e.max_index(out=I16[:, hp, 8:16], in_max=V16[:, hp, 8:16], in_values=S2[:, hp, :]), HB[hp], HB[hp])
                ALLH = [bH[hp] for hp in range(16)]
                op("dve", lambda e: e.tensor_copy(out=I16f[:], in_=I16[:]), ALLH, R)
                Vv = V16[:].rearrange("p (h two) k -> p h two k", two=2)
                cv = cand[:].rearrange("p h (a b) -> p h a b", a=16)
                op("dve", lambda e: e.tensor_tensor(out=cv, in0=Vv[:, :, 0, :].unsqueeze(3).to_broadcast([128, 8, 16, 16]),
                                                    in1=Vv[:, :, 1, :].unsqueeze(2).to_broadcast([128, 8, 16, 16]), op=ALU.add), ALLH + R, R)
                GB = [[bG2[h]] for h in range(8)]
                for h in range(8):
                    op("dve", lambda e: e.max(out=T16[:, h, 0:8], in_=cand[:, h, :]), R, GB[h])
                for h in range(8):
                    op("dve", lambda e: e.max_index(out=P16[:, h, 0:8], in_max=T16[:, h, 0:8], in_values=cand[:, h, :]), R + GB[h], GB[h])
                for h in range(8):
                    op("dve", lambda e: e.match_replace(out=cand2[:, h, :], in_to_replace=T16[:, h, 0:8], in_values=cand[:, h, :], imm_value=-1e30),
                       R + GB[h], GB[h])
                for h in range(8):
                    op("dve", lambda e: e.max(out=T16[:, h, 8:16], in_=cand2[:, h, :]), GB[h], GB[h])
                for h in range(8):
                    op("dve", lambda e: e.max_index(out=P16[:, h, 8:16], in_max=T16[:, h, 8:16], in_values=cand2[:, h, :]), GB[h], GB[h])
                ALLG = [bG2[h] for h in range(8)]
                op("dve", lambda e: e.tensor_scalar(out=negm[:], in0=T16[:, :, 0], scalar1=-1.0, scalar2=None, op0=ALU.mult), R + ALLG, R + ALLG)
                for h in range(8):
                    op("act", lambda e: e.activation(out=E[:, h, :], in_=T16[:, h, :], func=AF.Exp, bias=negm[:, h:h + 1], accum_out=Z[:, h:h + 1]),
                       R + [bG2[h]], R)

            def R2(tt):
                s = tt % 2
                R = [bS]
                Iv = I16f[:].rearrange("p (h two) k -> p h two k", two=2)
                op("dve", lambda e: e.reciprocal(out=Z[:], in_=Z[:]), R, R)
                gv = tok[:, 2, :].rearrange("p (h k) -> p h k", h=8)
                op("dve", lambda e: e.tensor_tensor(out=gv, in0=E[:], in1=Z[:].unsqueeze(2).to_broadcast([128, 8, 16]), op=ALU.mult), R, R)
                ALLG = [bG2[h] for h in range(8)]
                op("dve", lambda e: e.tensor_single_scalar(out=Pa[:], in_=P16[:], scalar=4, op=ALU.logical_shift_right), R + ALLG, R)
                op("dve", lambda e: e.tensor_single_scalar(out=Pb[:], in_=P16[:], scalar=15, op=ALU.bitwise_and), R + ALLG, R)
                op("dve", lambda e: e.tensor_copy(out=Paf[:], in_=Pa[:]), R, R)
                op("dve", lambda e: e.tensor_copy(out=Pbf[:], in_=Pb[:]), R, R)
                io4 = iota_b16.unsqueeze(1).unsqueeze(1).to_broadcast([128, 8, 16, 16])
                for which, Pf in ((0, Paf), (1, Pbf)):
                    op("dve", lambda e: e.tensor_tensor(out=oh[:], in0=Pf[:].unsqueeze(3).to_broadcast([128, 8, 16, 16]), in1=io4, op=ALU.is_equal), R + CONST, R)
                    op("dve", lambda e: e.tensor_tensor(out=oh[:], in0=oh[:], in1=Iv[:, :, which, :].unsqueeze(2).to_broadcast([128, 8, 16, 16]),
                                                        op=ALU.mult), R, R)
                    op("dve", lambda e: e.tensor_reduce(out=tok[:, which, :].rearrange("p (h k) -> p h k", h=8), in_=oh[:], axis=AX.X, op=ALU.add), R, R)
                for j in range(3):
                    op("pe", lambda e: e.transpose(out=pX[:, j * 128:(j + 1) * 128], in_=tok[:, j, :], identity=identf[:]), R + CONST, [bpX])
                op("act", lambda e: e.copy(out=tokT[s][:].rearrange("p a t -> p (a t)"), in_=pX[:, 0:384]), [bpX], [btokT[s]])

            def OH(tt, sb_lo, sb_hi):
                s = tt % 2
                g = tt % 2
                tT = tokT[s]
                RT = [btokT[s]]
                for sbi in range(sb_lo, sb_hi):
                    a = sbi % NB
                    t0 = sbi * TB
                    iob = iota_f[:].unsqueeze(1).to_broadcast([128, TB, 128])
                    op("dve", lambda e: e.tensor_tensor(out=Bb[a][:], in0=iob, in1=tT[:, 1, t0:t0 + TB].unsqueeze(2).to_broadcast([128, TB, 128]),
                                                        op=ALU.is_equal), RT + CONST, [bBb[a]])
                    for tl in range(TB):
                        op("dve", lambda e: e.tensor_scalar(out=Ab[a][:, tl, :], in0=iota_b[:], scalar1=tT[:, 0, t0 + tl:t0 + tl + 1],
                                                            scalar2=tT[:, 2, t0 + tl:t0 + tl + 1], op0=ALU.is_equal, op1=ALU.mult),
                           RT + CONST, [bAb[a][tl]])
                    for q8 in range(TB // 8):
                        pz = gcount[0] % 2
                        gcount[0] += 1
                        for tl in range(8):
                            tloc = q8 * 8 + tl
                            bank = tl // 4
                            oap = pG[pz][:, bank * 512:(bank + 1) * 512].rearrange("j (i t) -> j t i", t=4)[:, tl % 4, :]
                            op("pe", lambda e: e.matmul(oap, lhsT=Bb[a][:, tloc, :], rhs=Ab[a][:, tloc, :], start=True, stop=True),
                               [bBb[a], bAb[a][tloc]], [bpG[pz]])
                        tg0 = t0 + q8 * 8
                        op("act", lambda e: e.copy(out=Gs[g][:, :, tg0:tg0 + 8].rearrange("j i (b t) -> j b i t", b=2),
                                                   in_=pG[pz][:].rearrange("j (b i t) -> j b i t", b=2, t=4)), [bpG[pz]], [bGs[g]])

            def Gst(tt):
                g = tt % 2
                for i8 in range(8):
                    dma("sp" if i8 % 2 else "act", Gd[i8 * 16:(i8 + 1) * 16, :, tt * 128:(tt + 1) * 128].rearrange("i j t -> j i t"),
                        Gs[g][:, i8 * 16:(i8 + 1) * 16, :], reads=[bGs[g]])

            scores(0)
            R1(0)
            R2(0)
            scores(1)
            NSB = 128 // TB
            for tt in range(16):
                if tt + 1 < 16:
                    R1(tt + 1)
                OH(tt, 0, NSB - 2)
                if tt + 1 < 16:
                    R2(tt + 1)
                if tt + 2 < 16:
                    scores(tt + 2)
                OH(tt, NSB - 2, NSB)
                Gst(tt)
            phase_end()
        if stop_after == 6:
            return nc

        with ExitStack() as es:
            def sb(n, s, d):
                return es.enter_context(nc.sbuf_tensor(n, s, d))

            def ps(n, s, d):
                return es.enter_context(nc.psum_tensor(n, s, d))
            TP = 1024
            NTT = TP // 128
            GI = 4
            NCH = 128
            yacc = sb("yacc", [128, NTT, D], F32)
            byacc = [[B(), B()] for _ in range(NTT)]
            h2T = sb("h2T", [128, 16, TP], BF16)
            bh2T = B()
            NU_ = 4
            ub = [sb("ub%d" % i, [128, D], BF16) for i in range(NU_)]
            bub = [B() for _ in range(NU_)]
            UT = [sb("UT%d" % i, [128, 16, 128], BF16) for i in range(2)]
            bUT = [B(), B()]
            Vb = [sb("Vb%d" % i, [128, GI, D], BF16) for i in range(2)]
            bVb = [[B() for _ in range(GI)] for _ in range(2)]
            gst = [sb("gst%d" % i, [128, TP], BF16) for i in range(NU_)]
            bgst = [B() for _ in range(NU_)]
            ga = [sb("ga%d" % i, [128, 512], BF16) for i in range(2)]
            bga = [B(), B()]
            NW = GI + 1
            Wg = sb("Wg", [128, NW, TP], BF16)
            bWg = [B() for _ in range(NW)]
            gate2 = sb("gate2", [128, D], F32)
            fgb = sb("fgb", [128, D], F32)
            bgf = B()
            xb_ = [sb("xbf%d" % i, [128, D], F32) for i in range(2)]
            bxb_ = [B(), B()]
            ssf = [sb("ssf%d" % i, [128, 1], F32) for i in range(2)]
            bssf = [B(), B()]
            dma("sp", gate2[:], MODB[:, 5 * D:6 * D], writes=[bgf])
            dma("sp", fgb[:], fg_d.partition_broadcast(128), writes=[bgf])
            pTu = ps("pTu", [128, 16 * 128], BF16)
            bpTu = B()
            pa = [ps("pa%d" % i, [128, 512], F32) for i in range(4)]
            bpa = [B() for _ in range(4)]
            py = [ps("py%d" % i, [128, 512], F32) for i in range(2)]
            bpy = [B(), B()]
            ycnt = [0]
            def final_tile(tb_, tt):
                s = tt % 2
                xb, bxb = xb_[s], bxb_[s]
                r0 = tb_ + tt * 128
                YB = byacc[tt]
                dma("sp", xb[:], X1d[r0:r0 + 128, :], writes=[bxb])
                op("dve", lambda e: e.tensor_tensor(out=yacc[:, tt, :], in0=yacc[:, tt, :], in1=gate2[:], op=ALU.mult), YB + [bgf], YB)
                op("dve", lambda e: e.tensor_tensor(out=yacc[:, tt, :], in0=yacc[:, tt, :], in1=xb[:], op=ALU.add), YB + [bxb], YB)
                op("act", lambda e: e.activation(out=xb[:], in_=yacc[:, tt, :], func=AF.Square, accum_out=ssf[s][:]), YB, [bxb, bssf[s]])
                op("act", lambda e: e.activation(out=ssf[s][:], in_=ssf[s][:], func=AF.Sqrt, scale=1.0 / D, bias=1e-6), [bssf[s]], [bssf[s]])
                op("dve", lambda e: e.reciprocal(out=ssf[s][:], in_=ssf[s][:]), [bssf[s]], [bssf[s]])
                op("dve", lambda e: e.scalar_tensor_tensor(out=xb[:], in0=yacc[:, tt, :], scalar=ssf[s][:], in1=fgb[:], op0=ALU.mult, op1=ALU.mult),
                   YB + [bssf[s], bgf], [bxb])
                dma("sp", out_d[r0:r0 + 128, :], xb[:], reads=[bxb])

            pending_final = []
            hoisted = False
            for pas in range(NT // TP):
                tbase = pas * TP
                if not hoisted:
                    dma("sp", h2T[:], H2Td[:, :, tbase:tbase + TP].rearrange("c p t -> p c t"), writes=[bh2T])

                def loadU(i):
                    dma("pool", ub[i % NU_][:], peer_u[i * 128:(i + 1) * 128, :], writes=[bub[i % NU_]])
                    dma("sp", gst[i % NU_][:], Gd[i, :, tbase:tbase + TP], writes=[bgst[i % NU_]])

                def loadV(grp):
                    for gi in range(GI):
                        i = grp * GI + gi
                        dma("pool", Vb[grp % 2][:, gi, :], peer_v[i * 128:(i + 1) * 128, :], writes=[bVb[grp % 2][gi]])

                def Tr(i):
                    u2 = i % 2
                    for dc in range(16):
                        op("pe", lambda e: e.transpose(out=pTu[:, dc * 128:(dc + 1) * 128], in_=ub[i % NU_][:, dc * 128:(dc + 1) * 128], identity=identb[:]),
                           [bub[i % NU_]] + CONST, [bpTu])
                    op("act", lambda e: e.copy(out=UT[u2][:].rearrange("p a j -> p (a j)"), in_=pTu[:]), [bpTu], [bUT[u2]])

                def Amm(i):
                    u2 = i % 2
                    ws = i % NW
                    for dc in range(16):
                        for tg in range(2):
                            pz = (i % 2) * 2 + tg
                            op("pe", lambda e: e.matmul(pa[pz][:], lhsT=UT[u2][:, dc, :], rhs=h2T[:, dc, tg * 512:(tg + 1) * 512],
                                                        start=(dc == 0), stop=(dc == 15)), [bUT[u2], bh2T], [bpa[pz]])
                    for tg in range(2):
                        pz = (i % 2) * 2 + tg
                        op("act", lambda e: e.activation(out=ga[tg][:], in_=pa[pz][:], func=AF.Gelu), [bpa[pz]], [bga[tg]])
                        op("dve", lambda e: e.tensor_tensor(out=Wg[:, ws, tg * 512:(tg + 1) * 512], in0=ga[tg][:], in1=gst[i % NU_][:, tg * 512:(tg + 1) * 512],
                                                            op=ALU.mult), [bga[tg], bgst[i % NU_]], [bWg[ws]])

                def Ymm(grp, final_tb=None):
                    vb_ = Vb[grp % 2]
                    bv_ = bVb[grp % 2]
                    for tt in range(NTT):
                        for qd in range(4):
                            z = ycnt[0] % 2
                            ycnt[0] += 1
                            for gi in range(GI):
                                ws = (grp * GI + gi) % NW
                                op("pe", lambda e: e.matmul(py[z][:], lhsT=Wg[:, ws, tt * 128:(tt + 1) * 128],
                                                            rhs=vb_[:, gi, qd * 512:(qd + 1) * 512], start=(gi == 0), stop=(gi == GI - 1)),
                                   [bWg[ws], bv_[gi]], [bpy[z]])
                            ysl = yacc[:, tt, qd * 512:(qd + 1) * 512]
                            if grp == 0:
                                op("dve", lambda e: e.tensor_copy(out=ysl, in_=py[z][:]), [bpy[z]], [byacc[tt][qd // 2]])
                            else:
                                op("dve", lambda e: e.tensor_tensor(out=ysl, in0=py[z][:], in1=ysl, op=ALU.add),
                                   [bpy[z], byacc[tt][qd // 2]], [byacc[tt][qd // 2]])
                        if final_tb is not None:
                            final_tile(final_tb, tt)

                if not hoisted:
                    loadU(0)
                    loadU(1)
                    loadU(2)
                    loadV(0)
                    loadV(1)
                    Tr(0)
                for i in range(NCH):
                    if i + 1 < NCH:
                        Tr(i + 1)
                    if i + 3 < NCH:
                        loadU(i + 3)
                    Amm(i)
                    if pending_final and i < GI:
                        for _ in range(NTT // GI):
                            final_tile(*pending_final.pop(0))
                    if i % GI == 0 and i > 0:
                        g_ = i // GI - 1
                        Ymm(g_)
                        if g_ + 2 < NCH // GI:
                            loadV(g_ + 2)
                last_pass = (pas + 1 == NT // TP)
                hoisted = False
                if not last_pass:
                    tb_cur = tbase
                    tbase = (pas + 1) * TP
                    dma("sp", h2T[:], H2Td[:, :, tbase:tbase + TP].rearrange("c p t -> p c t"), writes=[bh2T])
                    loadU(0)
                    loadU(1)
                    loadU(2)
                    loadV(0)
                    Tr(0)
                    tbase = tb_cur
                    hoisted = True
                Ymm(NCH // GI - 1, final_tb=(tbase if last_pass else None))
                if hoisted:
                    loadV(1)
                if pas + 1 < NT // TP:
                    pending_final = [(tbase, tt) for tt in range(NTT)]
                else:
                    pass
            phase_end()
    return nc


def make_in_maps(x, c, positions, w_mod, b_mod, norm1_g, w_in, w_pool, pool_scale,
                 w_out, norm2_g, w_query, sub_keys, peer_u, peer_v, final_g):
    f = np.float32
    x = np.asarray(x, f)
    shared = {
        "w_mod": np.ascontiguousarray(np.asarray(w_mod, f)[0]),
        "b_mod": np.ascontiguousarray(np.asarray(b_mod, f)[0][None, :]),
        "norm1_g": np.ascontiguousarray(np.asarray(norm1_g, f)[0][None, :]),
        "w_in": np.ascontiguousarray(np.asarray(w_in, f)[0]),
        "w_pool": np.ascontiguousarray(np.asarray(w_pool, f)[0]),
        "psT": np.ascontiguousarray(np.asarray(pool_scale, f)[0].reshape(8, 128).T),
        "w_out": np.ascontiguousarray(np.asarray(w_out, f)[0]),
        "norm2_g": np.ascontiguousarray(np.asarray(norm2_g, f)[0][None, :]),
        "w_query": np.ascontiguousarray(np.asarray(w_query, f)[0]),
        "skT": np.ascontiguousarray(np.asarray(sub_keys, f)[0].reshape(16, 128, 128).transpose(2, 0, 1)),
        "peer_u": np.ascontiguousarray(np.asarray(peer_u, f)[0]),
        "peer_v": np.ascontiguousarray(np.asarray(peer_v, f)[0]),
        "final_g": np.ascontiguousarray(np.asarray(final_g, f)[None, :]),
        "ident": np.eye(128, dtype=f),
        "iota": np.arange(128, dtype=f)[None, :],
    }
    inv = 500000.0 ** (-np.arange(16, dtype=np.float64) * 2.0 / 32.0)
    shared["inv2"] = (inv / (2 * np.pi)).astype(f)[None, :]
    kq = np.arange(128)
    m_own = np.where(kq[None, :] >= kq[:, None], 0.0, NEG).astype(f)
    m_prev = np.where(kq[None, :] <= kq[:, None], 0.0, NEG).astype(f)
    m_none = np.full((128, 128), NEG, f)
    positions = np.asarray(positions, np.int32)
    c = np.asarray(c, f)
    maps = []
    for core in range(8):
        b, qt = divmod(core, 4)
        t0 = qt * NT
        m = dict(shared)
        m["x_own"] = np.ascontiguousarray(x[b, t0:t0 + NT])
        m["x_halo"] = np.ascontiguousarray(x[b, t0 - NT:t0]) if qt > 0 else np.zeros((NT, D), f)
        m["cT"] = np.ascontiguousarray(c[b].reshape(16, 128).T)
        pl = np.zeros(NTL, np.int32)
        pl[NT:] = positions[b, t0:t0 + NT]
        if qt > 0:
            pl[:NT] = positions[b, t0 - NT:t0]
        m["pos"] = np.ascontiguousarray(pl.reshape(32, 128).T)
        m["masks"] = np.ascontiguousarray(np.stack([m_own, m_prev, m_prev if qt > 0 else m_none], axis=1))
        tg = t0 + np.arange(NT)
        m["invcnt"] = np.stack([1.0 / np.minimum(tg + 1, p) for p in (2, 4, 8, 16)]).astype(f)
        m["flag"] = np.full((128, 1), 1.0 if qt > 0 else 0.0, f)
        maps.append(m)
    return maps


_NC = None


def kernel(**inputs):
    global _NC
    maps = make_in_maps(**inputs)
    nc = build()
    res = run_bass_kernel_spmd(nc, maps, core_ids=list(range(8)))
    out = np.zeros((2, 8192, D), np.float32)
    for core in range(8):
        b, qt = divmod(core, 4)
        out[b, qt * NT:(qt + 1) * NT] = res.results[core]["out"]
    return out
```
